# Optimizing a Trainium2 kernel written in Bass

```python
import math
import jax
import jax.numpy as jnp
from jax import lax
import numpy as np


D_MODEL = 1024
BATCH = 16
SEQ = 4096
DEPTH = 4

CTX_LEN = 256
GRID_W = 64
QBLK = 128
ROPE_BASE = 10000.0
NORM_EPS = 1e-6
NEG_INF = -1e30

SWA_HEADS = 8
SWA_KV_HEADS = 2
SWA_HEAD_DIM = 64
SWA_WINDOW = 128

MLA_HEADS = 8
MLA_Q_RANK = 256
MLA_KV_RANK = 128
MLA_NOPE_DIM = 64
MLA_ROPE_DIM = 32
MLA_V_DIM = 64

DIFF_HEADS = 8
DIFF_HEAD_DIM = 64
DIFF_EPS = 1e-5

N_GROUPS = 4
EXPERTS_PER_GROUP = 8
N_EXPERTS = N_GROUPS * EXPERTS_PER_GROUP
TOP_K = 2
EXPERT_FF = 512
MOE_BLK = 128

AB_IN_WIDTHS = (SWA_HEADS * SWA_HEAD_DIM, SWA_KV_HEADS * SWA_HEAD_DIM, SWA_KV_HEADS * SWA_HEAD_DIM, MLA_Q_RANK, MLA_KV_RANK, MLA_ROPE_DIM)
AB_IN_WIDTH = 512 + 128 + 128 + 256 + 128 + 32
AB_OUT_WIDTH = SWA_HEADS * SWA_HEAD_DIM + MLA_HEADS * MLA_V_DIM
DIFF_IN_WIDTHS = (DIFF_HEADS * 2 * DIFF_HEAD_DIM, DIFF_HEADS * 2 * DIFF_HEAD_DIM, DIFF_HEADS * 2 * DIFF_HEAD_DIM)
DIFF_IN_WIDTH = 3 * DIFF_HEADS * 2 * DIFF_HEAD_DIM
DIFF_OUT_WIDTH = DIFF_HEADS * 2 * DIFF_HEAD_DIM

kernel_name = 'hybrid_swa_mla_diff_hmoe_dit'


def _rmsnorm(x, g, eps=NORM_EPS):
    xf = x.astype(jnp.float32)
    y = xf * lax.rsqrt(jnp.mean(xf * xf, axis=-1, keepdims=True) + eps)
    return (y * g.astype(jnp.float32)).astype(x.dtype)


def _modulate(h, shift, scale):
    return h * (1 + scale) + shift


def _split(x, widths):
    idx = [int(i) for i in np.cumsum(widths)[:-1]]
    return jnp.split(x, idx, axis=-1)


def _axial_rope_tables(n_tokens, dim):
    rows = n_tokens // GRID_W
    row = jnp.repeat(jnp.arange(rows, dtype=jnp.float32), GRID_W)
    col = jnp.tile(jnp.arange(GRID_W, dtype=jnp.float32), rows)
    nf = dim // 4
    inv = ROPE_BASE ** (-jnp.arange(nf, dtype=jnp.float32) / nf)
    ang = jnp.concatenate([row[:, None] * inv, col[:, None] * inv], axis=-1)
    return jnp.cos(ang), jnp.sin(ang)


def _rope(x, tables):
    cos, sin = tables
    cos = cos[None, :, None, :]
    sin = sin[None, :, None, :]
    half = x.shape[-1] // 2
    x1 = x[..., :half].astype(jnp.float32)
    x2 = x[..., half:].astype(jnp.float32)
    return jnp.concatenate([x1 * cos - x2 * sin, x2 * cos + x1 * sin], axis=-1).astype(x.dtype)


def _dense_attention(q, k, v, scale):
    b, sq, h, d = q.shape
    dv = v.shape[-1]
    nb = sq // QBLK
    qb = jnp.moveaxis(q.reshape(b, nb, QBLK, h, d), 1, 0)

    def block(qi):
        s = jnp.einsum('bqhd,bkhd->bhqk', qi, k, preferred_element_type=jnp.float32) * scale
        p = jax.nn.softmax(s, axis=-1).astype(v.dtype)
        return jnp.einsum('bhqk,bkhd->bqhd', p, v)

    out = lax.map(block, qb)
    return jnp.moveaxis(out, 0, 1).reshape(b, sq, h, dv)


def _sink_softmax(s, sink_g):
    sk = jnp.broadcast_to(sink_g.astype(jnp.float32)[None, :, :, None, None], s.shape[:-1] + (1,))
    p = jax.nn.softmax(jnp.concatenate([s, sk], axis=-1), axis=-1)
    return p[..., :-1]


def _window_attention_with_sink(q, k, v, k_ctx, v_ctx, sink):
    b, s_len, hq, d = q.shape
    hkv = k.shape[2]
    g = hq // hkv
    dv = v.shape[-1]
    nb = s_len // QBLK
    kb_len = QBLK + 2 * SWA_WINDOW
    scale = d ** -0.5
    pad = ((0, 0), (SWA_WINDOW, SWA_WINDOW), (0, 0), (0, 0))
    kp = jnp.pad(k, pad)
    vp = jnp.pad(v, pad)
    qb = jnp.moveaxis(q.reshape(b, nb, QBLK, hkv, g, d), 1, 0)
    sink_g = sink.reshape(hkv, g)

    def block(args):
        i, qi = args
        start = i * QBLK
        kb = lax.dynamic_slice_in_dim(kp, start, kb_len, axis=1)
        vb = lax.dynamic_slice_in_dim(vp, start, kb_len, axis=1)
        qpos = start + jnp.arange(QBLK)
        kpos = start - SWA_WINDOW + jnp.arange(kb_len)
        mask = (jnp.abs(qpos[:, None] - kpos[None, :]) <= SWA_WINDOW) & ((kpos >= 0) & (kpos < s_len))[None, :]
        s_loc = jnp.einsum('bqhgd,bkhd->bhgqk', qi, kb, preferred_element_type=jnp.float32) * scale
        s_loc = jnp.where(mask, s_loc, NEG_INF)
        s_ctx = jnp.einsum('bqhgd,bkhd->bhgqk', qi, k_ctx, preferred_element_type=jnp.float32) * scale
        p = _sink_softmax(jnp.concatenate([s_loc, s_ctx], axis=-1), sink_g).astype(v.dtype)
        return (jnp.einsum('bhgqk,bkhd->bqhgd', p[..., :kb_len], vb)
                + jnp.einsum('bhgqk,bkhd->bqhgd', p[..., kb_len:], v_ctx))

    out = lax.map(block, (jnp.arange(nb), qb))
    return jnp.moveaxis(out, 0, 1).reshape(b, s_len, hq * dv)


def _context_sink_attention(q, k, v, sink):
    b, c_len, hq, d = q.shape
    hkv = k.shape[2]
    g = hq // hkv
    qg = q.reshape(b, c_len, hkv, g, d)
    s = jnp.einsum('bqhgd,bkhd->bhgqk', qg, k, preferred_element_type=jnp.float32) * (d ** -0.5)
    p = _sink_softmax(s, sink.reshape(hkv, g)).astype(v.dtype)
    return jnp.einsum('bhgqk,bkhd->bqhgd', p, v).reshape(b, c_len, hq * v.shape[-1])


def _swa_mla_mixer(h_lat, h_ctx, w_in, q_norm, w_uq, kv_norm, w_ukv, sink, w_out, rope_a, rope_b, ctx_out):
    def project(h):
        b, n, _ = h.shape
        aq, ak, av, cq, ckv, kr = _split(h @ w_in, AB_IN_WIDTHS)
        aq = aq.reshape(b, n, SWA_HEADS, SWA_HEAD_DIM)
        ak = ak.reshape(b, n, SWA_KV_HEADS, SWA_HEAD_DIM)
        av = av.reshape(b, n, SWA_KV_HEADS, SWA_HEAD_DIM)
        q = (_rmsnorm(cq, q_norm) @ w_uq).reshape(b, n, MLA_HEADS, MLA_NOPE_DIM + MLA_ROPE_DIM)
        kv = (_rmsnorm(ckv, kv_norm) @ w_ukv).reshape(b, n, MLA_HEADS, MLA_NOPE_DIM + MLA_V_DIM)
        return (aq, ak, av, q[..., :MLA_NOPE_DIM], q[..., MLA_NOPE_DIM:],
                kv[..., :MLA_NOPE_DIM], kr[:, :, None, :], kv[..., MLA_NOPE_DIM:])

    aq_l, ak_l, av_l, qn_l, qr_l, kn_l, kr_l, mv_l = project(h_lat)
    aq_c, ak_c, av_c, qn_c, qr_c, kn_c, kr_c, mv_c = project(h_ctx)
    aq_l = _rope(aq_l, rope_a)
    ak_l = _rope(ak_l, rope_a)
    qr_l = _rope(qr_l, rope_b)
    kr_l = _rope(kr_l, rope_b)

    def mla_qk(qn, qr, kn, kr):
        q = jnp.concatenate([qn, qr], axis=-1)
        k = jnp.concatenate([kn, jnp.broadcast_to(kr, kn.shape[:-1] + (MLA_ROPE_DIM,))], axis=-1)
        return q, k

    q_l, k_l = mla_qk(qn_l, qr_l, kn_l, kr_l)
    q_c, k_c = mla_qk(qn_c, qr_c, kn_c, kr_c)
    mla_scale = (MLA_NOPE_DIM + MLA_ROPE_DIM) ** -0.5
    b, s_len = h_lat.shape[0], h_lat.shape[1]
    o_a = _window_attention_with_sink(aq_l, ak_l, av_l, ak_c, av_c, sink)
    o_b = _dense_attention(q_l, jnp.concatenate([k_l, k_c], axis=1), jnp.concatenate([mv_l, mv_c], axis=1), mla_scale)
    out_lat = jnp.concatenate([o_a, o_b.reshape(b, s_len, -1)], axis=-1) @ w_out
    if not ctx_out:
        return out_lat, None
    c_len = h_ctx.shape[1]
    o_a_c = _context_sink_attention(aq_c, ak_c, av_c, sink)
    o_b_c = _dense_attention(q_c, k_c, mv_c, mla_scale).reshape(b, c_len, -1)
    out_ctx = jnp.concatenate([o_a_c, o_b_c], axis=-1) @ w_out
    return out_lat, out_ctx


def _diff_mixer(h_lat, h_ctx, w_in, lq1, lk1, lq2, lk2, subln, w_out, lambda_init, rope, ctx_out):
    def project(h):
        b, n, _ = h.shape
        q, k, v = _split(h @ w_in, DIFF_IN_WIDTHS)
        q = q.reshape(b, n, DIFF_HEADS, 2, DIFF_HEAD_DIM)
        k = k.reshape(b, n, DIFF_HEADS, 2, DIFF_HEAD_DIM)
        v = v.reshape(b, n, DIFF_HEADS, 2 * DIFF_HEAD_DIM)
        return q[..., 0, :], q[..., 1, :], k[..., 0, :], k[..., 1, :], v

    q1_l, q2_l, k1_l, k2_l, v_l = project(h_lat)
    q1_c, q2_c, k1_c, k2_c, v_c = project(h_ctx)
    q1_l = _rope(q1_l, rope)
    q2_l = _rope(q2_l, rope)
    k1_l = _rope(k1_l, rope)
    k2_l = _rope(k2_l, rope)
    lam = (jnp.exp(jnp.sum(lq1.astype(jnp.float32) * lk1.astype(jnp.float32)))
           - jnp.exp(jnp.sum(lq2.astype(jnp.float32) * lk2.astype(jnp.float32))) + lambda_init)
    scale = DIFF_HEAD_DIM ** -0.5

    def combine(a1, a2):
        o = a1 - lam.astype(a1.dtype) * a2
        o = _rmsnorm(o, subln, DIFF_EPS) * (1.0 - lambda_init)
        return o.reshape(o.shape[0], o.shape[1], -1) @ w_out

    k1_all = jnp.concatenate([k1_l, k1_c], axis=1)
    k2_all = jnp.concatenate([k2_l, k2_c], axis=1)
    v_all = jnp.concatenate([v_l, v_c], axis=1)
    out_lat = combine(_dense_attention(q1_l, k1_all, v_all, scale), _dense_attention(q2_l, k2_all, v_all, scale))
    if not ctx_out:
        return out_lat, None
    out_ctx = combine(_dense_attention(q1_c, k1_c, v_c, scale), _dense_attention(q2_c, k2_c, v_c, scale))
    return out_lat, out_ctx


def _hier_moe(h, w_group, b_group, w_expert, b_expert, w1, w3, w2):
    n_tok, d = h.shape
    g_logits = jnp.dot(h, w_group, preferred_element_type=jnp.float32) + b_group.astype(jnp.float32)
    g_prob = jax.nn.softmax(g_logits, axis=-1)
    grp = jnp.argmax(g_logits, axis=-1).astype(jnp.int32)
    p_grp = jnp.take_along_axis(g_prob, grp[:, None], axis=-1)
    e_logits = (jnp.dot(h, w_expert, preferred_element_type=jnp.float32) + b_expert.astype(jnp.float32)).reshape(n_tok, N_GROUPS, EXPERTS_PER_GROUP)
    e_logits = jnp.take_along_axis(e_logits, grp[:, None, None], axis=1)[:, 0]
    top_p, top_i = lax.top_k(jax.nn.softmax(e_logits, axis=-1), TOP_K)
    gate = p_grp * top_p / jnp.sum(top_p, axis=-1, keepdims=True)
    eid = grp[:, None] * EXPERTS_PER_GROUP + top_i.astype(jnp.int32)

    n_assign = n_tok * TOP_K
    flat_e = eid.reshape(n_assign)
    flat_tok = jnp.repeat(jnp.arange(n_tok, dtype=jnp.int32), TOP_K)
    flat_gate = gate.reshape(n_assign)
    order = jnp.argsort(flat_e)
    se = flat_e[order]
    stok = flat_tok[order]
    sgate = flat_gate[order]
    counts = jnp.zeros((N_EXPERTS,), jnp.int32).at[flat_e].add(1)
    padded = (counts + MOE_BLK - 1) // MOE_BLK * MOE_BLK
    pad_end = jnp.cumsum(padded)
    pad_start = pad_end - padded
    start = jnp.cumsum(counts) - counts
    dest = pad_start[se] + (jnp.arange(n_assign, dtype=jnp.int32) - start[se])
    n_blocks = (n_assign + N_EXPERTS * (MOE_BLK - 1) + MOE_BLK - 1) // MOE_BLK
    n_slots = n_blocks * MOE_BLK
    slot_tok = jnp.full((n_slots,), n_tok, jnp.int32).at[dest].set(stok)
    h_pad = jnp.concatenate([h, jnp.zeros((1, d), h.dtype)], axis=0)
    xb = h_pad[slot_tok].reshape(n_blocks, MOE_BLK, d)
    block_e = jnp.minimum(jnp.searchsorted(pad_end, jnp.arange(n_blocks, dtype=jnp.int32) * MOE_BLK, side='right'), N_EXPERTS - 1)

    def expert_block(args):
        xi, e = args
        return (jax.nn.silu(xi @ w1[e]) * (xi @ w3[e])) @ w2[e]

    yb = lax.map(expert_block, (xb, block_e)).reshape(n_slots, d)
    contrib = yb[dest] * sgate[:, None].astype(yb.dtype)
    return jnp.zeros((n_tok, d), h.dtype).at[stok].add(contrib)


def setup_inputs(seed: int = 0) -> dict:
    key = jax.random.key(seed)
    ks = jax.random.split(key, 30)
    d = D_MODEL
    n_even = (DEPTH + 1) // 2
    n_odd = DEPTH // 2

    def nrm(k, shape, scale):
        return jax.random.normal(k, shape, jnp.float32) * scale

    def gain(k, shape):
        return 1.0 + 0.1 * jax.random.normal(k, shape, jnp.float32)

    return {
        'x': nrm(ks[0], (BATCH, SEQ, d), 1.0),
        'c': nrm(ks[1], (BATCH, d), 1.0),
        'ctx': nrm(ks[2], (BATCH, CTX_LEN, d), 1.0),
        'c_ctx': nrm(ks[3], (d,), 1.0),
        'norm_mix': gain(ks[4], (DEPTH, d)),
        'norm_ffn': gain(ks[5], (DEPTH, d)),
        'ada_w': nrm(ks[6], (DEPTH, d, 6 * d), 0.5 * d ** -0.5),
        'ada_b': nrm(ks[7], (DEPTH, 6 * d), 0.02),
        'ab_w_in': nrm(ks[8], (n_even, d, AB_IN_WIDTH), d ** -0.5),
        'mla_q_norm': gain(ks[9], (n_even, MLA_Q_RANK)),
        'mla_w_uq': nrm(ks[10], (n_even, MLA_Q_RANK, MLA_HEADS * (MLA_NOPE_DIM + MLA_ROPE_DIM)), MLA_Q_RANK ** -0.5),
        'mla_kv_norm': gain(ks[11], (n_even, MLA_KV_RANK)),
        'mla_w_ukv': nrm(ks[12], (n_even, MLA_KV_RANK, MLA_HEADS * (MLA_NOPE_DIM + MLA_V_DIM)), MLA_KV_RANK ** -0.5),
        'swa_sink': nrm(ks[13], (n_even, SWA_HEADS), 1.0),
        'ab_w_out': nrm(ks[14], (n_even, AB_OUT_WIDTH, d), AB_OUT_WIDTH ** -0.5),
        'diff_w_in': nrm(ks[15], (n_odd, d, DIFF_IN_WIDTH), d ** -0.5),
        'diff_lambda_q1': nrm(ks[16], (n_odd, DIFF_HEAD_DIM), 0.1),
        'diff_lambda_k1': nrm(ks[17], (n_odd, DIFF_HEAD_DIM), 0.1),
        'diff_lambda_q2': nrm(ks[18], (n_odd, DIFF_HEAD_DIM), 0.1),
        'diff_lambda_k2': nrm(ks[19], (n_odd, DIFF_HEAD_DIM), 0.1),
        'diff_subln': gain(ks[20], (n_odd, 2 * DIFF_HEAD_DIM)),
        'diff_w_out': nrm(ks[21], (n_odd, DIFF_OUT_WIDTH, d), DIFF_OUT_WIDTH ** -0.5),
        'router_group_w': nrm(ks[22], (DEPTH, d, N_GROUPS), d ** -0.5),
        'router_group_b': nrm(ks[23], (DEPTH, N_GROUPS), 0.01),
        'router_expert_w': nrm(ks[24], (DEPTH, d, N_EXPERTS), d ** -0.5),
        'router_expert_b': nrm(ks[25], (DEPTH, N_EXPERTS), 0.01),
        'expert_w1': nrm(ks[26], (DEPTH, N_EXPERTS, d, EXPERT_FF), d ** -0.5),
        'expert_w3': nrm(ks[27], (DEPTH, N_EXPERTS, d, EXPERT_FF), d ** -0.5),
        'expert_w2': nrm(ks[28], (DEPTH, N_EXPERTS, EXPERT_FF, d), EXPERT_FF ** -0.5),
        'final_norm': gain(ks[29], (d,)),
    }


def reference(x, c, ctx, c_ctx, norm_mix, norm_ffn, ada_w, ada_b, ab_w_in, mla_q_norm, mla_w_uq, mla_kv_norm, mla_w_ukv, swa_sink, ab_w_out, diff_w_in, diff_lambda_q1, diff_lambda_k1, diff_lambda_q2, diff_lambda_k2, diff_subln, diff_w_out, router_group_w, router_group_b, router_expert_w, router_expert_b, expert_w1, expert_w3, expert_w2, final_norm):
    b, s_len, d = x.shape
    rope_64 = _axial_rope_tables(s_len, SWA_HEAD_DIM)
    rope_32 = _axial_rope_tables(s_len, MLA_ROPE_DIM)
    sc = jax.nn.silu(c)
    scc = jax.nn.silu(c_ctx)
    x_lat = x
    x_ctx = ctx
    n_lat = b * s_len
    for l in range(DEPTH):
        last = l == DEPTH - 1
        mod_l = [m[:, None, :] for m in jnp.split(sc @ ada_w[l] + ada_b[l], 6, axis=-1)]
        mod_c = jnp.split(scc @ ada_w[l] + ada_b[l], 6, axis=-1)
        h_lat = _modulate(_rmsnorm(x_lat, norm_mix[l]), mod_l[0], mod_l[1])
        h_ctx = _modulate(_rmsnorm(x_ctx, norm_mix[l]), mod_c[0], mod_c[1])
        j = l // 2
        if l % 2 == 0:
            o_lat, o_ctx = _swa_mla_mixer(h_lat, h_ctx, ab_w_in[j], mla_q_norm[j], mla_w_uq[j], mla_kv_norm[j], mla_w_ukv[j], swa_sink[j], ab_w_out[j], rope_64, rope_32, not last)
        else:
            lambda_init = 0.8 - 0.6 * math.exp(-0.3 * l)
            o_lat, o_ctx = _diff_mixer(h_lat, h_ctx, diff_w_in[j], diff_lambda_q1[j], diff_lambda_k1[j], diff_lambda_q2[j], diff_lambda_k2[j], diff_subln[j], diff_w_out[j], lambda_init, rope_64, not last)
        x_lat = x_lat + mod_l[2] * o_lat
        h_lat = _modulate(_rmsnorm(x_lat, norm_ffn[l]), mod_l[3], mod_l[4])
        if last:
            tokens = h_lat.reshape(n_lat, d)
        else:
            x_ctx = x_ctx + mod_c[2] * o_ctx
            h_ctx = _modulate(_rmsnorm(x_ctx, norm_ffn[l]), mod_c[3], mod_c[4])
            tokens = jnp.concatenate([h_lat.reshape(n_lat, d), h_ctx.reshape(-1, d)], axis=0)
        y = _hier_moe(tokens, router_group_w[l], router_group_b[l], router_expert_w[l], router_expert_b[l], expert_w1[l], expert_w3[l], expert_w2[l])
        x_lat = x_lat + mod_l[5] * y[:n_lat].reshape(b, s_len, d)
        if not last:
            x_ctx = x_ctx + mod_c[5] * y[n_lat:].reshape(x_ctx.shape)
    return _rmsnorm(x_lat, final_norm)
```

```python
import math
import contextlib
import numpy as np
import ml_dtypes
import concourse.bass as bass
import concourse.mybir as mybir
from concourse.bass_utils import run_bass_kernel_spmd

F32 = mybir.dt.float32
BF16 = mybir.dt.bfloat16
I32 = mybir.dt.int32
U8 = mybir.dt.uint8
ALU = mybir.AluOpType
AF = mybir.ActivationFunctionType
AX = mybir.AxisListType

D = 1024
SEQ = 4096
CTX = 256
T = SEQ + CTX
NCH = T // 128
NB = 2
NE = 32
SBW = 512
NSB = (NB * T * 2 + NE * (SBW - 1) + SBW - 1) // SBW
NSLOT = NSB * SBW
EPS = 1e-6
DEPTH = 4
WCAST_DMA = True
NTILE = NB * NCH

CE = ("pe", "act", "dve", "pool")
DQ = {"sp": 24, "actq": 6, "poolq": 10}
QENG = {"sp": "sp", "actq": "act", "poolq": "pool"}


class Sched:
    def __init__(self, nc, same_engine_sync=True):
        self.nc = nc
        self.ops = {e: [] for e in ("pe", "act", "dve", "pool", "sp")}
        self.seq = {e: 0 for e in CE}
        self.dman = {q: 0 for q in DQ}
        self.last_w = {}
        self.readers = {}
        self.known = {e: {} for e in self.ops}
        self.same = same_engine_sync
        self.nops = 0

    def _deps(self, r, w):
        deps = []
        for k in r:
            if k in self.last_w:
                deps.append(self.last_w[k])
        for k in w:
            if k in self.last_w:
                deps.append(self.last_w[k])
            deps.extend(self.readers.get(k, ()))
        return deps

    def _commit(self, ident, r, w):
        for k in r:
            self.readers.setdefault(k, []).append(ident)
        for k in w:
            self.last_w[k] = ident
            self.readers[k] = []

    def _waits(self, stream, deps, own=None):
        kn = self.known[stream]
        need = {}
        for (s, v) in deps:
            if s == own and not self.same:
                continue
            if kn.get(s, 0) >= v:
                continue
            if need.get(s, 0) < v:
                need[s] = v
        for s, v in need.items():
            kn[s] = v
        return list(need.items())

    def op(self, eng, fn, r=(), w=(), sig=True):
        pr = [k for k in r if k.startswith("pb")]
        if pr:
            w = list(w) + [k for k in pr if k not in w]
            r = [k for k in r if not k.startswith("pb")]
        deps = self._deps(r, w)
        ident = (eng, self.seq[eng] + 1)
        deps = [(s, v) for (s, v) in deps if not (s == eng and v > self.seq[eng])]
        waits = self._waits(eng, deps, own=eng)
        if sig:
            self.seq[eng] += 1
            self.known[eng][eng] = max(self.known[eng].get(eng, 0), 0)
        self.ops[eng].append((waits, fn, (eng, 1) if sig else None))
        self._commit(ident, r, w)
        self.nops += 1
        return ident

    def dma(self, q, fn, r=(), w=()):
        stream = QENG[q]
        n = self.dman[q]
        self.dman[q] += 1
        R = DQ[q]
        s = "%s_%d" % (q, n % R)
        ident = (s, 16 * (n // R + 1))
        deps = self._deps(r, w)
        if stream in CE:
            deps = [(a, v) for (a, v) in deps if not (a == stream and v > self.seq[stream])]
        if n >= R:
            deps.append((s, 16 * (n // R)))
        waits = self._waits(stream, deps, own=stream if stream in CE else None)
        self.ops[stream].append((waits, fn, (s, 16)))
        self._commit(ident, r, w)
        self.nops += 1
        return ident

    def _all_idents(self):
        deps = []
        for q, R in DQ.items():
            n = self.dman[q]
            for slot in range(min(n, R)):
                cnt = (n - 1 - slot) // R + 1
                deps.append(("%s_%d" % (q, slot), 16 * cnt))
        for e in CE:
            if self.seq[e]:
                deps.append((e, self.seq[e]))
        return deps

    def barrier(self):
        deps = self._all_idents()
        for stream in self.ops:
            d = [(s, v) for (s, v) in deps if s != stream]
            waits = self._waits(stream, d)
            if waits:
                self.ops[stream].append((waits, None, None))
        self.last_w = {}
        self.readers = {}

    def drain(self):
        waits = self._waits("sp", self._all_idents())
        self.ops["sp"].append((waits, None, None))

    def check(self):
        sem = {}
        ptr = {k: 0 for k in self.ops}
        progress = True
        while progress:
            progress = False
            for k, ops in self.ops.items():
                while ptr[k] < len(ops):
                    waits, fn, inc = ops[ptr[k]]
                    if any(sem.get(s_, 0) < v for s_, v in waits):
                        break
                    if inc is not None:
                        sem[inc[0]] = sem.get(inc[0], 0) + inc[1]
                    ptr[k] += 1
                    progress = True
        stuck = {k: (ptr[k], len(v)) for k, v in self.ops.items() if ptr[k] < len(v)}
        if stuck:
            msg = []
            for k, (p, n) in stuck.items():
                waits = self.ops[k][p][0]
                msg.append("%s@%d/%d waits %s have %s" % (k, p, n, waits, [(s_, sem.get(s_, 0)) for s_, _ in waits]))
            raise RuntimeError("DEADLOCK: " + " | ".join(msg))
        return max(sem.values()) if sem else 0

    def emit(self):
        self.check()
        nc = self.nc
        names = list(CE) + ["%s_%d" % (q, i) for q, R in DQ.items() for i in range(R)]
        sems = {}
        with contextlib.ExitStack() as st:
            for nme in names:
                sems[nme] = st.enter_context(nc.semaphore("s_" + nme))
            block = st.enter_context(nc.Block())

            def run(eng, stream):
                for waits, fn, inc in self.ops[stream]:
                    for s, v in waits:
                        eng.wait_ge(sems[s], v)
                    if fn is None:
                        continue
                    ins = fn(eng)
                    if inc is not None:
                        ins.then_inc(sems[inc[0]], inc[1])

            @block.tensor
            def _(e):
                run(e, "pe")

            @block.scalar
            def _(e):
                run(e, "act")

            @block.vector
            def _(e):
                run(e, "dve")

            @block.gpsimd
            def _(e):
                run(e, "pool")

            @block.sync
            def _(e):
                run(e, "sp")


def _rope_tables(dim):
    rows = SEQ // 64
    row = np.repeat(np.arange(rows, dtype=np.float32), 64)
    col = np.tile(np.arange(64, dtype=np.float32), rows)
    nf = dim // 4
    inv = (np.float32(10000.0) ** (-np.arange(nf, dtype=np.float32) / np.float32(nf))).astype(np.float32)
    ang = np.concatenate([row[:, None] * inv, col[:, None] * inv], axis=-1).astype(np.float32)
    return np.cos(ang).astype(np.float32), np.sin(ang).astype(np.float32)


def _host_consts():
    c = {}
    cos64, sin64 = _rope_tables(64)
    cos32, sin32 = _rope_tables(32)
    ct = np.ones((128, T), np.float32)
    st = np.zeros((128, T), np.float32)
    for p in range(128):
        d = p % 64
        ct[p, :SEQ] = cos64[:, d % 32]
        st[p, :SEQ] = -sin64[:, d] if d < 32 else sin64[:, d - 32]
    c["cos64"], c["sin64"] = ct, st
    cm = np.ones((96, T), np.float32)
    sm = np.zeros((96, T), np.float32)
    for p in range(64, 96):
        r = p - 64
        cm[p, :SEQ] = cos32[:, r % 16]
        sm[p, :SEQ] = -sin32[:, r] if r < 16 else sin32[:, r - 16]
    c["cosM"], c["sinM"] = cm, sm
    c["cosR"], c["sinR"] = np.ascontiguousarray(cm[64:96]), np.ascontiguousarray(sm[64:96])
    P64 = np.zeros((128, 128), np.float32)
    for m in range(128):
        k = m + 32 if (m % 64) < 32 else m - 32
        P64[k, m] = 1
    PM = np.zeros((96, 96), np.float32)
    for m in range(64, 96):
        r = m - 64
        k = 64 + (r + 16 if r < 16 else r - 16)
        PM[k, m] = 1
    PR = np.ascontiguousarray(PM[64:96, 64:96])
    bf = ml_dtypes.bfloat16
    c["P64"], c["PM"], c["PR"] = P64.astype(bf), PM.astype(bf), PR.astype(bf)
    c["identb"] = np.eye(128, dtype=np.float32).astype(bf)
    c["identf"] = np.eye(128, dtype=np.float32)
    c["onesb"] = np.ones((128, 128), np.float32).astype(bf)
    tri = np.zeros((128, 128), np.float32)
    for k in range(128):
        tri[k, k + 1:] = 1
    c["triU"] = tri.astype(bf)
    kr = np.arange(128)[:, None]
    qr = np.arange(128)[None, :]
    c["mprev"] = np.tile((qr <= kr).astype(np.float32), (1, 4)).astype(bf)
    c["mnext"] = np.tile((kr <= qr).astype(np.float32), (1, 4)).astype(bf)
    c["sbstart"] = np.tile((np.arange(NSB, dtype=np.float32) * SBW)[None, :, None], (128, 1, NE)).astype(np.float32)
    c["pidx"] = np.arange(128, dtype=np.float32).reshape(128, 1)
    return c


CONST_SPECS = {
    "cos64": ([128, T], F32), "sin64": ([128, T], F32), "cosM": ([96, T], F32), "sinM": ([96, T], F32),
    "cosR": ([32, T], F32), "sinR": ([32, T], F32), "P64": ([128, 128], BF16), "PM": ([96, 96], BF16),
    "PR": ([32, 32], BF16), "identb": ([128, 128], BF16), "identf": ([128, 128], F32),
    "onesb": ([128, 128], BF16), "triU": ([128, 128], BF16), "mprev": ([128, 512], BF16),
    "mnext": ([128, 512], BF16), "sbstart": ([128, NSB, NE], F32), "pidx": ([128, 1], F32),
}


def _layer_specs(l):
    j = l // 2
    s = {
        "ada_w_%d" % l: [D, 6 * D], "ada_b_%d" % l: [6 * D], "norm_mix_%d" % l: [D], "norm_ffn_%d" % l: [D],
        "router_w_%d" % l: [D, 36], "router_b_%d" % l: [36],
        "w1_%d" % l: [NE * 128, 4096], "w3_%d" % l: [NE * 128, 4096], "w2_%d" % l: [NE * 128, 4096],
    }
    if l % 2 == 0:
        s.update({"ab_w_in_%d" % l: [D, 1184], "mla_q_norm_%d" % l: [256], "mla_w_uq_%d" % l: [256, 768],
                  "mla_kv_norm_%d" % l: [128], "wukv_k_%d" % l: [128, 512], "wukv_v_%d" % l: [128, 512],
                  "swa_sink_%d" % l: [8], "w_out_%d" % l: [D, D]})
    else:
        s.update({"diff_w_in_%d" % l: [D, 3072], "lq1_%d" % l: [64], "lk1_%d" % l: [64], "lq2_%d" % l: [64],
                  "lk2_%d" % l: [64], "subln_%d" % l: [128], "w_out_%d" % l: [D, D]})
    return s


def _layer_arrays(inp, l):
    j = l // 2
    a = {
        "ada_w_%d" % l: inp["ada_w"][l], "ada_b_%d" % l: inp["ada_b"][l], "norm_mix_%d" % l: inp["norm_mix"][l],
        "norm_ffn_%d" % l: inp["norm_ffn"][l],
        "router_w_%d" % l: np.ascontiguousarray(np.concatenate([inp["router_group_w"][l], inp["router_expert_w"][l]], axis=1)),
        "router_b_%d" % l: np.ascontiguousarray(np.concatenate([inp["router_group_b"][l], inp["router_expert_b"][l]], axis=0)),
        "w1_%d" % l: inp["expert_w1"][l].reshape(NE, 8, 128, 512).transpose(0, 2, 1, 3).reshape(NE * 128, 4096),
        "w3_%d" % l: inp["expert_w3"][l].reshape(NE, 8, 128, 512).transpose(0, 2, 1, 3).reshape(NE * 128, 4096),
        "w2_%d" % l: inp["expert_w2"][l].reshape(NE, 4, 128, D).transpose(0, 2, 1, 3).reshape(NE * 128, 4096),
    }
    if l % 2 == 0:
        ukv = inp["mla_w_ukv"][j].reshape(128, 8, 128)
        a.update({"ab_w_in_%d" % l: inp["ab_w_in"][j], "mla_q_norm_%d" % l: inp["mla_q_norm"][j],
                  "mla_w_uq_%d" % l: inp["mla_w_uq"][j], "mla_kv_norm_%d" % l: inp["mla_kv_norm"][j],
                  "wukv_k_%d" % l: np.ascontiguousarray(ukv[:, :, :64].reshape(128, 512)),
                  "wukv_v_%d" % l: np.ascontiguousarray(ukv[:, :, 64:].reshape(128, 512)),
                  "swa_sink_%d" % l: inp["swa_sink"][j], "w_out_%d" % l: inp["ab_w_out"][j]})
    else:
        a.update({"diff_w_in_%d" % l: inp["diff_w_in"][j], "lq1_%d" % l: inp["diff_lambda_q1"][j],
                  "lk1_%d" % l: inp["diff_lambda_k1"][j], "lq2_%d" % l: inp["diff_lambda_q2"][j],
                  "lk2_%d" % l: inp["diff_lambda_k2"][j], "subln_%d" % l: inp["diff_subln"][j],
                  "w_out_%d" % l: inp["diff_w_out"][j]})
    return {k: np.ascontiguousarray(v, dtype=np.float32) for k, v in a.items()}


class MK:
    def __init__(self, layers, final=True, debug_outs=()):
        self.layers = list(layers)
        self.final = final
        self.nc = nc = bass.Bass("TRN2", target_bir_lowering=False)
        self.S = Sched(nc)
        self.din = {}
        dt = nc.dram_tensor
        self.din["x_in"] = dt("x_in", [NB, SEQ, D], F32, kind="ExternalInput").ap()
        self.din["ctx_in"] = dt("ctx_in", [NB, CTX, D], F32, kind="ExternalInput").ap()
        self.din["ccT"] = dt("ccT", [128, 8, 3], F32, kind="ExternalInput").ap()
        self.din["final_norm"] = dt("final_norm", [D], F32, kind="ExternalInput").ap()
        for k, (shp, dty) in CONST_SPECS.items():
            self.din[k] = dt(k, shp, dty, kind="ExternalInput").ap()
        for l in self.layers:
            for k, shp in _layer_specs(l).items():
                self.din[k] = dt(k, shp, F32, kind="ExternalInput").ap()
        self.out = dt("out", [NB, SEQ, D], F32, kind="ExternalOutput").ap()

        def scr(name, shape, dty):
            kind = "ExternalOutput" if name in debug_outs else "Internal"
            return dt(name, shape, dty, kind=kind).ap()
        self.xs = scr("xs", [NB, T, D], F32)
        self.modD = scr("modD", [DEPTH, 3, 6 * D], F32)
        self.QA = scr("QA", [NB, 512, T], BF16)
        self.KA = scr("KA", [NB, 128, T], BF16)
        self.VA = scr("VA", [NB, T, 128], BF16)
        self.QM = scr("QM", [NB, 8, 96, T], BF16)
        self.KN = scr("KN", [NB, 512, T], BF16)
        self.KR = scr("KR", [NB, 32, T], BF16)
        self.VM = scr("VM", [NB, T, 512], BF16)
        self.QD = scr("QD", [NB, 8, 128, T], BF16)
        self.KD = scr("KD", [NB, 8, 128, T], BF16)
        self.VD = scr("VD", [NB, T, D], BF16)
        self.OT = scr("OT", [NB, D, T], BF16)
        self.XB = scr("XB", [NSLOT, D], BF16)
        self.H2 = scr("H2", [NB, T, D], BF16)
        self.YB = scr("YB", [NSLOT + 128, D], F32)

        self.arena = nc.alloc_sbuf_tensor("arena", [128, 200 * 1024], U8)
        self.aoff = 0
        self.pb = [nc.alloc_psum_tensor("pb%d" % i, [128, 512], F32) for i in range(8)]
        self.pbi = 0
        self.uid = 0

    def reset(self, keep=0):
        self.aoff = keep

    def tile(self, shape, dty, name=None):
        esz = {F32: 4, BF16: 2, I32: 4, U8: 1}[dty]
        n = 1
        for s in shape[1:]:
            n *= s
        nbytes = (n * esz + 31) // 32 * 32
        assert self.aoff + nbytes <= 200 * 1024, ("SBUF arena overflow", name, self.aoff, nbytes)
        ap = self.arena[:, self.aoff:self.aoff + n * esz].bitcast(dty)
        self.aoff += nbytes
        if len(shape) == 3:
            ap = ap.rearrange("p (a b) -> p a b", a=shape[1])
        elif len(shape) == 4:
            ap = ap.rearrange("p (a b c) -> p a b c", a=shape[1], b=shape[2])
        if shape[0] < 128:
            ap = ap[0:shape[0]]
        self.uid += 1
        return ap, "%s#%d" % (name or "t", self.uid)

    def bank(self):
        i = self.pbi % 8
        self.pbi += 1
        return self.pb[i], "pb%d" % i

    def mm(self, out, lhsT, rhs, start, stop, r, w, sig=None):
        if sig is None:
            sig = stop
        self.S.op("pe", lambda e: e.matmul(out=out, lhsT=lhsT, rhs=rhs, start=start, stop=stop), r, w, sig)

    def tr(self, out, in_, ident, r, w, sig=True):
        self.S.op("pe", lambda e: e.transpose(out=out, in_=in_, identity=ident), r, w, sig)

    def act(self, out, in_, func, r, w, **kw):
        self.S.op("act", lambda e: e.activation(out=out, in_=in_, func=func, **kw), r, w)

    def tt(self, eng, out, in0, in1, op, r, w):
        self.S.op(eng, lambda e: e.tensor_tensor(out=out, in0=in0, in1=in1, op=op), r, w)

    def ts(self, eng, out, in0, s1, s2, op0, op1, r, w, **kw):
        if s2 is None:
            self.S.op(eng, lambda e: e.tensor_scalar(out=out, in0=in0, scalar1=s1, scalar2=None, op0=op0, **kw), r, w)
        else:
            self.S.op(eng, lambda e: e.tensor_scalar(out=out, in0=in0, scalar1=s1, scalar2=s2, op0=op0, op1=op1, **kw), r, w)

    def stt(self, eng, out, in0, scalar, in1, op0, op1, r, w):
        self.S.op(eng, lambda e: e.scalar_tensor_tensor(out=out, in0=in0, scalar=scalar, in1=in1, op0=op0, op1=op1), r, w)

    def cp(self, eng, out, in_, r, w):
        if eng == "act":
            self.S.op("act", lambda e: e.activation(out=out, in_=in_, func=AF.Copy), r, w)
        else:
            self.S.op(eng, lambda e: e.tensor_copy(out=out, in_=in_), r, w)

    def memset(self, eng, ap, val, w):
        self.S.op(eng, lambda e: e.memset(ap, val), (), w)

    def recip(self, out, in_, r, w):
        self.S.op("dve", lambda e: e.reciprocal(out=out, in_=in_), r, w)

    def dma(self, out, in_, r, w, q="sp", slow=False):
        if slow:
            self.S.dma(q, lambda e: e.dma_start(out=out, in_=in_, allow_slow_non_contiguous=True), r, w)
        else:
            self.S.dma(q, lambda e: e.dma_start(out=out, in_=in_), r, w)

    def colvec(self, dst, dkey, src1d, r=()):
        self.dma(dst, src1d.rearrange("(c p) -> p c", p=128), r, [dkey], slow=True)

    def bcast(self, dst, dkey, src1d, r=(), parts=128):
        self.dma(dst, src1d.partition_broadcast(parts), r, [dkey])

    def load_bf16(self, dst, dkey, src, stg, eng="pool", r=()):
        sap, skey = stg
        P, n = src.shape[0], src.shape[1]
        self.dma(sap[0:P, 0:n], src, r, [skey])
        self.cp(eng, dst, sap[0:P, 0:n], [skey], [dkey])

    def setup_consts(self):
        self.reset(0)
        C = {}
        for k in ("P64", "PM", "PR", "identb", "identf", "onesb", "triU", "mprev", "mnext", "pidx"):
            shp, dty = CONST_SPECS[k]
            ap, key = self.tile(shp, dty, k)
            self.dma(ap, self.din[k], (), [key])
            C[k] = (ap, key)
        C["rt_slot"] = self.tile([128, NTILE, 2], I32, "rt_slot")
        C["rt_pos"] = self.tile([128, NTILE, 2], F32, "rt_pos")
        C["rt_oh"] = self.tile([128, NTILE, 2, NE], F32, "rt_oh")
        C["widx"] = self.tile([128, NSB], I32, "widx")
        C["rt_gate"] = self.tile([128, NTILE, 2], F32, "rt_gate")
        C["sc"] = self.tile([128, 8, 3], F32, "sc")
        C["base"] = self.tile([128, NE], F32, "base")
        self.C = C
        self.keep = self.aoff
        ap, key = C["sc"]
        self.dma(ap, self.din["ccT"], (), [key])
        self.act(ap, ap, AF.Silu, [key], [key])

    def p0_mod(self, l):
        self.reset(self.keep)
        S = self.S
        sc, sck = self.C["sc"]
        wts = [self.tile([128, 8, 512], F32, "adaw") for _ in range(2)]
        bts = [self.tile([3, 512], F32, "adab") for _ in range(2)]
        ots = [self.tile([3, 512], F32, "modo") for _ in range(2)]
        aw = self.din["ada_w_%d" % l]
        ab = self.din["ada_b_%d" % l]
        for ct in range(12):
            wt, wk = wts[ct % 2]
            bt, bk = bts[ct % 2]
            ot, ok = ots[ct % 2]
            self.dma(wt, aw[:, ct * 512:(ct + 1) * 512].rearrange("(c p) n -> p c n", p=128), (), [wk])
            self.bcast(bt, bk, ab[ct * 512:(ct + 1) * 512], parts=3)
            ps, pk = self.bank()
            for c in range(8):
                self.mm(ps[0:3, :], sc[:, c, :], wt[:, c, :], c == 0, c == 7, [sck, wk], [pk])
            self.tt("dve", ot, ps[0:3, :], bt, ALU.add, [pk, bk], [ok])
            self.dma(self.modD[l, :, ct * 512:(ct + 1) * 512], ot, [ok], ["modD"])
        S.barrier()

    def modv(self, l, r, k):
        return self.modD[l, r, k * D:(k + 1) * D]

    def norm_hT(self, src, Tw, gcol, scol, tiles, hT):
        (xt, xk), (junk, jk), (ssq, sk), (rstd, rk), (xn, xnk) = tiles
        identb, ik = self.C["identb"]
        ns = Tw // 128
        import os
        nhc = int(os.environ.get("NHCUT", "99"))
        self.dma(xt[:, 0:ns, :], src.rearrange("(s p) d -> p s d", p=128), (), [xk])
        if nhc < 2:
            return
        self.memset("pool", ssq, 0.0, [sk])
        for s in range(ns):
            self.act(junk, xt[:, s, :], AF.Square, [xk], [jk, sk], accum_out=ssq[:, s:s + 1])
        if nhc < 3:
            return
        self.act(rstd[:, 0:ns], ssq[:, 0:ns], AF.Sqrt, [sk], [rk], scale=1.0 / D, bias=self.epsap(EPS))
        self.recip(rstd[:, 0:ns], rstd[:, 0:ns], [rk], [rk])
        if nhc < 4:
            return
        for s in range(ns):
            self.ts("dve" if s % 2 == 0 else "pool", xn[:, s, :], xt[:, s, :], rstd[:, s:s + 1], None, ALU.mult, None, [xk, rk], [xnk + str(s)])
        hTa, hk = hT
        if nhc < 5:
            return
        for cp_ in range(4):
            ps, pk = self.bank()
            psb = ps[:].bitcast(BF16)
            for cc in range(2):
                c = cp_ * 2 + cc
                for s in range(ns):
                    self.tr(psb[:, cc * 512 + s * 128: cc * 512 + (s + 1) * 128], xn[:, s, c * 128:(c + 1) * 128], identb,
                            [xnk + str(s), ik], [pk], sig=(cc == 1 and s == ns - 1))
            if nhc < 6:
                continue
            for cc in range(2):
                c = cp_ * 2 + cc
                if cp_ % 2 == 0:
                    self.act(hTa[:, c, 0:Tw], psb[:, cc * 512: cc * 512 + Tw], AF.Identity, [pk, gcol[1], scol[1]], [hk + str(c)],
                             scale=gcol[0][:, c:c + 1], bias=scol[0][:, c:c + 1])
                else:
                    self.ts("dve", hTa[:, c, 0:Tw], psb[:, cc * 512: cc * 512 + Tw], gcol[0][:, c:c + 1], scol[0][:, c:c + 1],
                            ALU.mult, ALU.add, [pk, gcol[1], scol[1]], [hk + str(c)])

    def epsap(self, val):
        if not hasattr(self, "_eps"):
            self._eps = {}
        if val not in self._eps:
            raise KeyError(val)
        return self._eps[val][0]

    def setup_regs(self):
        self.regs = {}

        def mk(name, val):
            def fn(e):
                r = e.alloc_register(name)
                self.regs[name] = r
                return e.reg_mov(r, val)
            return fn
        self.S.op("pool", mk("bslot", NSLOT - 1), (), (), sig=False)
        self.S.op("pool", mk("bw", NE * 128 - 1), (), (), sig=False)

    def setup_eps(self):
        self._eps = {}
        for val in (EPS, 1e-5):
            ap, key = self.tile([128, 1], F32, "eps")
            self.memset("pool", ap, float(val), [key])
            self._eps[val] = (ap, key)
        self.keep = self.aoff

    def mod_cols(self, l, kshift, kscale, normname):
        nm, nmk = self.tile([128, 8], F32, "nmcol")
        self.colvec(nm, nmk, self.din["%s_%d" % (normname, l)])
        res = []
        for r in range(3):
            g, gk = self.tile([128, 8], F32, "gcol")
            s, sk = self.tile([128, 8], F32, "scol")
            self.colvec(g, gk, self.modv(l, r, kscale), r=["modD"])
            self.colvec(s, sk, self.modv(l, r, kshift), r=["modD"])
            self.stt("dve", g, g, 1.0, nm, ALU.add, ALU.mult, [gk, nmk], [gk])
            res.append(((g, gk), (s, sk)))
        return res

    def xsrc(self, l, bl, tok0, n):
        if l == self.layers[0]:
            if tok0 < SEQ:
                return self.din["x_in"][bl, tok0:tok0 + n, :]
            return self.din["ctx_in"][bl, tok0 - SEQ:tok0 - SEQ + n, :]
        return self.xs[bl, tok0:tok0 + n, :]

    def ttiles(self):
        return [(i * 512, 512) for i in range(8)] + [(SEQ, 256)]

    def rope(self, psA, P, Tw, tok0, asb, perm, cosd, sind, tabs, out, okey, rkeys):
        (a_sb, ak), (t1, t1k), (t2, t2k) = asb
        (ct, ctk), (stt_, stk) = tabs
        self.cp("act", a_sb[0:P, 0:Tw], psA[0:P, 0:Tw], rkeys, [ak])
        psB, pbk = self.bank()
        self.mm(psB[0:P, 0:Tw], perm[0][0:P, 0:P], a_sb[0:P, 0:Tw], True, True, [ak, perm[1]], [pbk])
        self.tt("dve", t1[0:P, 0:Tw], psA[0:P, 0:Tw], ct[0:P, 0:Tw], ALU.mult, rkeys + [ctk], [t1k])
        self.tt("dve", t2[0:P, 0:Tw], psB[0:P, 0:Tw], stt_[0:P, 0:Tw], ALU.mult, [pbk, stk], [t2k])
        self.tt("pool", out, t1[0:P, 0:Tw], t2[0:P, 0:Tw], ALU.add, [t1k, t2k], [okey])

    def p1_even(self, l, bl):
        self.reset(self.keep)
        S = self.S
        C = self.C
        cols = self.mod_cols(l, 0, 1, "norm_mix")
        stg = self.tile([128, 4096], F32, "stg")
        win, wk = self.tile([128, 8, 1184], BF16, "win")
        wsrc = self.din["ab_w_in_%d" % l]
        for c in range(8):
            self.load_bf16(win[:, c, :], wk, wsrc[c * 128:(c + 1) * 128, :], stg)
        wuq, wuqk = self.tile([128, 2, 768], BF16, "wuq")
        for c in range(2):
            self.load_bf16(wuq[:, c, :], wuqk, self.din["mla_w_uq_%d" % l][c * 128:(c + 1) * 128, :], stg)
        wkk, wkkk = self.tile([128, 512], BF16, "wukvk")
        self.load_bf16(wkk, wkkk, self.din["wukv_k_%d" % l], stg)
        wkv, wkvk = self.tile([128, 512], BF16, "wukvv")
        self.load_bf16(wkv, wkvk, self.din["wukv_v_%d" % l], stg)
        qn, qnk = self.tile([128, 2], F32, "qn")
        self.dma(qn, self.din["mla_q_norm_%d" % l].rearrange("(c p) -> p c", p=128), (), [qnk], slow=True)
        kvn, kvnk = self.tile([128, 1], F32, "kvn")
        self.dma(kvn, self.din["mla_kv_norm_%d" % l].rearrange("(c p) -> p c", p=128), (), [kvnk], slow=True)
        ntiles = (self.tile([128, 4, D], F32, "xt"), self.tile([128, D], BF16, "junk"), self.tile([128, 4], F32, "ssq"),
                  self.tile([128, 4], F32, "rstd"), self.tile([128, 4, D], BF16, "xn"))
        hT = self.tile([128, 8, 512], BF16, "hT")
        asb = (self.tile([128, 512], BF16, "asb"), self.tile([128, 512], F32, "t1"), self.tile([128, 512], F32, "t2"))
        c64 = self.tile([128, 512], F32, "c64")
        s64 = self.tile([128, 512], F32, "s64")
        cM = self.tile([96, 512], F32, "cM")
        sM = self.tile([96, 512], F32, "sM")
        cR = self.tile([32, 512], F32, "cR")
        sR = self.tile([32, 512], F32, "sR")
        qa_o = self.tile([128, 4, 512], BF16, "qa_o")
        ka_o = self.tile([128, 512], BF16, "ka_o")
        va_o = self.tile([128, 4, 128], BF16, "va_o")
        qm_o = self.tile([96, 8, 512], BF16, "qm_o")
        kn_o = self.tile([128, 4, 512], BF16, "kn_o")
        vm_o = self.tile([128, 4, 512], BF16, "vm_o")
        kr_o = self.tile([32, 512], BF16, "kr_o")
        sq = self.tile([128, 512], BF16, "sq")
        rs = self.tile([128, 512], F32, "rs")
        cqn = self.tile([128, 2, 512], BF16, "cqn")
        ckvn = self.tile([128, 512], BF16, "ckvn")
        ones, onk = C["onesb"]
        hTa, hk = hT
        hkeys = [hk + str(c) for c in range(8)]
        import os
        cut = int(os.environ.get("P1CUT", "99"))
        for (tok0, Tw) in (self.ttiles() if cut == 99 else self.ttiles()[:1]):
            r = bl if tok0 < SEQ else 2
            ns = Tw // 128
            if cut < 1:
                break
            self.norm_hT(self.xsrc(l, bl, tok0, Tw), Tw, cols[r][0], cols[r][1], ntiles, hT)
            if cut < 2:
                break
            for (tab, nm_) in ((c64, "cos64"), (s64, "sin64"), (cM, "cosM"), (sM, "sinM"), (cR, "cosR"), (sR, "sinR")):
                P = self.din[nm_].shape[0]
                self.dma(tab[0][0:P, 0:Tw], self.din[nm_][:, tok0:tok0 + Tw], (), [tab[1]])

            def proj(col0, M, ps, pk, rhs_ap=None, rkeys=None, lw=None):
                for c in range(8):
                    self.mm(ps[0:M, 0:Tw], win[:, c, col0:col0 + M], hTa[:, c, 0:Tw], c == 0, c == 7, [wk, hk + str(c)], [pk])
            for m in range(4):
                ps, pk = self.bank()
                proj(m * 128, 128, ps, pk)
                self.rope(ps, 128, Tw, tok0, asb, C["P64"], None, None, (c64, s64), qa_o[0][:, m, 0:Tw], qa_o[1], [pk])
            self.dma(self.QA[bl, :, tok0:tok0 + Tw].rearrange("(m p) t -> p m t", p=128), qa_o[0][:, :, 0:Tw], [qa_o[1]], ["QA"])
            if cut < 3:
                break
            ps, pk = self.bank()
            proj(512, 128, ps, pk)
            self.rope(ps, 128, Tw, tok0, asb, C["P64"], None, None, (c64, s64), ka_o[0][:, 0:Tw], ka_o[1], [pk])
            self.dma(self.KA[bl, :, tok0:tok0 + Tw], ka_o[0][:, 0:Tw], [ka_o[1]], ["KA"])
            ps, pk = self.bank()
            for s in range(ns):
                for c in range(8):
                    self.mm(ps[:, s * 128:(s + 1) * 128], hTa[:, c, s * 128:(s + 1) * 128], win[:, c, 640:768], c == 0, c == 7,
                            [wk, hk + str(c)], [pk])
            self.cp("act", va_o[0][:, 0:ns, :], ps[:, 0:ns * 128].rearrange("p (s n) -> p s n", s=ns), [pk], [va_o[1]])
            self.dma(self.VA[bl, tok0:tok0 + Tw, :].rearrange("(s p) n -> p s n", p=128), va_o[0][:, 0:ns, :], [va_o[1]], ["VA"])
            if cut < 4:
                break
            pcq = []
            psN, pnk = self.bank()
            for j in range(2):
                ps, pk = self.bank()
                proj(768 + j * 128, 128, ps, pk)
                pcq.append((ps, pk))
                self.act(sq[0][:, 0:Tw], ps[:, 0:Tw], AF.Square, [pk], [sq[1]])
                self.mm(psN[:, 0:Tw], ones, sq[0][:, 0:Tw], j == 0, j == 1, [onk, sq[1]], [pnk])
            self.act(rs[0][:, 0:Tw], psN[:, 0:Tw], AF.Sqrt, [pnk], [rs[1]], scale=1.0 / 256, bias=self.epsap(EPS))
            self.recip(rs[0][:, 0:Tw], rs[0][:, 0:Tw], [rs[1]], [rs[1]])
            for j in range(2):
                self.stt("dve", cqn[0][:, j, 0:Tw], pcq[j][0][:, 0:Tw], qn[:, j:j + 1], rs[0][:, 0:Tw], ALU.mult, ALU.mult,
                         [pcq[j][1], qnk, rs[1]], [cqn[1]])
            for h in range(8):
                ps, pk = self.bank()
                for j in range(2):
                    self.mm(ps[0:96, 0:Tw], wuq[:, j, h * 96:(h + 1) * 96], cqn[0][:, j, 0:Tw], j == 0, j == 1, [wuqk, cqn[1]], [pk])
                self.rope(ps, 96, Tw, tok0, asb, C["PM"], None, None, (cM, sM), qm_o[0][:, h, 0:Tw], qm_o[1], [pk])
            self.dma(self.QM[bl, :, :, tok0:tok0 + Tw].rearrange("h p t -> p h t"), qm_o[0][:, :, 0:Tw], [qm_o[1]], ["QM"])
            if cut < 5:
                break
            ps, pk = self.bank()
            proj(1024, 128, ps, pk)
            self.act(sq[0][:, 0:Tw], ps[:, 0:Tw], AF.Square, [pk], [sq[1]])
            psN, pnk = self.bank()
            self.mm(psN[:, 0:Tw], ones, sq[0][:, 0:Tw], True, True, [onk, sq[1]], [pnk])
            self.act(rs[0][:, 0:Tw], psN[:, 0:Tw], AF.Sqrt, [pnk], [rs[1]], scale=1.0 / 128, bias=self.epsap(EPS))
            self.recip(rs[0][:, 0:Tw], rs[0][:, 0:Tw], [rs[1]], [rs[1]])
            self.stt("dve", ckvn[0][:, 0:Tw], ps[:, 0:Tw], kvn[:, 0:1], rs[0][:, 0:Tw], ALU.mult, ALU.mult, [pk, kvnk, rs[1]], [ckvn[1]])
            for m in range(4):
                ps, pk = self.bank()
                self.mm(ps[:, 0:Tw], wkk[:, m * 128:(m + 1) * 128], ckvn[0][:, 0:Tw], True, True, [wkkk, ckvn[1]], [pk])
                self.cp("act" if m % 2 == 0 else "dve", kn_o[0][:, m, 0:Tw], ps[:, 0:Tw], [pk], [kn_o[1]])
            self.dma(self.KN[bl, :, tok0:tok0 + Tw].rearrange("(m p) t -> p m t", p=128), kn_o[0][:, :, 0:Tw], [kn_o[1]], ["KN"])
            for s in range(ns):
                ps, pk = self.bank()
                self.mm(ps[:, :], ckvn[0][:, s * 128:(s + 1) * 128], wkv[:, :], True, True, [wkvk, ckvn[1]], [pk])
                self.cp("act" if s % 2 == 0 else "dve", vm_o[0][:, s, :], ps[:, :], [pk], [vm_o[1]])
            self.dma(self.VM[bl, tok0:tok0 + Tw, :].rearrange("(s p) n -> p s n", p=128), vm_o[0][:, 0:ns, :], [vm_o[1]], ["VM"])
            ps, pk = self.bank()
            proj(1152, 32, ps, pk)
            self.rope(ps, 32, Tw, tok0, asb, C["PR"], None, None, (cR, sR), kr_o[0][:, 0:Tw], kr_o[1], [pk])
            self.dma(self.KR[bl, :, tok0:tok0 + Tw], kr_o[0][:, 0:Tw], [kr_o[1]], ["KR"])
        S.barrier()

    def attn_block(self, Kt, Kkey, Krows, qrhs, Qkey, N, chunks, Vt, Vkey, scale, ptiles, acc, acck):
        n = len(chunks)
        for i, (kc, mask) in enumerate(chunks):
            ps, pk = self.bank_s()
            self.mm(ps[:, 0:N], Kt[0:Krows, kc * 128:(kc + 1) * 128], qrhs, True, True, [Kkey, Qkey], [pk])
            pt, ptk = ptiles[self.pti % len(ptiles)]
            self.pti += 1
            self.act(pt[:, 0:N], ps[:, 0:N], AF.Exp, [pk], [ptk], scale=scale)
            if mask is not None:
                self.tt("pool", pt[:, 0:N], pt[:, 0:N], mask[0][:, 0:N], ALU.mult, [ptk, mask[1]], [ptk])
            self.mm(acc[:, 0:N], Vt[:, kc, :], pt[:, 0:N], i == 0, i == n - 1, [Vkey, ptk], [acck])

    def bank_s(self):
        i = self.sbanks[self.sbi % len(self.sbanks)]
        self.sbi += 1
        return self.pb[i], "pb%d" % i

    def finish_div(self, acc, acck, N, ep, out, okey, sinkadd=None):
        (accs, ak), (rd0, rk) = ep
        if sinkadd is not None:
            self.tt("dve", accs[:, 0:N], acc[:, 0:N], sinkadd[0][:, 0:N], ALU.add, [acck, sinkadd[1]], [ak])
        else:
            self.cp("dve", accs[:, 0:N], acc[:, 0:N], [acck], [ak])
        self.recip(accs[64:128, 0:N], accs[64:128, 0:N], [ak], [ak])
        self.cp("pool", rd0[0:64, 0:N], accs[64:128, 0:N], [ak], [rk])
        self.tt("dve", out, accs[0:64, 0:N], rd0[0:64, 0:N], ALU.mult, [ak, rk], [okey])

    def p2_even(self, l, bl, ctx_out):
        self.reset(self.keep)
        S = self.S
        C = self.C
        self.sbanks = [0, 1, 2, 3, 4]
        self.sbi = 0
        self.pti = 0
        accb = [(self.pb[5], "pb5"), (self.pb[6], "pb6"), (self.pb[7], "pb7")]
        acci = 0
        ptiles = [self.tile([128, 512], BF16, "pt") for _ in range(4)]
        ep = (self.tile([128, 512], F32, "accs"), self.tile([64, 512], F32, "rd0"))
        otl = [self.tile([64, 512], BF16, "otile") for _ in range(2)]
        oti = 0
        snk, snkk = self.tile([128, 8], F32, "snk")
        self.bcast(snk, snkk, self.din["swa_sink_%d" % l])
        self.act(snk, snk, AF.Exp, [snkk], [snkk])
        sinkadd = []
        for g in range(2):
            sa, sak = self.tile([128, 512], F32, "sinkadd")
            self.memset("pool", sa, 0.0, [sak])
            for j in range(4):
                self.ts("dve", sa[64:128, j * 128:(j + 1) * 128], sa[64:128, j * 128:(j + 1) * 128], snk[64:128, g * 4 + j:g * 4 + j + 1], None,
                        ALU.add, None, [sak, snkk], [sak])
            sinkadd.append((sa, sak))
        Qg = [self.tile([64, 4, T], BF16, "Qg") for _ in range(1)]
        Kg = [self.tile([64, T], BF16, "Kg") for _ in range(2)]
        Vg = [self.tile([128, NCH, 128], BF16, "Vg") for _ in range(2)]
        for v, vk in Vg:
            self.memset("pool", v[:, :, 64:128], 1.0, [vk + "o"])
        mprev, mnext = C["mprev"], C["mnext"]
        for g in range(2):
            q, qk = Qg[0]
            k, kk = Kg[g % 2]
            v, vk = Vg[g % 2]
            self.dma(q, self.QA[bl, g * 256:(g + 1) * 256, :].rearrange("(j p) t -> p j t", p=64), ["QA"], [qk])
            self.dma(k, self.KA[bl, g * 64:(g + 1) * 64, :], ["KA"], [kk])
            self.dma(v[:, :, 0:64], self.VA[bl, :, g * 64:(g + 1) * 64].rearrange("(c p) n -> p c n", p=128), ["VA"], [vk])
            nblk = 34 if ctx_out else 32
            for i in range(nblk):
                if i < 32:
                    chunks = []
                    if i > 0:
                        chunks.append((i - 1, mprev))
                    chunks.append((i, None))
                    if i < 31:
                        chunks.append((i + 1, mnext))
                    chunks += [(32, None), (33, None)]
                else:
                    chunks = [(32, None), (33, None)]
                acc, acck = accb[acci % 3]
                acci += 1
                self.attn_block2(k, [kk], 64, q[:, :, i * 128:(i + 1) * 128], qk, 512, chunks, v, [vk, vk + "o"], 0.125, ptiles, acc, acck)
                ot, otk = otl[oti % 2]
                oti += 1
                self.finish_div(acc, acck, 512, ep, ot[:, :], otk, sinkadd=sinkadd[g])
                self.dma(self.OT[bl, g * 256:(g + 1) * 256, i * 128:(i + 1) * 128].rearrange("(j p) t -> p j t", p=64),
                         ot.rearrange("p (j t) -> p j t", j=4), [otk], ["OT"])
        Qh = [self.tile([96, T], BF16, "Qh") for _ in range(2)]
        Kh = [self.tile([96, T], BF16, "Kh") for _ in range(2)]
        Vh = Vg
        mscale = 96.0 ** -0.5
        for h in range(8):
            q, qk = Qh[h % 2]
            k, kk = Kh[h % 2]
            v, vk = Vh[h % 2]
            self.dma(q, self.QM[bl, h, :, :], ["QM"], [qk])
            self.dma(k[0:64, :], self.KN[bl, h * 64:(h + 1) * 64, :], ["KN"], [kk])
            self.dma(k[64:96, :], self.KR[bl, :, :], ["KR"], [kk + "r"])
            self.dma(v[:, :, 0:64], self.VM[bl, :, h * 64:(h + 1) * 64].rearrange("(c p) n -> p c n", p=128), ["VM"], [vk])
            qts = [(i * 512, 512, [(c, None) for c in range(NCH)]) for i in range(8)]
            if ctx_out:
                qts.append((SEQ, 256, [(32, None), (33, None)]))
            for (q0, N, chunks) in qts:
                acc, acck = accb[acci % 3]
                acci += 1
                self.attn_block2(k, [kk, kk + "r"], 96, q[:, q0:q0 + N], qk, N, chunks, v, [vk, vk + "o"], mscale, ptiles, acc, acck)
                ot, otk = otl[oti % 2]
                oti += 1
                self.finish_div(acc, acck, N, ep, ot[:, 0:N], otk)
                self.dma(self.OT[bl, 512 + h * 64: 512 + (h + 1) * 64, q0:q0 + N], ot[:, 0:N], [otk], ["OT"])
        S.barrier()

    def attn_block2(self, Kt, Kkeys, Krows, qrhs, Qkey, N, chunks, Vt, Vkeys, scale, ptiles, acc, acck):
        n = len(chunks)
        for i, (kc, mask) in enumerate(chunks):
            ps, pk = self.bank_s()
            self.mm(ps[:, 0:N], Kt[0:Krows, kc * 128:(kc + 1) * 128], qrhs, True, True, list(Kkeys) + [Qkey], [pk])
            pt, ptk = ptiles[self.pti % len(ptiles)]
            self.pti += 1
            self.act(pt[:, 0:N], ps[:, 0:N], AF.Exp, [pk], [ptk], scale=scale)
            if mask is not None:
                self.tt("pool", pt[:, 0:N], pt[:, 0:N], mask[0][:, 0:N], ALU.mult, [ptk, mask[1]], [ptk])
            self.mm(acc[:, 0:N], Vt[:, kc, :], pt[:, 0:N], i == 0, i == n - 1, list(Vkeys) + [ptk], [acck])

    def p1_diff(self, l, bl):
        self.reset(self.keep)
        S = self.S
        C = self.C
        cols = self.mod_cols(l, 0, 1, "norm_mix")
        stg = self.tile([128, 4096], F32, "stg")
        win, wk = self.tile([128, 8, 3072], BF16, "win")
        wsrc = self.din["diff_w_in_%d" % l]
        for c in range(8):
            self.load_bf16(win[:, c, :], wk, wsrc[c * 128:(c + 1) * 128, :], stg, eng="pool" if c % 2 == 0 else "dve")
        ntiles = (self.tile([128, 4, D], F32, "xt"), self.tile([128, D], BF16, "junk"), self.tile([128, 4], F32, "ssq"),
                  self.tile([128, 4], F32, "rstd"), self.tile([128, 4, D], BF16, "xn"))
        hT = self.tile([128, 8, 512], BF16, "hT")
        asb = (self.tile([128, 512], BF16, "asb"), self.tile([128, 512], F32, "t1"), self.tile([128, 512], F32, "t2"))
        c64 = self.tile([128, 512], F32, "c64")
        s64 = self.tile([128, 512], F32, "s64")
        q_o = self.tile([128, 8, 512], BF16, "q_o")
        k_o = self.tile([128, 8, 512], BF16, "k_o")
        v_o = self.tile([128, 4, D], BF16, "v_o")
        hTa, hk = hT
        for (tok0, Tw) in self.ttiles():
            r = bl if tok0 < SEQ else 2
            ns = Tw // 128
            self.norm_hT(self.xsrc(l, bl, tok0, Tw), Tw, cols[r][0], cols[r][1], ntiles, hT)
            for (tab, nm_) in ((c64, "cos64"), (s64, "sin64")):
                self.dma(tab[0][:, 0:Tw], self.din[nm_][:, tok0:tok0 + Tw], (), [tab[1]])
            for typ, (o_t, dst) in enumerate(((q_o, self.QD), (k_o, self.KD))):
                for m in range(8):
                    ps, pk = self.bank()
                    col0 = typ * 1024 + m * 128
                    for c in range(8):
                        self.mm(ps[:, 0:Tw], win[:, c, col0:col0 + 128], hTa[:, c, 0:Tw], c == 0, c == 7, [wk, hk + str(c)], [pk])
                    self.rope(ps, 128, Tw, tok0, asb, C["P64"], None, None, (c64, s64), o_t[0][:, m, 0:Tw], o_t[1], [pk])
                self.dma(dst[bl, :, :, tok0:tok0 + Tw].rearrange("h p t -> p h t"), o_t[0][:, :, 0:Tw], [o_t[1]], ["QKD%d" % typ])
            for s in range(ns):
                for half in range(2):
                    ps, pk = self.bank()
                    for c in range(8):
                        self.mm(ps[:, :], hTa[:, c, s * 128:(s + 1) * 128], win[:, c, 2048 + half * 512: 2048 + (half + 1) * 512],
                                c == 0, c == 7, [wk, hk + str(c)], [pk])
                    self.cp("act" if half == 0 else "dve", v_o[0][:, s, half * 512:(half + 1) * 512], ps[:, :], [pk], [v_o[1]])
            self.dma(self.VD[bl, tok0:tok0 + Tw, :].rearrange("(s p) n -> p s n", p=128), v_o[0][:, 0:ns, :], [v_o[1]], ["VD"])
        S.barrier()

    def p2_diff(self, l, bl, ctx_out):
        self.reset(self.keep)
        S = self.S
        C = self.C
        lam_init = 0.8 - 0.6 * math.exp(-0.3 * l)
        ones, onk = C["onesb"]
        lt = [self.tile([128, 64], F32, "lam") for _ in range(4)]
        for (t_, nm_) in zip(lt, ("lq1", "lk1", "lq2", "lk2")):
            self.bcast(t_[0], t_[1], self.din["%s_%d" % (nm_, l)])
        ls = self.tile([128, 2], F32, "ls")
        self.memset("pool", ls[0], 0.0, [ls[1]])
        lj = self.tile([128, 64], F32, "lj")
        for i in range(2):
            self.tt("dve", lj[0], lt[2 * i][0], lt[2 * i + 1][0], ALU.mult, [lt[2 * i][1], lt[2 * i + 1][1]], [lj[1]])
            self.S.op("dve", (lambda a, b: (lambda e: e.reduce_sum(out=a, in_=b, axis=AX.X)))(ls[0][:, i:i + 1], lj[0]), [lj[1]], [ls[1]])
        self.act(ls[0], ls[0], AF.Exp, [ls[1]], [ls[1]])
        nlam = self.tile([128, 1], F32, "nlam")
        self.tt("dve", nlam[0], ls[0][:, 1:2], ls[0][:, 0:1], ALU.subtract, [ls[1]], [nlam[1]])
        self.ts("dve", nlam[0], nlam[0], -lam_init, None, ALU.add, None, [nlam[1]], [nlam[1]])
        sub = self.tile([128, 1], F32, "sub")
        self.dma(sub[0], self.din["subln_%d" % l].rearrange("(c p) -> p c", p=128), (), [sub[1]], slow=True)
        self.ts("dve", sub[0], sub[0], 1.0 - lam_init, None, ALU.mult, None, [sub[1]], [sub[1]])

        self.sbanks = [0, 1, 2, 3]
        self.sbi = 0
        self.pti = 0
        ptiles = [self.tile([128, 512], BF16, "pt") for _ in range(6)]
        Qh = [self.tile([128, T], BF16, "Qh") for _ in range(2)]
        Kh = [self.tile([128, T], BF16, "Kh") for _ in range(2)]
        Vh = [self.tile([128, NCH, 128], BF16, "Vh") for _ in range(2)]
        r1 = self.tile([128, 512], F32, "r1")
        r2 = self.tile([128, 512], F32, "r2")
        o1 = self.tile([128, 512], F32, "o1")
        o2 = self.tile([128, 512], F32, "o2")
        sq = self.tile([128, 512], BF16, "sq")
        otl = [self.tile([128, 512], BF16, "otile") for _ in range(2)]
        oti = 0
        accs = [(self.pb[4], "pb4"), (self.pb[5], "pb5"), (self.pb[6], "pb6"), (self.pb[7], "pb7")]
        for h in range(8):
            q, qk = Qh[h % 2]
            k, kk = Kh[h % 2]
            v, vk = Vh[h % 2]
            self.dma(q, self.QD[bl, h, :, :], ["QKD0"], [qk])
            self.dma(k, self.KD[bl, h, :, :], ["QKD1"], [kk])
            self.dma(v, self.VD[bl, :, h * 128:(h + 1) * 128].rearrange("(c p) n -> p c n", p=128), ["VD"], [vk])
            qts = [(i * 512, 512, list(range(NCH))) for i in range(8)]
            if ctx_out:
                qts.append((SEQ, 256, [32, 33]))
            for (q0, N, chunks) in qts:
                n = len(chunks)
                for i, kc in enumerate(chunks):
                    pts = []
                    for mp in range(2):
                        ps, pk = self.bank_s()
                        self.mm(ps[:, 0:N], k[mp * 64:(mp + 1) * 64, kc * 128:(kc + 1) * 128], q[mp * 64:(mp + 1) * 64, q0:q0 + N], True, True,
                                [kk, qk], [pk])
                        pt, ptk = ptiles[self.pti % len(ptiles)]
                        self.pti += 1
                        self.act(pt[:, 0:N], ps[:, 0:N], AF.Exp, [pk], [ptk], scale=0.125)
                        pts.append((pt, ptk))
                    for mp in range(2):
                        pt, ptk = pts[mp]
                        self.mm(accs[2 * mp][0][:, 0:N], v[:, kc, :], pt[:, 0:N], i == 0, i == n - 1, [vk, ptk], [accs[2 * mp][1]])
                        self.mm(accs[2 * mp + 1][0][:, 0:N], ones, pt[:, 0:N], i == 0, i == n - 1, [onk, ptk], [accs[2 * mp + 1][1]])
                self.recip(r1[0][:, 0:N], accs[1][0][:, 0:N], [accs[1][1]], [r1[1]])
                self.recip(r2[0][:, 0:N], accs[3][0][:, 0:N], [accs[3][1]], [r2[1]])
                self.tt("dve", o1[0][:, 0:N], accs[0][0][:, 0:N], r1[0][:, 0:N], ALU.mult, [accs[0][1], r1[1]], [o1[1]])
                self.tt("dve", o2[0][:, 0:N], accs[2][0][:, 0:N], r2[0][:, 0:N], ALU.mult, [accs[2][1], r2[1]], [o2[1]])
                self.stt("dve", o1[0][:, 0:N], o2[0][:, 0:N], nlam[0][:, 0:1], o1[0][:, 0:N], ALU.mult, ALU.add, [o2[1], nlam[1], o1[1]], [o1[1]])
                self.act(sq[0][:, 0:N], o1[0][:, 0:N], AF.Square, [o1[1]], [sq[1]])
                psN, pnk = self.bank_s()
                self.mm(psN[:, 0:N], ones, sq[0][:, 0:N], True, True, [onk, sq[1]], [pnk])
                self.act(r1[0][:, 0:N], psN[:, 0:N], AF.Sqrt, [pnk], [r1[1]], scale=1.0 / 128, bias=self.epsap(1e-5))
                self.recip(r1[0][:, 0:N], r1[0][:, 0:N], [r1[1]], [r1[1]])
                ot, otk = otl[oti % 2]
                oti += 1
                self.stt("dve", ot[:, 0:N], o1[0][:, 0:N], sub[0][:, 0:1], r1[0][:, 0:N], ALU.mult, ALU.mult, [o1[1], sub[1], r1[1]], [otk])
                self.dma(self.OT[bl, h * 128:(h + 1) * 128, q0:q0 + N], ot[:, 0:N], [otk], ["OT"])
        S.barrier()

    def p3(self, l, bl, last):
        self.reset(self.keep)
        S = self.S
        C = self.C
        stg = self.tile([128, 4096], F32, "stg")
        nf, nfk = self.tile([128, D], F32, "nf")
        self.bcast(nf, nfk, self.din["norm_ffn_%d" % l])
        wo = {}
        g2 = {}
        s2 = {}
        wsrc = self.din["w_out_%d" % l]
        for r in ((bl,) if last else (bl, 2)):
            gt, gk = self.tile([128, D], F32, "gate")
            self.bcast(gt, gk, self.modv(l, r, 2))
            w_, wk_ = self.tile([128, 8, D], BF16, "wo")
            for c in range(8):
                sap, skey = stg
                self.dma(sap[:, 0:D], wsrc[c * 128:(c + 1) * 128, :], (), [skey])
                self.tt("dve" if c % 2 == 0 else "pool", w_[:, c, :], sap[:, 0:D], gt, ALU.mult, [skey, gk], [wk_])
            wo[r] = (w_, wk_)
            a, ak = self.tile([128, D], F32, "gs2")
            self.bcast(a, ak, self.modv(l, r, 4))
            self.stt("dve", a, a, 1.0, nf, ALU.add, ALU.mult, [ak, nfk], [ak])
            g2[r] = (a, ak)
            b, bk = self.tile([128, D], F32, "sh2")
            self.bcast(b, bk, self.modv(l, r, 3))
            s2[r] = (b, bk)
        wr, wrk = self.tile([128, 8, 36], F32, "wr")
        self.dma(wr, self.din["router_w_%d" % l].rearrange("(c p) n -> p c n", p=128), (), [wrk])
        rb, rbk = self.tile([128, 36], F32, "rb")
        self.bcast(rb, rbk, self.din["router_b_%d" % l])
        base, bsk = C["base"]
        if bl == 0:
            self.memset("pool", base, 0.0, [bsk])
        ots = [self.tile([128, 8, 128], BF16, "ot") for _ in range(2)]
        xts = [self.tile([128, D], F32, "xt") for _ in range(2)]
        junk = self.tile([128, D], BF16, "junk")
        h2 = self.tile([128, D], F32, "h2")
        h2b = [self.tile([128, D], BF16, "h2b") for _ in range(2)]
        h2T = self.tile([128, 8, 128], F32, "h2T")
        sm = {nm_: self.tile([128, w_], F32, nm_) for nm_, w_ in (("ssq", 1), ("rstd", 1), ("lg", 36), ("gmax", 1), ("ngmax", 1), ("ohg", 4),
                                                                   ("gsum", 1), ("pgrp", 1), ("negm", 32), ("elm", 32), ("top8", 8), ("oh1", 32),
                                                                   ("oh2", 32), ("ex", 1), ("den", 1), ("A", 32), ("posv", 32), ("ovf", 32),
                                                                   ("slotf", 2), ("gslotf", 2), ("junk32", 32), ("nm1", 1))}
        Ab = self.tile([128, NE], BF16, "Ab")
        identf, ifk = C["identf"]
        triU, tuk = C["triU"]
        ones, onk = C["onesb"]
        rt_slot, rsk = C["rt_slot"]
        rt_gate, rgk = C["rt_gate"]
        ntile = 32 if last else NCH
        for ti in range(ntile):
            tok0 = ti * 128
            r = bl if tok0 < SEQ else 2
            gti = bl * NCH + ti
            ot, otk = ots[ti % 2]
            xt, xk = xts[ti % 2]
            self.dma(ot, self.OT[bl, :, tok0:tok0 + 128].rearrange("(c p) t -> p c t", p=128), ["OT"], [otk])
            self.dma(xt, self.xsrc(l, bl, tok0, 128), (), [xk])
            w_, wk_ = wo[r]
            for half in range(2):
                ps, pk = self.bank()
                for c in range(8):
                    self.mm(ps[:, :], ot[:, c, :], w_[:, c, half * 512:(half + 1) * 512], c == 0, c == 7, [otk, wk_], [pk])
                self.tt("dve", xt[:, half * 512:(half + 1) * 512], xt[:, half * 512:(half + 1) * 512], ps[:, :], ALU.add, [xk, pk], [xk])
            self.dma(self.xs[bl, tok0:tok0 + 128, :], xt, [xk], ["xs"])
            t = lambda n_: sm[n_][0]
            k_ = lambda n_: sm[n_][1]
            self.memset("pool", t("ssq"), 0.0, [k_("ssq")])
            self.act(junk[0], xt, AF.Square, [xk], [junk[1], k_("ssq")], accum_out=t("ssq"))
            self.act(t("rstd"), t("ssq"), AF.Sqrt, [k_("ssq")], [k_("rstd")], scale=1.0 / D, bias=self.epsap(EPS))
            self.recip(t("rstd"), t("rstd"), [k_("rstd")], [k_("rstd")])
            self.stt("dve", h2[0], xt, t("rstd")[:, 0:1], g2[r][0], ALU.mult, ALU.mult, [xk, k_("rstd"), g2[r][1]], [h2[1]])
            self.tt("pool", h2[0], h2[0], s2[r][0], ALU.add, [h2[1], s2[r][1]], [h2[1]])
            hb, hbk = h2b[ti % 2]
            self.cp("act", hb, h2[0], [h2[1]], [hbk])
            for half in range(2):
                ps, pk = self.bank()
                for cc in range(4):
                    c = half * 4 + cc
                    self.tr(ps[:, cc * 128:(cc + 1) * 128], h2[0][:, c * 128:(c + 1) * 128], identf, [h2[1], ifk], [pk], sig=(cc == 3))
                self.cp("act" if half == 0 else "dve", h2T[0][:, half * 4:(half + 1) * 4, :], ps[:, :].rearrange("p (c t) -> p c t", c=4), [pk], [h2T[1]])
            ps, pk = self.bank()
            for c in range(8):
                self.mm(ps[:, 0:36], h2T[0][:, c, :], wr[:, c, :], c == 0, c == 7, [h2T[1], wrk], [pk])
            self.tt("dve", t("lg"), ps[:, 0:36], rb, ALU.add, [pk, rbk], [k_("lg")])
            self.S.op("dve", (lambda a, b: (lambda e: e.reduce_max(out=a, in_=b, axis=AX.X)))(t("gmax"), t("lg")[:, 0:4]), [k_("lg")], [k_("gmax")])
            self.ts("dve", t("ohg"), t("lg")[:, 0:4], t("gmax")[:, 0:1], None, ALU.is_equal, None, [k_("lg"), k_("gmax")], [k_("ohg")])
            self.ts("dve", t("ngmax"), t("gmax"), -1.0, None, ALU.mult, None, [k_("gmax")], [k_("ngmax")])
            self.memset("pool", t("gsum"), 0.0, [k_("gsum")])
            self.act(t("junk32")[:, 0:4], t("lg")[:, 0:4], AF.Exp, [k_("lg"), k_("ngmax")], [k_("junk32"), k_("gsum")], bias=t("ngmax")[:, 0:1],
                     accum_out=t("gsum"))
            self.recip(t("pgrp"), t("gsum"), [k_("gsum")], [k_("pgrp")])
            self.ts("dve", t("negm").rearrange("p (g e) -> p g e", g=4), t("ohg").unsqueeze(2).to_broadcast([128, 4, 8]), -1.0, 1e30,
                    ALU.add, ALU.mult, [k_("ohg")], [k_("negm")])
            self.tt("dve", t("elm"), t("lg")[:, 4:36], t("negm"), ALU.add, [k_("lg"), k_("negm")], [k_("elm")])
            self.S.op("dve", (lambda a, b: (lambda e: e.max(out=a, in_=b)))(t("top8"), t("elm")), [k_("elm")], [k_("top8")])
            rt_oh, rohk = C["rt_oh"]
            rt_pos, rpk = C["rt_pos"]
            oh1 = rt_oh[:, gti, 0, :]
            oh2 = rt_oh[:, gti, 1, :]
            ohk = rohk + str(gti)
            self.ts("dve", oh1, t("elm"), t("top8")[:, 0:1], None, ALU.is_equal, None, [k_("elm"), k_("top8")], [ohk + "a"])
            self.ts("dve", oh2, t("elm"), t("top8")[:, 1:2], None, ALU.is_equal, None, [k_("elm"), k_("top8")], [ohk + "b"])
            self.ts("dve", t("nm1"), t("top8")[:, 0:1], -1.0, None, ALU.mult, None, [k_("top8")], [k_("nm1")])
            self.act(t("ex"), t("top8")[:, 1:2], AF.Exp, [k_("top8"), k_("nm1")], [k_("ex")], bias=t("nm1")[:, 0:1])
            self.ts("dve", t("den"), t("ex"), 1.0, None, ALU.add, None, [k_("ex")], [k_("den")])
            self.recip(t("den"), t("den"), [k_("den")], [k_("den")])
            self.tt("dve", rt_gate[:, gti, 0:1], t("pgrp"), t("den"), ALU.mult, [k_("pgrp"), k_("den")], [rgk + "a%d" % gti])
            self.tt("dve", rt_gate[:, gti, 1:2], rt_gate[:, gti, 0:1], t("ex"), ALU.mult, [rgk + "a%d" % gti, k_("ex")], [rgk + "b%d" % gti])
            self.tt("dve", t("A"), oh1, oh2, ALU.add, [ohk + "a", ohk + "b"], [k_("A")])
            self.cp("dve", Ab[0], t("A"), [k_("A")], [Ab[1]])
            psr, prk = self.bank()
            self.mm(psr[:, 0:NE], triU, Ab[0], True, True, [tuk, Ab[1]], [prk])
            self.tt("dve", t("posv"), psr[:, 0:NE], base, ALU.add, [prk, bsk], [k_("posv")])
            psc, pck = self.bank()
            self.mm(psc[:, 0:NE], ones, Ab[0], True, True, [onk, Ab[1]], [pck])
            self.tt("dve", base, base, psc[:, 0:NE], ALU.add, [bsk, pck], [bsk])
            for kk_, (ohx, okx) in enumerate(((oh1, ohk + "a"), (oh2, ohk + "b"))):
                self.tt("dve", t("junk32"), ohx, t("posv"), ALU.mult, [okx, k_("posv")], [k_("junk32")])
                self.S.op("dve", (lambda a, b: (lambda e: e.reduce_sum(out=a, in_=b, axis=AX.X)))(rt_pos[:, gti, kk_:kk_ + 1], t("junk32")),
                          [k_("junk32")], [rpk + "%d_%d" % (gti, kk_)])
            self.dma(self.H2[bl, tok0:tok0 + 128, :], hb, [hbk], ["H2"])
        S.barrier()

    def p3b(self, l, last):
        self.reset(self.keep)
        S = self.S
        C = self.C
        base, bsk = C["base"]
        rt_oh, rohk = C["rt_oh"]
        rt_pos, rpk = C["rt_pos"]
        rt_slot, rsk = C["rt_slot"]
        widx, wik = C["widx"]
        pidx, pik = C["pidx"]
        padded = self.tile([128, NE], F32, "padded")
        tmp = self.tile([128, NE], F32, "tmp")
        pstart = self.tile([128, NE], F32, "pstart")
        pend = self.tile([128, NE], F32, "pend")
        sbs = self.tile([128, NSB, NE], F32, "sbs")
        self.dma(sbs[0], self.din["sbstart"], (), [sbs[1]])
        cmp_ = self.tile([128, NSB, NE], F32, "cmp")
        self.tt("dve", cmp_[0], base.unsqueeze(1).to_broadcast([128, NSB, NE]), sbs[0], ALU.is_gt, [bsk, sbs[1]], [cmp_[1]])
        self.S.op("dve", (lambda a, b: (lambda e: e.reduce_sum(out=a, in_=b, axis=AX.X)))(tmp[0], cmp_[0].rearrange("p s e -> p e s")),
                  [cmp_[1]], [tmp[1]])
        self.ts("dve", padded[0], tmp[0], float(SBW), None, ALU.mult, None, [tmp[1]], [padded[1]])
        self.memset("pool", pstart[0], 0.0, [pstart[1]])
        for e in range(1, NE):
            self.tt("dve", pstart[0][:, e:e + 1], pstart[0][:, e - 1:e], padded[0][:, e - 1:e], ALU.add, [pstart[1], padded[1]], [pstart[1]])
        self.tt("dve", pend[0], pstart[0], padded[0], ALU.add, [pstart[1], padded[1]], [pend[1]])
        self.tt("dve", cmp_[0], pend[0].unsqueeze(1).to_broadcast([128, NSB, NE]), sbs[0], ALU.is_le, [pend[1], sbs[1]], [cmp_[1]])
        be = self.tile([128, NSB], F32, "be")
        self.S.op("dve", (lambda a, b: (lambda e: e.reduce_sum(out=a, in_=b, axis=AX.X)))(be[0], cmp_[0]), [cmp_[1]], [be[1]])
        self.ts("dve", be[0], be[0], float(NE - 1), None, ALU.min, None, [be[1]], [be[1]])
        self.ts("dve", be[0], be[0], 128.0, None, ALU.mult, None, [be[1]], [be[1]])
        self.ts("dve", be[0], be[0], pidx[:, 0:1], None, ALU.add, None, [be[1], pik], [be[1]])
        self.cp("dve", widx, be[0], [be[1]], [wik])
        sl = self.tile([128, 2], F32, "sl")
        j32 = self.tile([128, NE], F32, "j32")
        hbs = [self.tile([128, D], BF16, "hb") for _ in range(3)]
        it = 0
        for bl in range(NB):
            for ti in range(32 if last else NCH):
                gti = bl * NCH + ti
                hb, hbk = hbs[it % 3]
                it += 1
                self.dma(hb, self.H2[bl, ti * 128:(ti + 1) * 128, :], ["H2"], [hbk])
                for kk_ in range(2):
                    self.tt("dve", j32[0], rt_oh[:, gti, kk_, :], pstart[0], ALU.mult, [pstart[1]], [j32[1]])
                    self.S.op("dve", (lambda a, b: (lambda e: e.reduce_sum(out=a, in_=b, axis=AX.X)))(sl[0][:, kk_:kk_ + 1], j32[0]),
                              [j32[1]], [sl[1]])
                self.tt("dve", sl[0], sl[0], rt_pos[:, gti, :], ALU.add, [sl[1]], [sl[1]])
                self.cp("dve", rt_slot[:, gti, :], sl[0], [sl[1]], [rsk + str(gti)])
                for kk_ in range(2):
                    self.S.dma("poolq", (lambda o_, i_, s_: (lambda e: e.indirect_dma_start(
                        out=o_, out_offset=bass.IndirectOffsetOnAxis(ap=s_, axis=0), in_=i_, in_offset=None,
                        bounds_check=self.regs["bslot"], oob_is_err=False)))(self.XB, hb, rt_slot[:, gti, kk_:kk_ + 1]),
                        [hbk, rsk + str(gti)], ["XB"])
        S.barrier()

    def p4(self, l):
        self.reset(self.keep)
        S = self.S
        C = self.C
        identb, ik = C["identb"]
        widx, wik = C["widx"]
        stgs = [None, None, None] if WCAST_DMA else [self.tile([128, 4096], F32, "stg") for _ in range(3)]
        w1b = [self.tile([128, 8, 512], BF16, "w1b") for _ in range(2)]
        w3b = [self.tile([128, 8, 512], BF16, "w3b") for _ in range(2)]
        w2b = [self.tile([128, 4, D], BF16, "w2b") for _ in range(2)]
        xbs = [self.tile([128, 4, D], BF16, "xb") for _ in range(2)]
        xbT = self.tile([128, 8, 512], BF16, "xbT")
        gT = self.tile([128, 4, 512], BF16, "gT")
        s1s = [self.tile([128, 512], F32, "s1") for _ in range(2)]
        youts = [self.tile([128, 4, D], F32, "yout") for _ in range(2)]
        W1, W3, W2 = self.din["w1_%d" % l], self.din["w3_%d" % l], self.din["w2_%d" % l]
        for sb in range(NSB):
            a1, k1 = w1b[sb % 2]
            a3, k3 = w3b[sb % 2]
            a2, k2 = w2b[sb % 2]
            for (dst, dk, src, st) in ((a1, k1, W1, stgs[0]), (a3, k3, W3, stgs[1]), (a2, k2, W2, stgs[2])):
                dflat = dst.rearrange("p a b -> p (a b)")
                if WCAST_DMA:
                    self.S.dma("poolq", (lambda o_, i_, s_: (lambda e: e.indirect_dma_start(
                        out=o_, out_offset=None, in_=i_, in_offset=bass.IndirectOffsetOnAxis(ap=s_, axis=0),
                        bounds_check=self.regs["bw"], oob_is_err=False)))(dflat, src, widx[:, sb:sb + 1]), [wik], [dk])
                else:
                    sap, skey = st
                    self.S.dma("poolq", (lambda o_, i_, s_: (lambda e: e.indirect_dma_start(
                        out=o_, out_offset=None, in_=i_, in_offset=bass.IndirectOffsetOnAxis(ap=s_, axis=0),
                        bounds_check=self.regs["bw"], oob_is_err=False)))(sap, src, widx[:, sb:sb + 1]), [wik], [skey])
                    self.cp("dve" if dst is a3 else "pool", dflat, sap, [skey], [dk])
            slot0 = sb * SBW
            xb, xbk = xbs[sb % 2]
            self.dma(xb, self.XB[slot0:slot0 + 512, :].rearrange("(s p) d -> p s d", p=128), ["XB"], [xbk])
            for cp_ in range(4):
                ps, pk = self.bank()
                psb = ps[:].bitcast(BF16)
                for cc in range(2):
                    c = cp_ * 2 + cc
                    for s_ in range(4):
                        self.tr(psb[:, cc * 512 + s_ * 128: cc * 512 + (s_ + 1) * 128], xb[:, s_, c * 128:(c + 1) * 128], identb, [xbk, ik], [pk],
                                sig=(cc == 1 and s_ == 3))
                ev = "act" if cp_ % 2 == 0 else "dve"
                self.cp(ev, xbT[0][:, cp_ * 2, :], psb[:, 0:512], [pk], [xbT[1] + str(cp_ * 2)])
                self.cp(ev, xbT[0][:, cp_ * 2 + 1, :], psb[:, 512:1024], [pk], [xbT[1] + str(cp_ * 2 + 1)])
            for f in range(4):
                ps1, pk1 = self.bank()
                for c in range(8):
                    self.mm(ps1[:, :], a1[:, c, f * 128:(f + 1) * 128], xbT[0][:, c, :], c == 0, c == 7, [k1, xbT[1] + str(c)], [pk1])
                ps3, pk3 = self.bank()
                for c in range(8):
                    self.mm(ps3[:, :], a3[:, c, f * 128:(f + 1) * 128], xbT[0][:, c, :], c == 0, c == 7, [k3, xbT[1] + str(c)], [pk3])
                s1, s1k = s1s[f % 2]
                self.act(s1, ps1[:, :], AF.Silu, [pk1], [s1k])
                self.tt("dve", gT[0][:, f, :], s1, ps3[:, :], ALU.mult, [s1k, pk3], [gT[1] + str(f)])
            yo, yok = youts[sb % 2]
            for s_ in range(4):
                for half in range(2):
                    psy, pky = self.bank()
                    for f in range(4):
                        self.mm(psy[:, :], gT[0][:, f, s_ * 128:(s_ + 1) * 128], a2[:, f, half * 512:(half + 1) * 512], f == 0, f == 3,
                                [gT[1] + str(f), k2], [pky])
                    self.cp("act" if half == 0 else "dve", yo[:, s_, half * 512:(half + 1) * 512], psy[:, :], [pky], [yok])
            self.dma(self.YB[slot0:slot0 + 512, :].rearrange("(s p) d -> p s d", p=128), yo, [yok], ["YB"])
        S.barrier()

    def p5(self, l, last, final):
        self.reset(self.keep)
        S = self.S
        C = self.C
        rt_gslot, rgsk = C["rt_slot"]
        rt_gate, rgk = C["rt_gate"]
        m5 = {}
        for r in ((0, 1) if last else (0, 1, 2)):
            a, ak = self.tile([128, D], F32, "m5")
            self.bcast(a, ak, self.modv(l, r, 5))
            m5[r] = (a, ak)
        if final:
            fn, fnk = self.tile([128, D], F32, "fn")
            self.bcast(fn, fnk, self.din["final_norm"])
        y1s = [self.tile([128, D], F32, "y1") for _ in range(2)]
        y2s = [self.tile([128, D], F32, "y2") for _ in range(2)]
        xts = [self.tile([128, D], F32, "xt") for _ in range(2)]
        junk = self.tile([128, D], BF16, "junk")
        ssq = self.tile([128, 1], F32, "ssq")
        rstd = self.tile([128, 1], F32, "rstd")
        it = 0
        for bl in range(NB):
            for ti in range(32 if last else NCH):
                tok0 = ti * 128
                r = bl if tok0 < SEQ else 2
                gti = bl * NCH + ti
                y1, y1k = y1s[it % 2]
                y2, y2k = y2s[it % 2]
                xt, xk = xts[it % 2]
                it += 1
                for (yy, yk, kk_) in ((y1, y1k, 0), (y2, y2k, 1)):
                    self.S.dma("poolq", (lambda o_, i_, s_: (lambda e: e.indirect_dma_start(
                        out=o_, out_offset=None, in_=i_, in_offset=bass.IndirectOffsetOnAxis(ap=s_, axis=0),
                        bounds_check=self.regs["bslot"], oob_is_err=False)))(yy, self.YB, rt_gslot[:, gti, kk_:kk_ + 1]),
                        ["YB"], [yk])
                self.dma(xt, self.xs[bl, tok0:tok0 + 128, :], ["xs"], [xk])
                self.ts("dve", y1, y1, rt_gate[:, gti, 0:1], None, ALU.mult, None, [y1k], [y1k])
                self.stt("dve", y1, y2, rt_gate[:, gti, 1:2], y1, ALU.mult, ALU.add, [y2k, y1k], [y1k])
                self.tt("pool", y1, y1, m5[r][0], ALU.mult, [y1k, m5[r][1]], [y1k])
                self.tt("dve", xt, xt, y1, ALU.add, [xk, y1k], [xk])
                if final and tok0 < SEQ:
                    self.memset("pool", ssq[0], 0.0, [ssq[1]])
                    self.act(junk[0], xt, AF.Square, [xk], [junk[1], ssq[1]], accum_out=ssq[0])
                    self.act(rstd[0], ssq[0], AF.Sqrt, [ssq[1]], [rstd[1]], scale=1.0 / D, bias=self.epsap(EPS))
                    self.recip(rstd[0], rstd[0], [rstd[1]], [rstd[1]])
                    self.stt("dve", xt, xt, rstd[0][:, 0:1], fn, ALU.mult, ALU.mult, [xk, rstd[1], fnk], [xk])
                    self.dma(self.out[bl, tok0:tok0 + 128, :], xt, [xk], ["out"])
                else:
                    if tok0 < SEQ and not final and last:
                        self.dma(self.out[bl, tok0:tok0 + 128, :], xt, [xk], ["out"])
                    self.dma(self.xs[bl, tok0:tok0 + 128, :], xt, [xk], ["xs"])
        S.barrier()

    def zero_scratch(self):
        self.reset(self.keep)
        z, zk = self.tile([128, 8192], BF16, "zero")
        self.memset("pool", z, 0.0, [zk])
        XBv = self.XB.rearrange("(n p a) d -> n p (a d)", p=128, a=8)
        for i in range(NSLOT // 1024):
            self.dma(XBv[i], z, [zk], ["XB"])
        self.S.barrier()

    def build(self, stop=None):
        self.setup_consts()
        self.setup_eps()
        self.setup_regs()
        self.zero_scratch()

        def fin():
            self.S.drain()
            self.S.emit()
            return self.nc
        for l in self.layers:
            last = (l == DEPTH - 1)
            self.p0_mod(l)
            if stop == "p0":
                return fin()
            for bl in range(NB):
                if l % 2 == 0:
                    self.p1_even(l, bl)
                    if stop == "p1":
                        return fin()
                    self.p2_even(l, bl, not last)
                else:
                    self.p1_diff(l, bl)
                    if stop == "p1":
                        return fin()
                    self.p2_diff(l, bl, not last)
                if stop == "p2":
                    return fin()
            for bl in range(NB):
                self.p3(l, bl, last)
            if stop == "p3":
                return fin()
            self.p3b(l, last)
            if stop == "p3b":
                return fin()
            self.p4(l)
            if stop == "p4":
                return fin()
            self.p5(l, last, self.final and l == self.layers[-1])
        return fin()


_CONSTS = None


def make_in_maps(inp, layers, n_cores, x_override=None, ctx_override=None):
    global _CONSTS
    if _CONSTS is None:
        _CONSTS = _host_consts()
    shared = dict(_CONSTS)
    shared["final_norm"] = np.ascontiguousarray(inp["final_norm"], dtype=np.float32)
    for l in layers:
        shared.update(_layer_arrays(inp, l))
    x = inp["x"] if x_override is None else x_override
    ctx = inp["ctx"] if ctx_override is None else ctx_override
    maps = []
    for i in range(n_cores):
        m = dict(shared)
        m["x_in"] = np.ascontiguousarray(x[NB * i:NB * (i + 1)], dtype=np.float32)
        m["ctx_in"] = np.ascontiguousarray(ctx[NB * i:NB * (i + 1)], dtype=np.float32)
        cc = np.stack([inp["c"][NB * i], inp["c"][NB * i + 1], inp["c_ctx"]], axis=0).astype(np.float32)
        m["ccT"] = np.ascontiguousarray(cc.reshape(3, 8, 128).transpose(2, 1, 0))
        maps.append(m)
    return maps


def kernel(**inputs):
    inp = {k: np.asarray(v) for k, v in inputs.items()}
    layers = list(range(DEPTH))
    mk = MK(layers, final=True)
    nc = mk.build()
    maps = make_in_maps(inp, layers, 8)
    res = run_bass_kernel_spmd(nc, maps, core_ids=list(range(8)))
    out = np.concatenate([np.asarray(r["out"]) for r in res.results], axis=0)
    return out.astype(np.float32)
```

```python
import math
import contextlib
import numpy as np
import ml_dtypes
import concourse.bass as bass
import concourse.mybir as mybir
from concourse.bass_utils import run_bass_kernel_spmd

F32 = mybir.dt.float32
BF16 = mybir.dt.bfloat16
I32 = mybir.dt.int32
U8 = mybir.dt.uint8
ALU = mybir.AluOpType
AF = mybir.ActivationFunctionType
AX = mybir.AxisListType

D = 1024
SEQ = 4096
CTX = 256
T = SEQ + CTX
NCH = T // 128
NB = 2
NE = 32
SBW = 512
NSB = (NB * T * 2 + NE * (SBW - 1) + SBW - 1) // SBW
NSLOT = NSB * SBW
EPS = 1e-6
DEPTH = 4
WCAST_DMA = True
NTILE = NB * NCH

CE = ("pe", "act", "dve", "pool")
DQ = {"sp": 24, "actq": 6, "poolq": 10}
QENG = {"sp": "sp", "actq": "act", "poolq": "pool"}


class Sched:
    def __init__(self, nc, same_engine_sync=True):
        self.nc = nc
        self.ops = {e: [] for e in ("pe", "act", "dve", "pool", "sp")}
        self.seq = {e: 0 for e in CE}
        self.dman = {q: 0 for q in DQ}
        self.last_w = {}
        self.readers = {}
        self.known = {e: {} for e in self.ops}
        self.same = same_engine_sync
        self.nops = 0
        self.pe_noself = False

    def _deps(self, r, w):
        deps = []
        for k in r:
            if k in self.last_w:
                deps.append(self.last_w[k])
        for k in w:
            if k in self.last_w:
                deps.append(self.last_w[k])
            deps.extend(self.readers.get(k, ()))
        return deps

    def _commit(self, ident, r, w):
        for k in r:
            self.readers.setdefault(k, []).append(ident)
        for k in w:
            self.last_w[k] = ident
            self.readers[k] = []

    def _waits(self, stream, deps, own=None):
        kn = self.known[stream]
        need = {}
        for (s, v) in deps:
            if s == own and not self.same:
                continue
            if kn.get(s, 0) >= v:
                continue
            if need.get(s, 0) < v:
                need[s] = v
        for s, v in need.items():
            kn[s] = v
        return list(need.items())

    def op(self, eng, fn, r=(), w=(), sig=True):
        pr = [k for k in r if k.startswith("pb")]
        if pr:
            w = list(w) + [k for k in pr if k not in w]
            r = [k for k in r if not k.startswith("pb")]
        deps = self._deps(r, w)
        ident = (eng, self.seq[eng] + 1)
        if eng == "pe" and self.pe_noself:
            deps = [(s, v) for (s, v) in deps if s != "pe"]
        deps = [(s, v) for (s, v) in deps if not (s == eng and v > self.seq[eng])]
        waits = self._waits(eng, deps, own=eng)
        if sig:
            self.seq[eng] += 1
            self.known[eng][eng] = max(self.known[eng].get(eng, 0), 0)
        self.ops[eng].append((waits, fn, (eng, 1) if sig else None))
        self._commit(ident, r, w)
        self.nops += 1
        return ident

    def dma(self, q, fn, r=(), w=()):
        stream = QENG[q]
        n = self.dman[q]
        self.dman[q] += 1
        R = DQ[q]
        s = "%s_%d" % (q, n % R)
        ident = (s, 16 * (n // R + 1))
        deps = self._deps(r, w)
        if stream in CE:
            deps = [(a, v) for (a, v) in deps if not (a == stream and v > self.seq[stream])]
        if n >= R:
            deps.append((s, 16 * (n // R)))
        waits = self._waits(stream, deps, own=stream if stream in CE else None)
        self.ops[stream].append((waits, fn, (s, 16)))
        self._commit(ident, r, w)
        self.nops += 1
        return ident

    def _all_idents(self):
        deps = []
        for q, R in DQ.items():
            n = self.dman[q]
            for slot in range(min(n, R)):
                cnt = (n - 1 - slot) // R + 1
                deps.append(("%s_%d" % (q, slot), 16 * cnt))
        for e in CE:
            if self.seq[e]:
                deps.append((e, self.seq[e]))
        return deps

    def barrier(self):
        deps = self._all_idents()
        for stream in self.ops:
            d = [(s, v) for (s, v) in deps if s != stream]
            waits = self._waits(stream, d)
            if waits:
                self.ops[stream].append((waits, None, None))
        self.last_w = {}
        self.readers = {}

    def drain(self):
        waits = self._waits("sp", self._all_idents())
        self.ops["sp"].append((waits, None, None))

    def check(self):
        sem = {}
        ptr = {k: 0 for k in self.ops}
        progress = True
        while progress:
            progress = False
            for k, ops in self.ops.items():
                while ptr[k] < len(ops):
                    waits, fn, inc = ops[ptr[k]]
                    if any(sem.get(s_, 0) < v for s_, v in waits):
                        break
                    if inc is not None:
                        sem[inc[0]] = sem.get(inc[0], 0) + inc[1]
                    ptr[k] += 1
                    progress = True
        stuck = {k: (ptr[k], len(v)) for k, v in self.ops.items() if ptr[k] < len(v)}
        if stuck:
            msg = []
            for k, (p, n) in stuck.items():
                waits = self.ops[k][p][0]
                msg.append("%s@%d/%d waits %s have %s" % (k, p, n, waits, [(s_, sem.get(s_, 0)) for s_, _ in waits]))
            raise RuntimeError("DEADLOCK: " + " | ".join(msg))
        return max(sem.values()) if sem else 0

    def emit(self):
        self.check()
        nc = self.nc
        names = list(CE) + ["%s_%d" % (q, i) for q, R in DQ.items() for i in range(R)]
        sems = {}
        with contextlib.ExitStack() as st:
            for nme in names:
                sems[nme] = st.enter_context(nc.semaphore("s_" + nme))
            block = st.enter_context(nc.Block())

            def run(eng, stream):
                for waits, fn, inc in self.ops[stream]:
                    for s, v in waits:
                        eng.wait_ge(sems[s], v)
                    if fn is None:
                        continue
                    ins = fn(eng)
                    if inc is not None:
                        ins.then_inc(sems[inc[0]], inc[1])

            @block.tensor
            def _(e):
                run(e, "pe")

            @block.scalar
            def _(e):
                run(e, "act")

            @block.vector
            def _(e):
                run(e, "dve")

            @block.gpsimd
            def _(e):
                run(e, "pool")

            @block.sync
            def _(e):
                run(e, "sp")


def _rope_tables(dim):
    rows = SEQ // 64
    row = np.repeat(np.arange(rows, dtype=np.float32), 64)
    col = np.tile(np.arange(64, dtype=np.float32), rows)
    nf = dim // 4
    inv = (np.float32(10000.0) ** (-np.arange(nf, dtype=np.float32) / np.float32(nf))).astype(np.float32)
    ang = np.concatenate([row[:, None] * inv, col[:, None] * inv], axis=-1).astype(np.float32)
    return np.cos(ang).astype(np.float32), np.sin(ang).astype(np.float32)


def _host_consts():
    c = {}
    cos64, sin64 = _rope_tables(64)
    cos32, sin32 = _rope_tables(32)
    ct = np.ones((128, T), np.float32)
    st = np.zeros((128, T), np.float32)
    for p in range(128):
        d = p % 64
        ct[p, :SEQ] = cos64[:, d % 32]
        st[p, :SEQ] = -sin64[:, d] if d < 32 else sin64[:, d - 32]
    c["cos64"], c["sin64"] = ct, st
    cm = np.ones((96, T), np.float32)
    sm = np.zeros((96, T), np.float32)
    for p in range(64, 96):
        r = p - 64
        cm[p, :SEQ] = cos32[:, r % 16]
        sm[p, :SEQ] = -sin32[:, r] if r < 16 else sin32[:, r - 16]
    c["cosM"], c["sinM"] = cm, sm
    c["cosR"], c["sinR"] = np.ascontiguousarray(cm[64:96]), np.ascontiguousarray(sm[64:96])
    P64 = np.zeros((128, 128), np.float32)
    for m in range(128):
        k = m + 32 if (m % 64) < 32 else m - 32
        P64[k, m] = 1
    PM = np.zeros((96, 96), np.float32)
    for m in range(64, 96):
        r = m - 64
        k = 64 + (r + 16 if r < 16 else r - 16)
        PM[k, m] = 1
    PR = np.ascontiguousarray(PM[64:96, 64:96])
    bf = ml_dtypes.bfloat16
    c["P64"], c["PM"], c["PR"] = P64.astype(bf), PM.astype(bf), PR.astype(bf)
    c["identb"] = np.eye(128, dtype=np.float32).astype(bf)
    c["identf"] = np.eye(128, dtype=np.float32)
    c["onesb"] = np.ones((128, 128), np.float32).astype(bf)
    tri = np.zeros((128, 128), np.float32)
    for k in range(128):
        tri[k, k + 1:] = 1
    c["triU"] = tri.astype(bf)
    kr = np.arange(128)[:, None]
    qr = np.arange(128)[None, :]
    c["mprev"] = np.tile((qr <= kr).astype(np.float32), (1, 4)).astype(bf)
    c["mnext"] = np.tile((kr <= qr).astype(np.float32), (1, 4)).astype(bf)
    c["sbstart"] = np.tile((np.arange(NSB, dtype=np.float32) * SBW)[None, :, None], (128, 1, NE)).astype(np.float32)
    c["pidx"] = np.arange(128, dtype=np.float32).reshape(128, 1)
    return c


CONST_SPECS = {
    "cos64": ([128, T], F32), "sin64": ([128, T], F32), "cosM": ([96, T], F32), "sinM": ([96, T], F32),
    "cosR": ([32, T], F32), "sinR": ([32, T], F32), "P64": ([128, 128], BF16), "PM": ([96, 96], BF16),
    "PR": ([32, 32], BF16), "identb": ([128, 128], BF16), "identf": ([128, 128], F32),
    "onesb": ([128, 128], BF16), "triU": ([128, 128], BF16), "mprev": ([128, 512], BF16),
    "mnext": ([128, 512], BF16), "sbstart": ([128, NSB, NE], F32), "pidx": ([128, 1], F32),
}


def _layer_specs(l):
    j = l // 2
    s = {
        "ada_w_%d" % l: [D, 6 * D], "ada_b_%d" % l: [6 * D], "norm_mix_%d" % l: [D], "norm_ffn_%d" % l: [D],
        "router_w_%d" % l: [D, 36], "router_b_%d" % l: [36],
        "w1_%d" % l: [NE * 128, 4096], "w3_%d" % l: [NE * 128, 4096], "w2_%d" % l: [NE * 128, 4096],
    }
    if l % 2 == 0:
        s.update({"ab_w_in_%d" % l: [D, 1184], "mla_q_norm_%d" % l: [256], "mla_w_uq_%d" % l: [256, 768],
                  "mla_kv_norm_%d" % l: [128], "wukv_k_%d" % l: [128, 512], "wukv_v_%d" % l: [128, 512],
                  "swa_sink_%d" % l: [8], "w_out_%d" % l: [D, D]})
    else:
        s.update({"diff_w_in_%d" % l: [D, 3072], "lq1_%d" % l: [64], "lk1_%d" % l: [64], "lq2_%d" % l: [64],
                  "lk2_%d" % l: [64], "subln_%d" % l: [128], "w_out_%d" % l: [D, D]})
    return s


def _layer_arrays(inp, l):
    j = l // 2
    a = {
        "ada_w_%d" % l: inp["ada_w"][l], "ada_b_%d" % l: inp["ada_b"][l], "norm_mix_%d" % l: inp["norm_mix"][l],
        "norm_ffn_%d" % l: inp["norm_ffn"][l],
        "router_w_%d" % l: np.ascontiguousarray(np.concatenate([inp["router_group_w"][l], inp["router_expert_w"][l]], axis=1)),
        "router_b_%d" % l: np.ascontiguousarray(np.concatenate([inp["router_group_b"][l], inp["router_expert_b"][l]], axis=0)),
        "w1_%d" % l: inp["expert_w1"][l].reshape(NE, 8, 128, 512).transpose(0, 2, 1, 3).reshape(NE * 128, 4096),
        "w3_%d" % l: inp["expert_w3"][l].reshape(NE, 8, 128, 512).transpose(0, 2, 1, 3).reshape(NE * 128, 4096),
        "w2_%d" % l: inp["expert_w2"][l].reshape(NE, 4, 128, D).transpose(0, 2, 1, 3).reshape(NE * 128, 4096),
    }
    if l % 2 == 0:
        ukv = inp["mla_w_ukv"][j].reshape(128, 8, 128)
        a.update({"ab_w_in_%d" % l: inp["ab_w_in"][j], "mla_q_norm_%d" % l: inp["mla_q_norm"][j],
                  "mla_w_uq_%d" % l: inp["mla_w_uq"][j], "mla_kv_norm_%d" % l: inp["mla_kv_norm"][j],
                  "wukv_k_%d" % l: np.ascontiguousarray(ukv[:, :, :64].reshape(128, 512)),
                  "wukv_v_%d" % l: np.ascontiguousarray(ukv[:, :, 64:].reshape(128, 512)),
                  "swa_sink_%d" % l: inp["swa_sink"][j], "w_out_%d" % l: inp["ab_w_out"][j]})
    else:
        a.update({"diff_w_in_%d" % l: inp["diff_w_in"][j], "lq1_%d" % l: inp["diff_lambda_q1"][j],
                  "lk1_%d" % l: inp["diff_lambda_k1"][j], "lq2_%d" % l: inp["diff_lambda_q2"][j],
                  "lk2_%d" % l: inp["diff_lambda_k2"][j], "subln_%d" % l: inp["diff_subln"][j],
                  "w_out_%d" % l: inp["diff_w_out"][j]})
    return {k: np.ascontiguousarray(v, dtype=np.float32) for k, v in a.items()}


class MK:
    def __init__(self, layers, final=True, debug_outs=()):
        self.layers = list(layers)
        self.final = final
        self.nc = nc = bass.Bass("TRN2", target_bir_lowering=False)
        self.S = Sched(nc)
        self.din = {}
        dt = nc.dram_tensor
        self.din["x_in"] = dt("x_in", [NB, SEQ, D], F32, kind="ExternalInput").ap()
        self.din["ctx_in"] = dt("ctx_in", [NB, CTX, D], F32, kind="ExternalInput").ap()
        self.din["ccT"] = dt("ccT", [128, 8, 3], F32, kind="ExternalInput").ap()
        self.din["final_norm"] = dt("final_norm", [D], F32, kind="ExternalInput").ap()
        for k, (shp, dty) in CONST_SPECS.items():
            self.din[k] = dt(k, shp, dty, kind="ExternalInput").ap()
        for l in self.layers:
            for k, shp in _layer_specs(l).items():
                self.din[k] = dt(k, shp, F32, kind="ExternalInput").ap()
        self.out = dt("out", [NB, SEQ, D], F32, kind="ExternalOutput").ap()

        def scr(name, shape, dty):
            kind = "ExternalOutput" if name in debug_outs else "Internal"
            return dt(name, shape, dty, kind=kind).ap()
        self.xs = scr("xs", [NB, T, D], F32)
        self.modD = scr("modD", [DEPTH, 3, 6 * D], F32)
        self.QA = scr("QA", [NB, 512, T], BF16)
        self.KA = scr("KA", [NB, 128, T], BF16)
        self.VA = scr("VA", [NB, T, 128], BF16)
        self.QM = scr("QM", [NB, 8, 96, T], BF16)
        self.KN = scr("KN", [NB, 512, T], BF16)
        self.KR = scr("KR", [NB, 32, T], BF16)
        self.VM = scr("VM", [NB, T, 512], BF16)
        self.QD = scr("QD", [NB, 8, 128, T], BF16)
        self.KD = scr("KD", [NB, 8, 128, T], BF16)
        self.VD = scr("VD", [NB, T, D], BF16)
        self.OT = scr("OT", [NB, D, T], BF16)
        self.XB = scr("XB", [NSLOT, D], BF16)
        self.H2 = scr("H2", [NB, T, D], BF16)
        self.YB = scr("YB", [NSLOT + 128, D], F32)

        self.arena = nc.alloc_sbuf_tensor("arena", [128, 200 * 1024], U8)
        self.aoff = 0
        self.pbw = [nc.alloc_psum_tensor("pbw%d" % i, [128, 1024], F32) for i in range(4)]
        self.pb = [self.pbw[i // 2][:, (i % 2) * 512:(i % 2 + 1) * 512] for i in range(8)]
        self.pbi = 0
        self.uid = 0

    def reset(self, keep=0):
        self.aoff = keep

    def tile(self, shape, dty, name=None):
        esz = {F32: 4, BF16: 2, I32: 4, U8: 1}[dty]
        n = 1
        for s in shape[1:]:
            n *= s
        nbytes = (n * esz + 31) // 32 * 32
        assert self.aoff + nbytes <= 200 * 1024, ("SBUF arena overflow", name, self.aoff, nbytes)
        ap = self.arena[:, self.aoff:self.aoff + n * esz].bitcast(dty)
        self.aoff += nbytes
        if len(shape) == 3:
            ap = ap.rearrange("p (a b) -> p a b", a=shape[1])
        elif len(shape) == 4:
            ap = ap.rearrange("p (a b c) -> p a b c", a=shape[1], b=shape[2])
        if shape[0] < 128:
            ap = ap[0:shape[0]]
        self.uid += 1
        return ap, "%s#%d" % (name or "t", self.uid)

    def bank(self):
        i = self.pbi % 8
        self.pbi += 1
        return self.pb[i], "pb%d" % i

    def mm(self, out, lhsT, rhs, start, stop, r, w, sig=None):
        if sig is None:
            sig = stop
        self.S.pe_noself = not start
        self.S.op("pe", lambda e: e.matmul(out=out, lhsT=lhsT, rhs=rhs, start=start, stop=stop), r, w, sig)
        self.S.pe_noself = False

    def tr(self, out, in_, ident, r, w, sig=True):
        self.S.op("pe", lambda e: e.transpose(out=out, in_=in_, identity=ident), r, w, sig)

    def act(self, out, in_, func, r, w, **kw):
        self.S.op("act", lambda e: e.activation(out=out, in_=in_, func=func, **kw), r, w)

    def tt(self, eng, out, in0, in1, op, r, w):
        self.S.op(eng, lambda e: e.tensor_tensor(out=out, in0=in0, in1=in1, op=op), r, w)

    def ts(self, eng, out, in0, s1, s2, op0, op1, r, w, **kw):
        if s2 is None:
            self.S.op(eng, lambda e: e.tensor_scalar(out=out, in0=in0, scalar1=s1, scalar2=None, op0=op0, **kw), r, w)
        else:
            self.S.op(eng, lambda e: e.tensor_scalar(out=out, in0=in0, scalar1=s1, scalar2=s2, op0=op0, op1=op1, **kw), r, w)

    def stt(self, eng, out, in0, scalar, in1, op0, op1, r, w):
        self.S.op(eng, lambda e: e.scalar_tensor_tensor(out=out, in0=in0, scalar=scalar, in1=in1, op0=op0, op1=op1), r, w)

    def cp(self, eng, out, in_, r, w):
        if eng == "act":
            self.S.op("act", lambda e: e.activation(out=out, in_=in_, func=AF.Copy), r, w)
        else:
            self.S.op(eng, lambda e: e.tensor_copy(out=out, in_=in_), r, w)

    def memset(self, eng, ap, val, w):
        self.S.op(eng, lambda e: e.memset(ap, val), (), w)

    def recip(self, out, in_, r, w):
        self.S.op("dve", lambda e: e.reciprocal(out=out, in_=in_), r, w)

    def dma(self, out, in_, r, w, q="sp", slow=False):
        if slow:
            self.S.dma(q, lambda e: e.dma_start(out=out, in_=in_, allow_slow_non_contiguous=True), r, w)
        else:
            self.S.dma(q, lambda e: e.dma_start(out=out, in_=in_), r, w)

    def colvec(self, dst, dkey, src1d, r=()):
        self.dma(dst, src1d.rearrange("(c p) -> p c", p=128), r, [dkey], slow=True)

    def bcast(self, dst, dkey, src1d, r=(), parts=128):
        self.dma(dst, src1d.partition_broadcast(parts), r, [dkey])

    def load_bf16(self, dst, dkey, src, stg, eng="pool", r=()):
        sap, skey = stg
        P, n = src.shape[0], src.shape[1]
        self.dma(sap[0:P, 0:n], src, r, [skey])
        self.cp(eng, dst, sap[0:P, 0:n], [skey], [dkey])

    def setup_consts(self):
        self.reset(0)
        C = {}
        for k in ("P64", "PM", "PR", "identb", "identf", "onesb", "triU", "mprev", "mnext", "pidx"):
            shp, dty = CONST_SPECS[k]
            ap, key = self.tile(shp, dty, k)
            self.dma(ap, self.din[k], (), [key])
            C[k] = (ap, key)
        C["rt_slot"] = self.tile([128, NTILE, 2], I32, "rt_slot")
        C["rt_pos"] = self.tile([128, NTILE, 2], F32, "rt_pos")
        C["rt_oh"] = self.tile([128, NTILE, 2, NE], F32, "rt_oh")
        C["widx"] = self.tile([128, NSB], I32, "widx")
        C["rt_gate"] = self.tile([128, NTILE, 2], F32, "rt_gate")
        C["sc"] = self.tile([128, 8, 3], F32, "sc")
        C["base"] = self.tile([128, NE], F32, "base")
        self.C = C
        self.keep = self.aoff
        ap, key = C["sc"]
        self.dma(ap, self.din["ccT"], (), [key])
        self.act(ap, ap, AF.Silu, [key], [key])

    def p0_mod(self, l):
        self.reset(self.keep)
        S = self.S
        sc, sck = self.C["sc"]
        wts = [self.tile([128, 8, 512], F32, "adaw") for _ in range(2)]
        bts = [self.tile([3, 512], F32, "adab") for _ in range(2)]
        ots = [self.tile([3, 512], F32, "modo") for _ in range(2)]
        aw = self.din["ada_w_%d" % l]
        ab = self.din["ada_b_%d" % l]
        for ct in range(12):
            wt, wk = wts[ct % 2]
            bt, bk = bts[ct % 2]
            ot, ok = ots[ct % 2]
            self.dma(wt, aw[:, ct * 512:(ct + 1) * 512].rearrange("(c p) n -> p c n", p=128), (), [wk])
            self.bcast(bt, bk, ab[ct * 512:(ct + 1) * 512], parts=3)
            ps, pk = self.bank()
            for c in range(8):
                self.mm(ps[0:3, :], sc[:, c, :], wt[:, c, :], c == 0, c == 7, [sck, wk], [pk])
            self.tt("dve", ot, ps[0:3, :], bt, ALU.add, [pk, bk], [ok])
            self.dma(self.modD[l, :, ct * 512:(ct + 1) * 512], ot, [ok], ["modD"])
        S.barrier()

    def modv(self, l, r, k):
        return self.modD[l, r, k * D:(k + 1) * D]

    def norm_stats(self, src, Tw, tiles):
        (xt, xk), (junk, jk), (ssq, sk), (rstd, rk), (xn, xnk) = tiles
        ns = Tw // 128
        self.dma(xt[:, 0:ns, :], src.rearrange("(s p) d -> p s d", p=128), (), [xk])
        self.memset("pool", ssq, 0.0, [sk])
        for s in range(ns):
            self.act(junk, xt[:, s, :], AF.Square, [xk], [jk, sk], accum_out=ssq[:, s:s + 1])
        self.act(rstd[:, 0:ns], ssq[:, 0:ns], AF.Sqrt, [sk], [rk], scale=1.0 / D, bias=self.epsap(EPS))
        self.recip(rstd[:, 0:ns], rstd[:, 0:ns], [rk], [rk])
        for s in range(ns):
            self.ts("dve" if s % 2 == 0 else "pool", xn[:, s, :], xt[:, s, :], rstd[:, s:s + 1], None, ALU.mult, None, [xk, rk], [xnk + str(s)])

    def norm_trans(self, Tw, gcol, scol, tiles, hT):
        (xt, xk), (junk, jk), (ssq, sk), (rstd, rk), (xn, xnk) = tiles
        identb, ik = self.C["identb"]
        ns = Tw // 128
        hTa, hk = hT
        for cp_ in range(4):
            ps, pk = self.bank()
            psb = ps[:].bitcast(BF16)
            for cc in range(2):
                c = cp_ * 2 + cc
                for s in range(ns):
                    self.tr(psb[:, cc * 512 + s * 128: cc * 512 + (s + 1) * 128], xn[:, s, c * 128:(c + 1) * 128], identb,
                            [xnk + str(s), ik], [pk], sig=(cc == 1 and s == ns - 1))
            for cc in range(2):
                c = cp_ * 2 + cc
                if cp_ % 2 == 0:
                    self.act(hTa[:, c, 0:Tw], psb[:, cc * 512: cc * 512 + Tw], AF.Identity, [pk, gcol[1], scol[1]], [hk + str(c)],
                             scale=gcol[0][:, c:c + 1], bias=scol[0][:, c:c + 1])
                else:
                    self.ts("dve", hTa[:, c, 0:Tw], psb[:, cc * 512: cc * 512 + Tw], gcol[0][:, c:c + 1], scol[0][:, c:c + 1],
                            ALU.mult, ALU.add, [pk, gcol[1], scol[1]], [hk + str(c)])

    def epsap(self, val):
        if not hasattr(self, "_eps"):
            self._eps = {}
        if val not in self._eps:
            raise KeyError(val)
        return self._eps[val][0]

    def setup_regs(self):
        self.regs = {}

        def mk(name, val):
            def fn(e):
                r = e.alloc_register(name)
                self.regs[name] = r
                return e.reg_mov(r, val)
            return fn
        self.S.op("pool", mk("bslot", NSLOT - 1), (), (), sig=False)
        self.S.op("pool", mk("bw", NE * 128 - 1), (), (), sig=False)

    def setup_eps(self):
        self._eps = {}
        for val in (EPS, 1e-5):
            ap, key = self.tile([128, 1], F32, "eps")
            self.memset("pool", ap, float(val), [key])
            self._eps[val] = (ap, key)
        self.keep = self.aoff

    def mod_cols(self, l, kshift, kscale, normname):
        nm, nmk = self.tile([128, 8], F32, "nmcol")
        self.colvec(nm, nmk, self.din["%s_%d" % (normname, l)])
        res = []
        for r in range(3):
            g, gk = self.tile([128, 8], F32, "gcol")
            s, sk = self.tile([128, 8], F32, "scol")
            self.colvec(g, gk, self.modv(l, r, kscale), r=["modD"])
            self.colvec(s, sk, self.modv(l, r, kshift), r=["modD"])
            self.stt("dve", g, g, 1.0, nm, ALU.add, ALU.mult, [gk, nmk], [gk])
            res.append(((g, gk), (s, sk)))
        return res

    def xsrc(self, l, bl, tok0, n):
        if l == self.layers[0]:
            if tok0 < SEQ:
                return self.din["x_in"][bl, tok0:tok0 + n, :]
            return self.din["ctx_in"][bl, tok0 - SEQ:tok0 - SEQ + n, :]
        return self.xs[bl, tok0:tok0 + n, :]

    def ttiles(self):
        return [(i * 512, 512) for i in range(8)] + [(SEQ, 256)]

    def rope(self, psA, P, Tw, tok0, asb, perm, cosd, sind, tabs, out, okey, rkeys):
        (a_sb, ak), (t1, t1k), (t2, t2k) = asb
        (ct, ctk), (stt_, stk) = tabs
        self.cp("act", a_sb[0:P, 0:Tw], psA[0:P, 0:Tw], rkeys, [ak])

        def tail():
            psB, pbk = self.bank()
            self.mm(psB[0:P, 0:Tw], perm[0][0:P, 0:P], a_sb[0:P, 0:Tw], True, True, [ak, perm[1]], [pbk])
            self.tt("dve", t1[0:P, 0:Tw], psA[0:P, 0:Tw], ct[0:P, 0:Tw], ALU.mult, rkeys + [ctk], [t1k])
            self.tt("dve", t2[0:P, 0:Tw], psB[0:P, 0:Tw], stt_[0:P, 0:Tw], ALU.mult, [pbk, stk], [t2k])
            self.tt("pool", out, t1[0:P, 0:Tw], t2[0:P, 0:Tw], ALU.add, [t1k, t2k], [okey])
        return tail

    def p1_even(self, l, bl):
        self.reset(self.keep)
        S = self.S
        C = self.C
        cols = self.mod_cols(l, 0, 1, "norm_mix")
        stg = self.tile([128, 4096], F32, "stg")
        win, wk = self.tile([128, 8, 1184], BF16, "win")
        wsrc = self.din["ab_w_in_%d" % l]
        for c in range(8):
            self.load_bf16(win[:, c, :], wk, wsrc[c * 128:(c + 1) * 128, :], stg)
        wuq, wuqk = self.tile([128, 2, 768], BF16, "wuq")
        for c in range(2):
            self.load_bf16(wuq[:, c, :], wuqk, self.din["mla_w_uq_%d" % l][c * 128:(c + 1) * 128, :], stg)
        wkk, wkkk = self.tile([128, 512], BF16, "wukvk")
        self.load_bf16(wkk, wkkk, self.din["wukv_k_%d" % l], stg)
        wkv, wkvk = self.tile([128, 512], BF16, "wukvv")
        self.load_bf16(wkv, wkvk, self.din["wukv_v_%d" % l], stg)
        qn, qnk = self.tile([128, 2], F32, "qn")
        self.dma(qn, self.din["mla_q_norm_%d" % l].rearrange("(c p) -> p c", p=128), (), [qnk], slow=True)
        kvn, kvnk = self.tile([128, 1], F32, "kvn")
        self.dma(kvn, self.din["mla_kv_norm_%d" % l].rearrange("(c p) -> p c", p=128), (), [kvnk], slow=True)
        nts = [(self.tile([128, 4, D], F32, "xt"), self.tile([128, D], BF16, "junk"), self.tile([128, 4], F32, "ssq"),
                self.tile([128, 4], F32, "rstd"), self.tile([128, 4, D], BF16, "xn")) for _ in range(2)]
        hTs = [self.tile([128, 8, 512], BF16, "hT") for _ in range(2)]
        asbs = [(self.tile([128, 512], BF16, "asb"), self.tile([128, 512], F32, "t1"), self.tile([128, 512], F32, "t2")) for _ in range(2)]
        c64 = self.tile([128, 512], F32, "c64")
        s64 = self.tile([128, 512], F32, "s64")
        cM = self.tile([96, 512], F32, "cM")
        sM = self.tile([96, 512], F32, "sM")
        cR = self.tile([32, 512], F32, "cR")
        sR = self.tile([32, 512], F32, "sR")
        qa_o = self.tile([128, 4, 512], BF16, "qa_o")
        ka_o = self.tile([128, 512], BF16, "ka_o")
        va_o = self.tile([128, 4, 128], BF16, "va_o")
        qm_o = self.tile([96, 8, 512], BF16, "qm_o")
        kn_o = self.tile([128, 4, 512], BF16, "kn_o")
        vm_o = self.tile([128, 4, 512], BF16, "vm_o")
        kr_o = self.tile([32, 512], BF16, "kr_o")
        sq = self.tile([128, 512], BF16, "sq")
        rs = self.tile([128, 512], F32, "rs")
        cqn = self.tile([128, 2, 512], BF16, "cqn")
        ckvn = self.tile([128, 512], BF16, "ckvn")
        ones, onk = C["onesb"]
        tts = self.ttiles()
        stx = {"ri": 0, "pend": None}

        def defer(tail):
            if stx["pend"] is not None:
                stx["pend"]()
            stx["pend"] = tail

        def flush():
            if stx["pend"] is not None:
                stx["pend"]()
                stx["pend"] = None

        def next_asb():
            stx["ri"] += 1
            return asbs[stx["ri"] % 2]

        def stats(t):
            tok0, Tw = tts[t]
            self.norm_stats(self.xsrc(l, bl, tok0, Tw), Tw, nts[t % 2])

        def trans(t):
            tok0, Tw = tts[t]
            r = bl if tok0 < SEQ else 2
            self.norm_trans(Tw, cols[r][0], cols[r][1], nts[t % 2], hTs[t % 2])

        def projall(t):
            tok0, Tw = tts[t]
            ns = Tw // 128
            hTa, hk = hTs[t % 2]
            for (tab, nm_) in ((c64, "cos64"), (s64, "sin64"), (cM, "cosM"), (sM, "sinM"), (cR, "cosR"), (sR, "sinR")):
                P = self.din[nm_].shape[0]
                self.dma(tab[0][0:P, 0:Tw], self.din[nm_][:, tok0:tok0 + Tw], (), [tab[1]])

            def proj(col0, M, ps, pk, rhs_ap=None, rkeys=None, lw=None):
                for c in range(8):
                    self.mm(ps[0:M, 0:Tw], win[:, c, col0:col0 + M], hTa[:, c, 0:Tw], c == 0, c == 7, [wk, hk + str(c)], [pk])
            for m in range(4):
                ps, pk = self.bank()
                proj(m * 128, 128, ps, pk)
                defer(self.rope(ps, 128, Tw, tok0, next_asb(), C["P64"], None, None, (c64, s64), qa_o[0][:, m, 0:Tw], qa_o[1], [pk]))
            flush()
            self.dma(self.QA[bl, :, tok0:tok0 + Tw].rearrange("(m p) t -> p m t", p=128), qa_o[0][:, :, 0:Tw], [qa_o[1]], ["QA"])
            ps, pk = self.bank()
            proj(512, 128, ps, pk)
            defer(self.rope(ps, 128, Tw, tok0, next_asb(), C["P64"], None, None, (c64, s64), ka_o[0][:, 0:Tw], ka_o[1], [pk]))
            flush()
            self.dma(self.KA[bl, :, tok0:tok0 + Tw], ka_o[0][:, 0:Tw], [ka_o[1]], ["KA"])
            ps, pk = self.bank()
            for s in range(ns):
                for c in range(8):
                    self.mm(ps[:, s * 128:(s + 1) * 128], hTa[:, c, s * 128:(s + 1) * 128], win[:, c, 640:768], c == 0, c == 7,
                            [wk, hk + str(c)], [pk])
            self.cp("act", va_o[0][:, 0:ns, :], ps[:, 0:ns * 128].rearrange("p (s n) -> p s n", s=ns), [pk], [va_o[1]])
            self.dma(self.VA[bl, tok0:tok0 + Tw, :].rearrange("(s p) n -> p s n", p=128), va_o[0][:, 0:ns, :], [va_o[1]], ["VA"])
            pcq = []
            psN, pnk = self.bank()
            for j in range(2):
                ps, pk = self.bank()
                proj(768 + j * 128, 128, ps, pk)
                pcq.append((ps, pk))
                self.act(sq[0][:, 0:Tw], ps[:, 0:Tw], AF.Square, [pk], [sq[1]])
                self.mm(psN[:, 0:Tw], ones, sq[0][:, 0:Tw], j == 0, j == 1, [onk, sq[1]], [pnk])
            self.act(rs[0][:, 0:Tw], psN[:, 0:Tw], AF.Sqrt, [pnk], [rs[1]], scale=1.0 / 256, bias=self.epsap(EPS))
            self.recip(rs[0][:, 0:Tw], rs[0][:, 0:Tw], [rs[1]], [rs[1]])
            for j in range(2):
                self.stt("dve", cqn[0][:, j, 0:Tw], pcq[j][0][:, 0:Tw], qn[:, j:j + 1], rs[0][:, 0:Tw], ALU.mult, ALU.mult,
                         [pcq[j][1], qnk, rs[1]], [cqn[1]])
            for h in range(8):
                ps, pk = self.bank()
                for j in range(2):
                    self.mm(ps[0:96, 0:Tw], wuq[:, j, h * 96:(h + 1) * 96], cqn[0][:, j, 0:Tw], j == 0, j == 1, [wuqk, cqn[1]], [pk])
                defer(self.rope(ps, 96, Tw, tok0, next_asb(), C["PM"], None, None, (cM, sM), qm_o[0][:, h, 0:Tw], qm_o[1], [pk]))
            flush()
            self.dma(self.QM[bl, :, :, tok0:tok0 + Tw].rearrange("h p t -> p h t"), qm_o[0][:, :, 0:Tw], [qm_o[1]], ["QM"])
            ps, pk = self.bank()
            proj(1024, 128, ps, pk)
            self.act(sq[0][:, 0:Tw], ps[:, 0:Tw], AF.Square, [pk], [sq[1]])
            psN, pnk = self.bank()
            self.mm(psN[:, 0:Tw], ones, sq[0][:, 0:Tw], True, True, [onk, sq[1]], [pnk])
            self.act(rs[0][:, 0:Tw], psN[:, 0:Tw], AF.Sqrt, [pnk], [rs[1]], scale=1.0 / 128, bias=self.epsap(EPS))
            self.recip(rs[0][:, 0:Tw], rs[0][:, 0:Tw], [rs[1]], [rs[1]])
            self.stt("dve", ckvn[0][:, 0:Tw], ps[:, 0:Tw], kvn[:, 0:1], rs[0][:, 0:Tw], ALU.mult, ALU.mult, [pk, kvnk, rs[1]], [ckvn[1]])
            for m in range(4):
                ps, pk = self.bank()
                self.mm(ps[:, 0:Tw], wkk[:, m * 128:(m + 1) * 128], ckvn[0][:, 0:Tw], True, True, [wkkk, ckvn[1]], [pk])
                self.cp("act" if m % 2 == 0 else "dve", kn_o[0][:, m, 0:Tw], ps[:, 0:Tw], [pk], [kn_o[1]])
            self.dma(self.KN[bl, :, tok0:tok0 + Tw].rearrange("(m p) t -> p m t", p=128), kn_o[0][:, :, 0:Tw], [kn_o[1]], ["KN"])
            for s in range(ns):
                ps, pk = self.bank()
                self.mm(ps[:, :], ckvn[0][:, s * 128:(s + 1) * 128], wkv[:, :], True, True, [wkvk, ckvn[1]], [pk])
                self.cp("act" if s % 2 == 0 else "dve", vm_o[0][:, s, :], ps[:, :], [pk], [vm_o[1]])
            self.dma(self.VM[bl, tok0:tok0 + Tw, :].rearrange("(s p) n -> p s n", p=128), vm_o[0][:, 0:ns, :], [vm_o[1]], ["VM"])
            ps, pk = self.bank()
            proj(1152, 32, ps, pk)
            defer(self.rope(ps, 32, Tw, tok0, next_asb(), C["PR"], None, None, (cR, sR), kr_o[0][:, 0:Tw], kr_o[1], [pk]))
            flush()
            self.dma(self.KR[bl, :, tok0:tok0 + Tw], kr_o[0][:, 0:Tw], [kr_o[1]], ["KR"])

        stats(0)
        trans(0)
        for t in range(len(tts)):
            if t + 1 < len(tts):
                stats(t + 1)
            projall(t)
            if t + 1 < len(tts):
                trans(t + 1)
        S.barrier()

    def attn_block(self, Kt, Kkey, Krows, qrhs, Qkey, N, chunks, Vt, Vkey, scale, ptiles, acc, acck):
        n = len(chunks)
        for i, (kc, mask) in enumerate(chunks):
            ps, pk = self.bank_s()
            self.mm(ps[:, 0:N], Kt[0:Krows, kc * 128:(kc + 1) * 128], qrhs, True, True, [Kkey, Qkey], [pk])
            pt, ptk = ptiles[self.pti % len(ptiles)]
            self.pti += 1
            self.act(pt[:, 0:N], ps[:, 0:N], AF.Exp, [pk], [ptk], scale=scale)
            if mask is not None:
                self.tt("pool", pt[:, 0:N], pt[:, 0:N], mask[0][:, 0:N], ALU.mult, [ptk, mask[1]], [ptk])
            self.mm(acc[:, 0:N], Vt[:, kc, :], pt[:, 0:N], i == 0, i == n - 1, [Vkey, ptk], [acck])

    def bank_s(self):
        i = self.sbanks[self.sbi % len(self.sbanks)]
        self.sbi += 1
        return self.pb[i], "pb%d" % i

    def finish_div(self, acc, acck, N, ep, out, okey, sinkadd=None):
        (accs, ak), (rd0, rk) = ep
        if sinkadd is not None:
            self.tt("dve", accs[:, 0:N], acc[:, 0:N], sinkadd[0][:, 0:N], ALU.add, [acck, sinkadd[1]], [ak])
        else:
            self.cp("dve", accs[:, 0:N], acc[:, 0:N], [acck], [ak])
        self.recip(accs[64:128, 0:N], accs[64:128, 0:N], [ak], [ak])
        self.cp("pool", rd0[0:64, 0:N], accs[64:128, 0:N], [ak], [rk])
        self.tt("dve", out, accs[0:64, 0:N], rd0[0:64, 0:N], ALU.mult, [ak, rk], [okey])

    def pipeline(self, items, LA):
        n = len(items)
        for i in range(n + LA):
            if i < n:
                items[i][0]()
            if i >= LA:
                items[i - LA][1]()

    def p2_even(self, l, bl, ctx_out):
        self.reset(self.keep)
        S = self.S
        C = self.C
        LA = 3
        self.sbanks = [0, 1, 2, 3, 4]
        self.sbi = 0
        self.pti = 0
        accb = [(self.pb[5], "pb5"), (self.pb[6], "pb6"), (self.pb[7], "pb7")]
        ptiles = [self.tile([128, 512], BF16, "pt") for _ in range(LA + 3)]
        eps_ = [(self.tile([128, 512], F32, "accs"), self.tile([64, 512], F32, "rd0")) for _ in range(2)]
        otl = [self.tile([64, 512], BF16, "otile") for _ in range(3)]
        st = {"acci": 0, "oti": 0, "epi": 0}
        snk, snkk = self.tile([128, 8], F32, "snk")
        self.bcast(snk, snkk, self.din["swa_sink_%d" % l])
        self.act(snk, snk, AF.Exp, [snkk], [snkk])
        sinkadd = []
        for g in range(2):
            sa, sak = self.tile([128, 512], F32, "sinkadd")
            self.memset("pool", sa, 0.0, [sak])
            for j in range(4):
                self.ts("dve", sa[64:128, j * 128:(j + 1) * 128], sa[64:128, j * 128:(j + 1) * 128], snk[64:128, g * 4 + j:g * 4 + j + 1], None,
                        ALU.add, None, [sak, snkk], [sak])
            sinkadd.append((sa, sak))
        Qg = [self.tile([64, 4, T], BF16, "Qg") for _ in range(2)]
        Kg = [self.tile([96, T], BF16, "Kg") for _ in range(2)]
        Vg = [self.tile([128, NCH, 128], BF16, "Vg") for _ in range(2)]
        Qh = [self.tile([96, T], BF16, "Qh") for _ in range(2)]
        for v, vk in Vg:
            self.memset("pool", v[:, :, 64:128], 1.0, [vk + "o"])
        mprev, mnext = C["mprev"], C["mnext"]
        mscale = 96.0 ** -0.5
        items = []

        def make_items(kt, kkeys, krows, qrhs, qk, N, chunks, vt, vkeys, scale, fin):
            accs = {}
            n = len(chunks)
            for i, (kc, mask) in enumerate(chunks):
                cell = {}

                def A(kc=kc, mask=mask, cell=cell):
                    ps, pk = self.bank_s()
                    self.mm(ps[:, 0:N], kt[0:krows, kc * 128:(kc + 1) * 128], qrhs, True, True, list(kkeys) + [qk], [pk])
                    pt, ptk = ptiles[self.pti % len(ptiles)]
                    self.pti += 1
                    self.act(pt[:, 0:N], ps[:, 0:N], AF.Exp, [pk], [ptk], scale=scale)
                    if mask is not None:
                        self.tt("pool", pt[:, 0:N], pt[:, 0:N], mask[0][:, 0:N], ALU.mult, [ptk, mask[1]], [ptk])
                    cell["pt"] = (pt, ptk)

                def B(i=i, kc=kc, cell=cell):
                    if i == 0:
                        accs["a"] = accb[st["acci"] % 3]
                        st["acci"] += 1
                    acc, acck = accs["a"]
                    pt, ptk = cell["pt"]
                    self.mm(acc[:, 0:N], vt[:, kc, :], pt[:, 0:N], i == 0, i == n - 1, list(vkeys) + [ptk], [acck])
                    if i == n - 1:
                        fin(acc, acck)
                items.append((A, B))

        def loads_swa(g):
            q, qk = Qg[g % 2]
            k, kk = Kg[g % 2]
            v, vk = Vg[g % 2]
            self.dma(q, self.QA[bl, g * 256:(g + 1) * 256, :].rearrange("(j p) t -> p j t", p=64), ["QA"], [qk])
            self.dma(k[0:64, :], self.KA[bl, g * 64:(g + 1) * 64, :], ["KA"], [kk])
            self.dma(v[:, :, 0:64], self.VA[bl, :, g * 64:(g + 1) * 64].rearrange("(c p) n -> p c n", p=128), ["VA"], [vk])

        def loads_mla(h):
            q, qk = Qh[h % 2]
            k, kk = Kg[h % 2]
            v, vk = Vg[h % 2]
            self.dma(q, self.QM[bl, h, :, :], ["QM"], [qk])
            self.dma(k[0:64, :], self.KN[bl, h * 64:(h + 1) * 64, :], ["KN"], [kk])
            self.dma(k[64:96, :], self.KR[bl, :, :], ["KR"], [kk + "r"])
            self.dma(v[:, :, 0:64], self.VM[bl, :, h * 64:(h + 1) * 64].rearrange("(c p) n -> p c n", p=128), ["VM"], [vk])

        def fin_swa(g, i):
            def f(acc, acck):
                ot, otk = otl[st["oti"] % 3]
                st["oti"] += 1
                ep = eps_[st["epi"] % 2]
                st["epi"] += 1
                self.finish_div(acc, acck, 512, ep, ot[:, :], otk, sinkadd=sinkadd[g])
                self.dma(self.OT[bl, g * 256:(g + 1) * 256, i * 128:(i + 1) * 128].rearrange("(j p) t -> p j t", p=64),
                         ot.rearrange("p (j t) -> p j t", j=4), [otk], ["OT"], q="poolq")
            return f

        def fin_mla(h, q0, N):
            def f(acc, acck):
                ot, otk = otl[st["oti"] % 3]
                st["oti"] += 1
                ep = eps_[st["epi"] % 2]
                st["epi"] += 1
                self.finish_div(acc, acck, N, ep, ot[:, 0:N], otk)
                self.dma(self.OT[bl, 512 + h * 64: 512 + (h + 1) * 64, q0:q0 + N], ot[:, 0:N], [otk], ["OT"], q="poolq")
            return f

        units = [("swa", 0), ("swa", 1)] + [("mla", h) for h in range(8)]

        def emit_loads(u):
            kind, idx = units[u]
            (loads_swa if kind == "swa" else loads_mla)(idx)

        emit_loads(0)
        for u, (kind, idx) in enumerate(units):
            first = len(items)
            if kind == "swa":
                g = idx
                q, qk = Qg[g % 2]
                k, kk = Kg[g % 2]
                v, vk = Vg[g % 2]
                for i in range(34 if ctx_out else 32):
                    if i < 32:
                        chunks = []
                        if i > 0:
                            chunks.append((i - 1, mprev))
                        chunks.append((i, None))
                        if i < 31:
                            chunks.append((i + 1, mnext))
                        chunks += [(32, None), (33, None)]
                    else:
                        chunks = [(32, None), (33, None)]
                    make_items(k, [kk], 64, q[:, :, i * 128:(i + 1) * 128], qk, 512, chunks, v, [vk, vk + "o"], 0.125, fin_swa(g, i))
            else:
                h = idx
                q, qk = Qh[h % 2]
                k, kk = Kg[h % 2]
                v, vk = Vg[h % 2]
                qts = [(i * 512, 512, [(c, None) for c in range(NCH)]) for i in range(8)]
                if ctx_out:
                    qts.append((SEQ, 256, [(32, None), (33, None)]))
                for (q0, N, chunks) in qts:
                    make_items(k, [kk, kk + "r"], 96, q[:, q0:q0 + N], qk, N, chunks, v, [vk, vk + "o"], mscale, fin_mla(h, q0, N))
            if u + 1 < len(units):
                A0, B0 = items[first + LA]

                def A0w(A0=A0, u=u):
                    emit_loads(u + 1)
                    A0()
                items[first + LA] = (A0w, B0)
        self.pipeline(items, LA)
        S.barrier()

    def attn_block2(self, Kt, Kkeys, Krows, qrhs, Qkey, N, chunks, Vt, Vkeys, scale, ptiles, acc, acck):
        n = len(chunks)
        for i, (kc, mask) in enumerate(chunks):
            ps, pk = self.bank_s()
            self.mm(ps[:, 0:N], Kt[0:Krows, kc * 128:(kc + 1) * 128], qrhs, True, True, list(Kkeys) + [Qkey], [pk])
            pt, ptk = ptiles[self.pti % len(ptiles)]
            self.pti += 1
            self.act(pt[:, 0:N], ps[:, 0:N], AF.Exp, [pk], [ptk], scale=scale)
            if mask is not None:
                self.tt("pool", pt[:, 0:N], pt[:, 0:N], mask[0][:, 0:N], ALU.mult, [ptk, mask[1]], [ptk])
            self.mm(acc[:, 0:N], Vt[:, kc, :], pt[:, 0:N], i == 0, i == n - 1, list(Vkeys) + [ptk], [acck])

    def p1_diff(self, l, bl):
        self.reset(self.keep)
        S = self.S
        C = self.C
        cols = self.mod_cols(l, 0, 1, "norm_mix")
        stg = self.tile([128, 4096], F32, "stg")
        win, wk = self.tile([128, 8, 3072], BF16, "win")
        wsrc = self.din["diff_w_in_%d" % l]
        for c in range(8):
            self.load_bf16(win[:, c, :], wk, wsrc[c * 128:(c + 1) * 128, :], stg, eng="pool" if c % 2 == 0 else "dve")
        nts = [(self.tile([128, 4, D], F32, "xt"), self.tile([128, D], BF16, "junk"), self.tile([128, 4], F32, "ssq"),
                self.tile([128, 4], F32, "rstd"), self.tile([128, 4, D], BF16, "xn")) for _ in range(2)]
        hTs = [self.tile([128, 8, 512], BF16, "hT") for _ in range(2)]
        asbs = [(self.tile([128, 512], BF16, "asb"), self.tile([128, 512], F32, "t1"), self.tile([128, 512], F32, "t2")) for _ in range(2)]
        c64 = self.tile([128, 512], F32, "c64")
        s64 = self.tile([128, 512], F32, "s64")
        q_o = self.tile([128, 8, 512], BF16, "q_o")
        k_o = self.tile([128, 8, 512], BF16, "k_o")
        v_o = self.tile([128, 4, D], BF16, "v_o")
        tts = self.ttiles()
        stx = {"ri": 0, "pend": None}

        def defer(tail):
            if stx["pend"] is not None:
                stx["pend"]()
            stx["pend"] = tail

        def flush():
            if stx["pend"] is not None:
                stx["pend"]()
                stx["pend"] = None

        def next_asb():
            stx["ri"] += 1
            return asbs[stx["ri"] % 2]

        def stats(t):
            tok0, Tw = tts[t]
            self.norm_stats(self.xsrc(l, bl, tok0, Tw), Tw, nts[t % 2])

        def trans(t):
            tok0, Tw = tts[t]
            r = bl if tok0 < SEQ else 2
            self.norm_trans(Tw, cols[r][0], cols[r][1], nts[t % 2], hTs[t % 2])

        def projall(t):
            tok0, Tw = tts[t]
            ns = Tw // 128
            hTa, hk = hTs[t % 2]
            for (tab, nm_) in ((c64, "cos64"), (s64, "sin64")):
                self.dma(tab[0][:, 0:Tw], self.din[nm_][:, tok0:tok0 + Tw], (), [tab[1]])
            for typ, (o_t, dst) in enumerate(((q_o, self.QD), (k_o, self.KD))):
                for m in range(8):
                    ps, pk = self.bank()
                    col0 = typ * 1024 + m * 128
                    for c in range(8):
                        self.mm(ps[:, 0:Tw], win[:, c, col0:col0 + 128], hTa[:, c, 0:Tw], c == 0, c == 7, [wk, hk + str(c)], [pk])
                    defer(self.rope(ps, 128, Tw, tok0, next_asb(), C["P64"], None, None, (c64, s64), o_t[0][:, m, 0:Tw], o_t[1], [pk]))
                flush()
                self.dma(dst[bl, :, :, tok0:tok0 + Tw].rearrange("h p t -> p h t"), o_t[0][:, :, 0:Tw], [o_t[1]], ["QKD%d" % typ])
            for s_ in range(ns):
                for half in range(2):
                    ps, pk = self.bank()
                    for c in range(8):
                        self.mm(ps[:, :], hTa[:, c, s_ * 128:(s_ + 1) * 128], win[:, c, 2048 + half * 512: 2048 + (half + 1) * 512],
                                c == 0, c == 7, [wk, hk + str(c)], [pk])
                    self.cp("act" if half == 0 else "dve", v_o[0][:, s_, half * 512:(half + 1) * 512], ps[:, :], [pk], [v_o[1]])
            self.dma(self.VD[bl, tok0:tok0 + Tw, :].rearrange("(s p) n -> p s n", p=128), v_o[0][:, 0:ns, :], [v_o[1]], ["VD"])

        stats(0)
        trans(0)
        for t in range(len(tts)):
            if t + 1 < len(tts):
                stats(t + 1)
            projall(t)
            if t + 1 < len(tts):
                trans(t + 1)
        S.barrier()

    def p2_diff(self, l, bl, ctx_out):
        self.reset(self.keep)
        S = self.S
        C = self.C
        lam_init = 0.8 - 0.6 * math.exp(-0.3 * l)
        ones, onk = C["onesb"]
        lt = [self.tile([128, 64], F32, "lam") for _ in range(4)]
        for (t_, nm_) in zip(lt, ("lq1", "lk1", "lq2", "lk2")):
            self.bcast(t_[0], t_[1], self.din["%s_%d" % (nm_, l)])
        ls = self.tile([128, 2], F32, "ls")
        self.memset("pool", ls[0], 0.0, [ls[1]])
        lj = self.tile([128, 64], F32, "lj")
        for i in range(2):
            self.tt("dve", lj[0], lt[2 * i][0], lt[2 * i + 1][0], ALU.mult, [lt[2 * i][1], lt[2 * i + 1][1]], [lj[1]])
            self.S.op("dve", (lambda a, b: (lambda e: e.reduce_sum(out=a, in_=b, axis=AX.X)))(ls[0][:, i:i + 1], lj[0]), [lj[1]], [ls[1]])
        self.act(ls[0], ls[0], AF.Exp, [ls[1]], [ls[1]])
        nlam = self.tile([128, 1], F32, "nlam")
        self.tt("dve", nlam[0], ls[0][:, 1:2], ls[0][:, 0:1], ALU.subtract, [ls[1]], [nlam[1]])
        self.ts("dve", nlam[0], nlam[0], -lam_init, None, ALU.add, None, [nlam[1]], [nlam[1]])
        sub = self.tile([128, 1], F32, "sub")
        self.dma(sub[0], self.din["subln_%d" % l].rearrange("(c p) -> p c", p=128), (), [sub[1]], slow=True)
        self.ts("dve", sub[0], sub[0], 1.0 - lam_init, None, ALU.mult, None, [sub[1]], [sub[1]])

        LA = 2
        self.sbanks = [0, 1, 2, 3]
        self.sbi = 0
        self.pti = 0
        ptiles = [self.tile([128, 512], BF16, "pt") for _ in range(2 * (LA + 2))]
        Qh = [self.tile([128, T], BF16, "Qh") for _ in range(2)]
        Kh = [self.tile([128, T], BF16, "Kh") for _ in range(2)]
        Vh = [self.tile([128, NCH, 128], BF16, "Vh") for _ in range(2)]
        r1 = self.tile([128, 512], F32, "r1")
        r2 = self.tile([128, 512], F32, "r2")
        o1 = self.tile([128, 512], F32, "o1")
        o2 = self.tile([128, 512], F32, "o2")
        sq = self.tile([128, 512], BF16, "sq")
        otl = [self.tile([128, 512], BF16, "otile") for _ in range(2)]
        st = {"oti": 0}
        accs = [(self.pb[4], "pb4"), (self.pb[5], "pb5"), (self.pb[6], "pb6"), (self.pb[7], "pb7")]
        items = []

        def loads(h):
            q, qk = Qh[h % 2]
            k, kk = Kh[h % 2]
            v, vk = Vh[h % 2]
            self.dma(q, self.QD[bl, h, :, :], ["QKD0"], [qk])
            self.dma(k, self.KD[bl, h, :, :], ["QKD1"], [kk])
            self.dma(v, self.VD[bl, :, h * 128:(h + 1) * 128].rearrange("(c p) n -> p c n", p=128), ["VD"], [vk])

        def epilogue(h, q0, N):
            self.recip(r1[0][:, 0:N], accs[1][0][:, 0:N], [accs[1][1]], [r1[1]])
            self.recip(r2[0][:, 0:N], accs[3][0][:, 0:N], [accs[3][1]], [r2[1]])
            self.tt("dve", o1[0][:, 0:N], accs[0][0][:, 0:N], r1[0][:, 0:N], ALU.mult, [accs[0][1], r1[1]], [o1[1]])
            self.tt("dve", o2[0][:, 0:N], accs[2][0][:, 0:N], r2[0][:, 0:N], ALU.mult, [accs[2][1], r2[1]], [o2[1]])
            self.stt("dve", o1[0][:, 0:N], o2[0][:, 0:N], nlam[0][:, 0:1], o1[0][:, 0:N], ALU.mult, ALU.add, [o2[1], nlam[1], o1[1]], [o1[1]])
            self.act(sq[0][:, 0:N], o1[0][:, 0:N], AF.Square, [o1[1]], [sq[1]])
            psN, pnk = self.bank_s()
            self.mm(psN[:, 0:N], ones, sq[0][:, 0:N], True, True, [onk, sq[1]], [pnk])
            self.act(r1[0][:, 0:N], psN[:, 0:N], AF.Sqrt, [pnk], [r1[1]], scale=1.0 / 128, bias=self.epsap(1e-5))
            self.recip(r1[0][:, 0:N], r1[0][:, 0:N], [r1[1]], [r1[1]])
            ot, otk = otl[st["oti"] % 2]
            st["oti"] += 1
            self.stt("dve", ot[:, 0:N], o1[0][:, 0:N], sub[0][:, 0:1], r1[0][:, 0:N], ALU.mult, ALU.mult, [o1[1], sub[1], r1[1]], [otk])
            self.dma(self.OT[bl, h * 128:(h + 1) * 128, q0:q0 + N], ot[:, 0:N], [otk], ["OT"], q="poolq")

        loads(0)
        for h in range(8):
            q, qk = Qh[h % 2]
            k, kk = Kh[h % 2]
            v, vk = Vh[h % 2]
            first = len(items)
            qts = [(i * 512, 512, list(range(NCH))) for i in range(8)]
            if ctx_out:
                qts.append((SEQ, 256, [32, 33]))
            for (q0, N, chunks) in qts:
                n = len(chunks)
                for i, kc in enumerate(chunks):
                    cell = {}

                    def A(kc=kc, cell=cell, q=q, k=k, qk=qk, kk=kk, q0=q0, N=N):
                        pts = []
                        for mp in range(2):
                            ps, pk = self.bank_s()
                            self.mm(ps[:, 0:N], k[mp * 64:(mp + 1) * 64, kc * 128:(kc + 1) * 128], q[mp * 64:(mp + 1) * 64, q0:q0 + N], True, True,
                                    [kk, qk], [pk])
                            pt, ptk = ptiles[self.pti % len(ptiles)]
                            self.pti += 1
                            self.act(pt[:, 0:N], ps[:, 0:N], AF.Exp, [pk], [ptk], scale=0.125)
                            pts.append((pt, ptk))
                        cell["pts"] = pts

                    def B(i=i, n=n, kc=kc, cell=cell, v=v, vk=vk, h=h, q0=q0, N=N):
                        for mp in range(2):
                            pt, ptk = cell["pts"][mp]
                            self.mm(accs[2 * mp][0][:, 0:N], v[:, kc, :], pt[:, 0:N], i == 0, i == n - 1, [vk, ptk], [accs[2 * mp][1]])
                            self.mm(accs[2 * mp + 1][0][:, 0:N], ones, pt[:, 0:N], i == 0, i == n - 1, [onk, ptk], [accs[2 * mp + 1][1]])
                        if i == n - 1:
                            epilogue(h, q0, N)
                    items.append((A, B))
            if h + 1 < 8:
                A0, B0 = items[first + LA]

                def A0w(A0=A0, h=h):
                    loads(h + 1)
                    A0()
                items[first + LA] = (A0w, B0)
        self.pipeline(items, LA)
        S.barrier()

    def p3(self, l, bl, last):
        self.reset(self.keep)
        S = self.S
        C = self.C
        stg = self.tile([128, 4096], F32, "stg")
        nf, nfk = self.tile([128, D], F32, "nf")
        self.bcast(nf, nfk, self.din["norm_ffn_%d" % l])
        wo = {}
        g2 = {}
        s2 = {}
        wsrc = self.din["w_out_%d" % l]
        for r in ((bl,) if last else (bl, 2)):
            gt, gk = self.tile([128, D], F32, "gate")
            self.bcast(gt, gk, self.modv(l, r, 2))
            w_, wk_ = self.tile([128, 8, D], BF16, "wo")
            for c in range(8):
                sap, skey = stg
                self.dma(sap[:, 0:D], wsrc[c * 128:(c + 1) * 128, :], (), [skey])
                self.tt("dve" if c % 2 == 0 else "pool", w_[:, c, :], sap[:, 0:D], gt, ALU.mult, [skey, gk], [wk_])
            wo[r] = (w_, wk_)
            a, ak = self.tile([128, D], F32, "gs2")
            self.bcast(a, ak, self.modv(l, r, 4))
            self.stt("dve", a, a, 1.0, nf, ALU.add, ALU.mult, [ak, nfk], [ak])
            g2[r] = (a, ak)
            b, bk = self.tile([128, D], F32, "sh2")
            self.bcast(b, bk, self.modv(l, r, 3))
            s2[r] = (b, bk)
        wr, wrk = self.tile([128, 8, 36], F32, "wr")
        self.dma(wr, self.din["router_w_%d" % l].rearrange("(c p) n -> p c n", p=128), (), [wrk])
        rb, rbk = self.tile([128, 36], F32, "rb")
        self.bcast(rb, rbk, self.din["router_b_%d" % l])
        base, bsk = C["base"]
        if bl == 0:
            self.memset("pool", base, 0.0, [bsk])
        ots = [self.tile([128, 8, 128], BF16, "ot") for _ in range(3)]
        xts = [self.tile([128, D], F32, "xt") for _ in range(3)]
        junk = self.tile([128, D], BF16, "junk")
        h2s = [self.tile([128, D], F32, "h2") for _ in range(3)]
        h2b = [self.tile([128, D], BF16, "h2b") for _ in range(2)]
        h2Ts = [self.tile([128, 8, 128], F32, "h2T") for _ in range(2)]
        sms = [{nm_: self.tile([128, w_], F32, nm_) for nm_, w_ in (("lg", 36), ("gmax", 1), ("ngmax", 1), ("ohg", 4),
                                                                    ("gsum", 1), ("pgrp", 1), ("negm", 32), ("elm", 32), ("top8", 8),
                                                                    ("ex", 1), ("den", 1), ("A", 32), ("posv", 32),
                                                                    ("junk32", 32), ("junk32b", 32), ("nm1", 1))} for _ in range(2)]
        ssqs = [self.tile([128, 1], F32, "ssq") for _ in range(3)]
        rstds = [self.tile([128, 1], F32, "rstd") for _ in range(3)]
        Abs = [self.tile([128, NE], BF16, "Ab") for _ in range(3)]
        identf, ifk = C["identf"]
        triU, tuk = C["triU"]
        ones, onk = C["onesb"]
        rt_gate, rgk = C["rt_gate"]
        rt_oh, rohk = C["rt_oh"]
        rt_pos, rpk = C["rt_pos"]
        ntile = 32 if last else NCH
        cells = {}

        def S1a(ti):
            tok0 = ti * 128
            r = bl if tok0 < SEQ else 2
            ot, otk = ots[ti % 3]
            xt, xk = xts[ti % 3]
            self.dma(ot, self.OT[bl, :, tok0:tok0 + 128].rearrange("(c p) t -> p c t", p=128), ["OT"], [otk])
            self.dma(xt, self.xsrc(l, bl, tok0, 128), (), [xk])
            w_, wk_ = wo[r]
            for half in range(2):
                ps, pk = self.bank()
                for c in range(8):
                    self.mm(ps[:, :], ot[:, c, :], w_[:, c, half * 512:(half + 1) * 512], c == 0, c == 7, [otk, wk_], [pk])
                self.tt("dve", xt[:, half * 512:(half + 1) * 512], xt[:, half * 512:(half + 1) * 512], ps[:, :], ALU.add, [xk, pk], [xk])
            self.dma(self.xs[bl, tok0:tok0 + 128, :], xt, [xk], ["xs"], q="poolq")
            ssq, ssk = ssqs[ti % 3]
            rstd, rsk_ = rstds[ti % 3]
            self.memset("pool", ssq, 0.0, [ssk])
            self.act(junk[0], xt, AF.Square, [xk], [junk[1], ssk], accum_out=ssq)
            self.act(rstd, ssq, AF.Sqrt, [ssk], [rsk_], scale=1.0 / D, bias=self.epsap(EPS))

        def S1b(ti):
            tok0 = ti * 128
            r = bl if tok0 < SEQ else 2
            xt, xk = xts[ti % 3]
            rstd, rsk_ = rstds[ti % 3]
            self.recip(rstd, rstd, [rsk_], [rsk_])
            h2, h2k = h2s[ti % 3]
            self.stt("dve", h2, xt, rstd[:, 0:1], g2[r][0], ALU.mult, ALU.mult, [xk, rsk_, g2[r][1]], [h2k])
            self.tt("pool", h2, h2, s2[r][0], ALU.add, [h2k, s2[r][1]], [h2k])
            hb, hbk = h2b[ti % 2]
            self.cp("act", hb, h2, [h2k], [hbk])
            self.dma(self.H2[bl, tok0:tok0 + 128, :], hb, [hbk], ["H2"], q="poolq")

        def S2a(ti):
            h2, h2k = h2s[ti % 3]
            h2T = h2Ts[ti % 2]
            for half in range(2):
                ps, pk = self.bank()
                for cc in range(4):
                    c = half * 4 + cc
                    self.tr(ps[:, cc * 128:(cc + 1) * 128], h2[:, c * 128:(c + 1) * 128], identf, [h2k, ifk], [pk], sig=(cc == 3))
                self.cp("act" if half == 0 else "dve", h2T[0][:, half * 4:(half + 1) * 4, :], ps[:, :].rearrange("p (c t) -> p c t", c=4), [pk],
                        [h2T[1] + str(half)])
            ps, pk = self.bank()
            for c in range(8):
                self.mm(ps[:, 0:36], h2T[0][:, c, :], wr[:, c, :], c == 0, c == 7, [h2T[1] + str(c // 4), wrk], [pk])
            cells[ti] = (ps, pk)

        def S2b(ti):
            gti = bl * NCH + ti
            sm = sms[ti % 2]
            t = lambda n_: sm[n_][0]
            k_ = lambda n_: sm[n_][1]
            ps, pk = cells.pop(ti)
            self.tt("dve", t("lg"), ps[:, 0:36], rb, ALU.add, [pk, rbk], [k_("lg")])
            self.S.op("dve", (lambda a, b: (lambda e: e.reduce_max(out=a, in_=b, axis=AX.X)))(t("gmax"), t("lg")[:, 0:4]), [k_("lg")], [k_("gmax")])
            self.ts("dve", t("ohg"), t("lg")[:, 0:4], t("gmax")[:, 0:1], None, ALU.is_equal, None, [k_("lg"), k_("gmax")], [k_("ohg")])
            self.ts("dve", t("ngmax"), t("gmax"), -1.0, None, ALU.mult, None, [k_("gmax")], [k_("ngmax")])
            self.memset("pool", t("gsum"), 0.0, [k_("gsum")])
            self.act(t("junk32")[:, 0:4], t("lg")[:, 0:4], AF.Exp, [k_("lg"), k_("ngmax")], [k_("junk32"), k_("gsum")], bias=t("ngmax")[:, 0:1],
                     accum_out=t("gsum"))
            self.ts("dve", t("negm").rearrange("p (g e) -> p g e", g=4), t("ohg").unsqueeze(2).to_broadcast([128, 4, 8]), -1.0, 1e30,
                    ALU.add, ALU.mult, [k_("ohg")], [k_("negm")])
            self.tt("dve", t("elm"), t("lg")[:, 4:36], t("negm"), ALU.add, [k_("lg"), k_("negm")], [k_("elm")])
            self.S.op("dve", (lambda a, b: (lambda e: e.max(out=a, in_=b)))(t("top8"), t("elm")), [k_("elm")], [k_("top8")])
            oh1 = rt_oh[:, gti, 0, :]
            oh2 = rt_oh[:, gti, 1, :]
            ohk = rohk + str(gti)
            self.ts("dve", oh1, t("elm"), t("top8")[:, 0:1], None, ALU.is_equal, None, [k_("elm"), k_("top8")], [ohk + "a"])
            self.ts("dve", oh2, t("elm"), t("top8")[:, 1:2], None, ALU.is_equal, None, [k_("elm"), k_("top8")], [ohk + "b"])
            self.ts("dve", t("nm1"), t("top8")[:, 0:1], -1.0, None, ALU.mult, None, [k_("top8")], [k_("nm1")])
            self.act(t("ex"), t("top8")[:, 1:2], AF.Exp, [k_("top8"), k_("nm1")], [k_("ex")], bias=t("nm1")[:, 0:1])

        def S2c(ti):
            gti = bl * NCH + ti
            sm = sms[ti % 2]
            t = lambda n_: sm[n_][0]
            k_ = lambda n_: sm[n_][1]
            oh1 = rt_oh[:, gti, 0, :]
            oh2 = rt_oh[:, gti, 1, :]
            ohk = rohk + str(gti)
            self.recip(t("pgrp"), t("gsum"), [k_("gsum")], [k_("pgrp")])
            self.ts("dve", t("den"), t("ex"), 1.0, None, ALU.add, None, [k_("ex")], [k_("den")])
            self.recip(t("den"), t("den"), [k_("den")], [k_("den")])
            self.tt("dve", rt_gate[:, gti, 0:1], t("pgrp"), t("den"), ALU.mult, [k_("pgrp"), k_("den")], [rgk + "a%d" % gti])
            self.tt("dve", rt_gate[:, gti, 1:2], rt_gate[:, gti, 0:1], t("ex"), ALU.mult, [rgk + "a%d" % gti, k_("ex")], [rgk + "b%d" % gti])
            self.tt("dve", t("A"), oh1, oh2, ALU.add, [ohk + "a", ohk + "b"], [k_("A")])
            Ab = Abs[ti % 3]
            self.cp("dve", Ab[0], t("A"), [k_("A")], [Ab[1]])

        def S3(ti):
            gti = bl * NCH + ti
            sm = sms[ti % 2]
            t = lambda n_: sm[n_][0]
            k_ = lambda n_: sm[n_][1]
            Ab = Abs[ti % 3]
            oh1 = rt_oh[:, gti, 0, :]
            oh2 = rt_oh[:, gti, 1, :]
            ohk = rohk + str(gti)
            psr, prk = self.bank()
            self.mm(psr[:, 0:NE], triU, Ab[0], True, True, [tuk, Ab[1]], [prk])
            psc, pck = self.bank()
            self.mm(psc[:, 0:NE], ones, Ab[0], True, True, [onk, Ab[1]], [pck])
            self.tt("dve", t("posv"), psr[:, 0:NE], base, ALU.add, [prk, bsk], [k_("posv")])
            self.tt("dve", base, base, psc[:, 0:NE], ALU.add, [bsk, pck], [bsk])
            for kk_, (ohx, okx) in enumerate(((oh1, ohk + "a"), (oh2, ohk + "b"))):
                self.tt("dve", t("junk32b"), ohx, t("posv"), ALU.mult, [okx, k_("posv")], [k_("junk32b")])
                self.S.op("dve", (lambda a, b: (lambda e: e.reduce_sum(out=a, in_=b, axis=AX.X)))(rt_pos[:, gti, kk_:kk_ + 1], t("junk32b")),
                          [k_("junk32b")], [rpk + "%d_%d" % (gti, kk_)])

        stages = [S1a, S1b, S2a, S2b, S2c, S3]
        for i in range(ntile + len(stages) - 1):
            for d, fn in enumerate(stages):
                if 0 <= i - d < ntile:
                    fn(i - d)
        S.barrier()

    def p3b(self, l, last):
        self.reset(self.keep)
        S = self.S
        C = self.C
        base, bsk = C["base"]
        rt_oh, rohk = C["rt_oh"]
        rt_pos, rpk = C["rt_pos"]
        rt_slot, rsk = C["rt_slot"]
        widx, wik = C["widx"]
        pidx, pik = C["pidx"]
        padded = self.tile([128, NE], F32, "padded")
        tmp = self.tile([128, NE], F32, "tmp")
        pstart = self.tile([128, NE], F32, "pstart")
        pend = self.tile([128, NE], F32, "pend")
        sbs = self.tile([128, NSB, NE], F32, "sbs")
        self.dma(sbs[0], self.din["sbstart"], (), [sbs[1]])
        cmp_ = self.tile([128, NSB, NE], F32, "cmp")
        self.tt("dve", cmp_[0], base.unsqueeze(1).to_broadcast([128, NSB, NE]), sbs[0], ALU.is_gt, [bsk, sbs[1]], [cmp_[1]])
        self.S.op("dve", (lambda a, b: (lambda e: e.reduce_sum(out=a, in_=b, axis=AX.X)))(tmp[0], cmp_[0].rearrange("p s e -> p e s")),
                  [cmp_[1]], [tmp[1]])
        self.ts("dve", padded[0], tmp[0], float(SBW), None, ALU.mult, None, [tmp[1]], [padded[1]])
        self.memset("pool", pstart[0], 0.0, [pstart[1]])
        for e in range(1, NE):
            self.tt("dve", pstart[0][:, e:e + 1], pstart[0][:, e - 1:e], padded[0][:, e - 1:e], ALU.add, [pstart[1], padded[1]], [pstart[1]])
        self.tt("dve", pend[0], pstart[0], padded[0], ALU.add, [pstart[1], padded[1]], [pend[1]])
        self.tt("dve", cmp_[0], pend[0].unsqueeze(1).to_broadcast([128, NSB, NE]), sbs[0], ALU.is_le, [pend[1], sbs[1]], [cmp_[1]])
        be = self.tile([128, NSB], F32, "be")
        self.S.op("dve", (lambda a, b: (lambda e: e.reduce_sum(out=a, in_=b, axis=AX.X)))(be[0], cmp_[0]), [cmp_[1]], [be[1]])
        self.ts("dve", be[0], be[0], float(NE - 1), None, ALU.min, None, [be[1]], [be[1]])
        self.ts("dve", be[0], be[0], 128.0, None, ALU.mult, None, [be[1]], [be[1]])
        self.ts("dve", be[0], be[0], pidx[:, 0:1], None, ALU.add, None, [be[1], pik], [be[1]])
        self.cp("dve", widx, be[0], [be[1]], [wik])
        sl = self.tile([128, 2], F32, "sl")
        j32 = self.tile([128, NE], F32, "j32")
        hbs = [self.tile([128, D], BF16, "hb") for _ in range(3)]
        it = 0
        for bl in range(NB):
            for ti in range(32 if last else NCH):
                gti = bl * NCH + ti
                hb, hbk = hbs[it % 3]
                it += 1
                self.dma(hb, self.H2[bl, ti * 128:(ti + 1) * 128, :], ["H2"], [hbk])
                for kk_ in range(2):
                    self.tt("dve", j32[0], rt_oh[:, gti, kk_, :], pstart[0], ALU.mult, [pstart[1]], [j32[1]])
                    self.S.op("dve", (lambda a, b: (lambda e: e.reduce_sum(out=a, in_=b, axis=AX.X)))(sl[0][:, kk_:kk_ + 1], j32[0]),
                              [j32[1]], [sl[1]])
                self.tt("dve", sl[0], sl[0], rt_pos[:, gti, :], ALU.add, [sl[1]], [sl[1]])
                self.cp("dve", rt_slot[:, gti, :], sl[0], [sl[1]], [rsk + str(gti)])
                for kk_ in range(2):
                    self.S.dma("poolq", (lambda o_, i_, s_: (lambda e: e.indirect_dma_start(
                        out=o_, out_offset=bass.IndirectOffsetOnAxis(ap=s_, axis=0), in_=i_, in_offset=None,
                        bounds_check=self.regs["bslot"], oob_is_err=False)))(self.XB, hb, rt_slot[:, gti, kk_:kk_ + 1]),
                        [hbk, rsk + str(gti)], ["XB"])
        S.barrier()

    def p4(self, l):
        self.reset(self.keep)
        S = self.S
        C = self.C
        identb, ik = C["identb"]
        widx, wik = C["widx"]
        w1b = [self.tile([128, 8, 512], BF16, "w1b") for _ in range(2)]
        w3b = [self.tile([128, 8, 512], BF16, "w3b") for _ in range(2)]
        w2b = [self.tile([128, 4, D], BF16, "w2b") for _ in range(2)]
        xbs = [self.tile([128, 4, D], BF16, "xb") for _ in range(2)]
        xbTs = [self.tile([128, 8, 512], BF16, "xbT") for _ in range(2)]
        gTs = [self.tile([128, 4, 512], BF16, "gT") for _ in range(2)]
        s1s = [self.tile([128, 512], F32, "s1") for _ in range(2)]
        youts = [self.tile([128, 4, D], F32, "yout") for _ in range(2)]
        W1, W3, W2 = self.din["w1_%d" % l], self.din["w3_%d" % l], self.din["w2_%d" % l]

        def LW(sb):
            for (dstt, src) in ((w1b[sb % 2], W1), (w3b[sb % 2], W3), (w2b[sb % 2], W2)):
                dst, dk = dstt
                dflat = dst.rearrange("p a b -> p (a b)")
                self.S.dma("poolq", (lambda o_, i_, s_: (lambda e: e.indirect_dma_start(
                    out=o_, out_offset=None, in_=i_, in_offset=bass.IndirectOffsetOnAxis(ap=s_, axis=0),
                    bounds_check=self.regs["bw"], oob_is_err=False)))(dflat, src, widx[:, sb:sb + 1]), [wik], [dk])

        def LX(sb):
            xb, xbk = xbs[sb % 2]
            self.dma(xb, self.XB[sb * SBW:(sb + 1) * SBW, :].rearrange("(s p) d -> p s d", p=128), ["XB"], [xbk])

        def Tr(sb):
            xb, xbk = xbs[sb % 2]
            xbT = xbTs[sb % 2]
            for cp_ in range(4):
                ps, pk = self.bank()
                psb = ps[:].bitcast(BF16)
                for cc in range(2):
                    c = cp_ * 2 + cc
                    for s_ in range(4):
                        self.tr(psb[:, cc * 512 + s_ * 128: cc * 512 + (s_ + 1) * 128], xb[:, s_, c * 128:(c + 1) * 128], identb, [xbk, ik], [pk],
                                sig=(cc == 1 and s_ == 3))
                ev = "act" if cp_ % 2 == 0 else "dve"
                self.cp(ev, xbT[0][:, cp_ * 2, :], psb[:, 0:512], [pk], [xbT[1] + str(cp_ * 2)])
                self.cp(ev, xbT[0][:, cp_ * 2 + 1, :], psb[:, 512:1024], [pk], [xbT[1] + str(cp_ * 2 + 1)])

        def H(sb):
            a1, k1 = w1b[sb % 2]
            a3, k3 = w3b[sb % 2]
            xbT = xbTs[sb % 2]
            gT = gTs[sb % 2]
            for f in range(4):
                ps1, pk1 = self.bank()
                for c in range(8):
                    self.mm(ps1[:, :], a1[:, c, f * 128:(f + 1) * 128], xbT[0][:, c, :], c == 0, c == 7, [k1, xbT[1] + str(c)], [pk1])
                ps3, pk3 = self.bank()
                for c in range(8):
                    self.mm(ps3[:, :], a3[:, c, f * 128:(f + 1) * 128], xbT[0][:, c, :], c == 0, c == 7, [k3, xbT[1] + str(c)], [pk3])
                s1, s1k = s1s[f % 2]
                self.act(s1, ps1[:, :], AF.Silu, [pk1], [s1k])
                self.tt("dve", gT[0][:, f, :], s1, ps3[:, :], ALU.mult, [s1k, pk3], [gT[1] + str(f)])

        def Y(sb):
            a2, k2 = w2b[sb % 2]
            gT = gTs[sb % 2]
            yo, yok = youts[sb % 2]
            for s_ in range(4):
                for half in range(2):
                    psy, pky = self.bank()
                    for f in range(4):
                        self.mm(psy[:, :], gT[0][:, f, s_ * 128:(s_ + 1) * 128], a2[:, f, half * 512:(half + 1) * 512], f == 0, f == 3,
                                [gT[1] + str(f), k2], [pky])
                    self.cp("act" if half == 0 else "dve", yo[:, s_, half * 512:(half + 1) * 512], psy[:, :], [pky], [yok])
            self.dma(self.YB[sb * SBW:(sb + 1) * SBW, :].rearrange("(s p) d -> p s d", p=128), yo, [yok], ["YB"])

        LW(0)
        LX(0)
        Tr(0)
        for sb in range(NSB):
            if sb + 1 < NSB:
                LW(sb + 1)
                LX(sb + 1)
            H(sb)
            if sb + 1 < NSB:
                Tr(sb + 1)
            Y(sb)
        S.barrier()

    def p5(self, l, last, final):
        self.reset(self.keep)
        S = self.S
        C = self.C
        rt_gslot, rgsk = C["rt_slot"]
        rt_gate, rgk = C["rt_gate"]
        m5 = {}
        for r in ((0, 1) if last else (0, 1, 2)):
            a, ak = self.tile([128, D], F32, "m5")
            self.bcast(a, ak, self.modv(l, r, 5))
            m5[r] = (a, ak)
        if final:
            fn, fnk = self.tile([128, D], F32, "fn")
            self.bcast(fn, fnk, self.din["final_norm"])
        NBUF = 3
        y1s = [self.tile([128, D], F32, "y1") for _ in range(NBUF)]
        y2s = [self.tile([128, D], F32, "y2") for _ in range(NBUF)]
        xts = [self.tile([128, D], F32, "xt") for _ in range(NBUF)]
        junk = self.tile([128, D], BF16, "junk")
        ssqs = [self.tile([128, 1], F32, "ssq") for _ in range(2)]
        rstds = [self.tile([128, 1], F32, "rstd") for _ in range(2)]
        tiles = [(bl, ti) for bl in range(NB) for ti in range(32 if last else NCH)]

        def loads(it):
            bl, ti = tiles[it]
            tok0 = ti * 128
            gti = bl * NCH + ti
            for (yy, yk), kk_ in ((y1s[it % NBUF], 0), (y2s[it % NBUF], 1)):
                self.S.dma("poolq", (lambda o_, i_, s_: (lambda e: e.indirect_dma_start(
                    out=o_, out_offset=None, in_=i_, in_offset=bass.IndirectOffsetOnAxis(ap=s_, axis=0),
                    bounds_check=self.regs["bslot"], oob_is_err=False)))(yy, self.YB, rt_gslot[:, gti, kk_:kk_ + 1]),
                    ["YB"], [yk])
            xt, xk = xts[it % NBUF]
            self.dma(xt, self.xs[bl, tok0:tok0 + 128, :], ["xs_t%d" % gti], [xk])

        def compute(it):
            bl, ti = tiles[it]
            tok0 = ti * 128
            r = bl if tok0 < SEQ else 2
            gti = bl * NCH + ti
            y1, y1k = y1s[it % NBUF]
            y2, y2k = y2s[it % NBUF]
            xt, xk = xts[it % NBUF]
            self.ts("dve", y1, y1, rt_gate[:, gti, 0:1], None, ALU.mult, None, [y1k], [y1k])
            self.stt("dve", y1, y2, rt_gate[:, gti, 1:2], y1, ALU.mult, ALU.add, [y2k, y1k], [y1k])
            self.tt("dve", y1, y1, m5[r][0], ALU.mult, [y1k, m5[r][1]], [y1k])
            self.tt("dve", xt, xt, y1, ALU.add, [xk, y1k], [xk])
            if final and tok0 < SEQ:
                ssq = ssqs[it % 2]
                rstd = rstds[it % 2]
                self.memset("pool", ssq[0], 0.0, [ssq[1]])
                self.act(junk[0], xt, AF.Square, [xk], [junk[1], ssq[1]], accum_out=ssq[0])
                self.act(rstd[0], ssq[0], AF.Sqrt, [ssq[1]], [rstd[1]], scale=1.0 / D, bias=self.epsap(EPS))
                self.recip(rstd[0], rstd[0], [rstd[1]], [rstd[1]])
                self.stt("dve", xt, xt, rstd[0][:, 0:1], fn, ALU.mult, ALU.mult, [xk, rstd[1], fnk], [xk])
                self.dma(self.out[bl, tok0:tok0 + 128, :], xt, [xk], ["out"])
            else:
                if tok0 < SEQ and not final and last:
                    self.dma(self.out[bl, tok0:tok0 + 128, :], xt, [xk], ["out"])
                self.dma(self.xs[bl, tok0:tok0 + 128, :], xt, [xk], ["xs_t%d" % gti])

        n = len(tiles)
        for it in range(min(NBUF - 1, n)):
            loads(it)
        for it in range(n):
            if it + NBUF - 1 < n:
                loads(it + NBUF - 1)
            compute(it)
        S.barrier()

    def zero_scratch(self):
        self.reset(self.keep)
        z, zk = self.tile([128, 8192], BF16, "zero")
        self.memset("pool", z, 0.0, [zk])
        XBv = self.XB.rearrange("(n p a) d -> n p (a d)", p=128, a=8)
        for i in range(NSLOT // 1024):
            self.dma(XBv[i], z, [zk], ["XB"])
        self.S.barrier()

    def build(self, stop=None):
        self.setup_consts()
        self.setup_eps()
        self.setup_regs()
        self.zero_scratch()

        def fin():
            self.S.drain()
            self.S.emit()
            return self.nc
        for l in self.layers:
            last = (l == DEPTH - 1)
            self.p0_mod(l)
            if stop == "p0":
                return fin()
            for bl in range(NB):
                if l % 2 == 0:
                    self.p1_even(l, bl)
                    if stop == "p1":
                        return fin()
                    self.p2_even(l, bl, not last)
                else:
                    self.p1_diff(l, bl)
                    if stop == "p1":
                        return fin()
                    self.p2_diff(l, bl, not last)
                if stop == "p2":
                    return fin()
            for bl in range(NB):
                self.p3(l, bl, last)
            if stop == "p3":
                return fin()
            self.p3b(l, last)
            if stop == "p3b":
                return fin()
            self.p4(l)
            if stop == "p4":
                return fin()
            self.p5(l, last, self.final and l == self.layers[-1])
        return fin()


_CONSTS = None


def make_in_maps(inp, layers, n_cores, x_override=None, ctx_override=None):
    global _CONSTS
    if _CONSTS is None:
        _CONSTS = _host_consts()
    shared = dict(_CONSTS)
    shared["final_norm"] = np.ascontiguousarray(inp["final_norm"], dtype=np.float32)
    for l in layers:
        shared.update(_layer_arrays(inp, l))
    x = inp["x"] if x_override is None else x_override
    ctx = inp["ctx"] if ctx_override is None else ctx_override
    maps = []
    for i in range(n_cores):
        m = dict(shared)
        m["x_in"] = np.ascontiguousarray(x[NB * i:NB * (i + 1)], dtype=np.float32)
        m["ctx_in"] = np.ascontiguousarray(ctx[NB * i:NB * (i + 1)], dtype=np.float32)
        cc = np.stack([inp["c"][NB * i], inp["c"][NB * i + 1], inp["c_ctx"]], axis=0).astype(np.float32)
        m["ccT"] = np.ascontiguousarray(cc.reshape(3, 8, 128).transpose(2, 1, 0))
        maps.append(m)
    return maps


def kernel(**inputs):
    inp = {k: np.asarray(v) for k, v in inputs.items()}
    layers = list(range(DEPTH))
    mk = MK(layers, final=True)
    nc = mk.build()
    maps = make_in_maps(inp, layers, 8)
    res = run_bass_kernel_spmd(nc, maps, core_ids=list(range(8)))
    out = np.concatenate([np.asarray(r["out"]) for r in res.results], axis=0)
    return out.astype(np.float32)
```

```python
import math
import contextlib
import numpy as np
import ml_dtypes
import concourse.bass as bass
import concourse.mybir as mybir
from concourse.bass_utils import run_bass_kernel_spmd

F32 = mybir.dt.float32
BF16 = mybir.dt.bfloat16
I32 = mybir.dt.int32
U8 = mybir.dt.uint8
ALU = mybir.AluOpType
AF = mybir.ActivationFunctionType
AX = mybir.AxisListType

D = 1024
SEQ = 4096
CTX = 256
T = SEQ + CTX
NCH = T // 128
NB = 2
NE = 32
SBW = 512
NSB = (NB * T * 2 + NE * (SBW - 1) + SBW - 1) // SBW
NSLOT = NSB * SBW
EPS = 1e-6
DEPTH = 4
WCAST_DMA = True
NTILE = NB * NCH

CE = ("pe", "act", "dve", "pool")
DQ = {"sp": 24, "actq": 6, "poolq": 10}
QENG = {"sp": "sp", "actq": "act", "poolq": "pool"}


class Sched:
    def __init__(self, nc, same_engine_sync=True):
        self.nc = nc
        self.ops = {e: [] for e in ("pe", "act", "dve", "pool", "sp")}
        self.seq = {e: 0 for e in CE}
        self.dman = {q: 0 for q in DQ}
        self.last_w = {}
        self.readers = {}
        self.known = {e: {} for e in self.ops}
        self.same = same_engine_sync
        self.nops = 0
        self.pe_noself = False

    def _deps(self, r, w):
        deps = []
        for k in r:
            if k in self.last_w:
                deps.append(self.last_w[k])
        for k in w:
            if k in self.last_w:
                deps.append(self.last_w[k])
            deps.extend(self.readers.get(k, ()))
        return deps

    def _commit(self, ident, r, w):
        for k in r:
            self.readers.setdefault(k, []).append(ident)
        for k in w:
            self.last_w[k] = ident
            self.readers[k] = []

    def _waits(self, stream, deps, own=None):
        kn = self.known[stream]
        need = {}
        for (s, v) in deps:
            if s == own and not self.same:
                continue
            if kn.get(s, 0) >= v:
                continue
            if need.get(s, 0) < v:
                need[s] = v
        for s, v in need.items():
            kn[s] = v
        return list(need.items())

    def op(self, eng, fn, r=(), w=(), sig=True):
        pr = [k for k in r if k.startswith("pb")]
        if pr:
            w = list(w) + [k for k in pr if k not in w]
            r = [k for k in r if not k.startswith("pb")]
        deps = self._deps(r, w)
        ident = (eng, self.seq[eng] + 1)
        if eng == "pe" and self.pe_noself:
            deps = [(s, v) for (s, v) in deps if s != "pe"]
        deps = [(s, v) for (s, v) in deps if not (s == eng and v > self.seq[eng])]
        waits = self._waits(eng, deps, own=eng)
        if sig:
            self.seq[eng] += 1
            self.known[eng][eng] = max(self.known[eng].get(eng, 0), 0)
        self.ops[eng].append((waits, fn, (eng, 1) if sig else None))
        self._commit(ident, r, w)
        self.nops += 1
        return ident

    def dma(self, q, fn, r=(), w=()):
        stream = QENG[q]
        n = self.dman[q]
        self.dman[q] += 1
        R = DQ[q]
        s = "%s_%d" % (q, n % R)
        ident = (s, 16 * (n // R + 1))
        deps = self._deps(r, w)
        if stream in CE:
            deps = [(a, v) for (a, v) in deps if not (a == stream and v > self.seq[stream])]
        if n >= R:
            deps.append((s, 16 * (n // R)))
        waits = self._waits(stream, deps, own=stream if stream in CE else None)
        self.ops[stream].append((waits, fn, (s, 16)))
        self._commit(ident, r, w)
        self.nops += 1
        return ident

    def _all_idents(self):
        deps = []
        for q, R in DQ.items():
            n = self.dman[q]
            for slot in range(min(n, R)):
                cnt = (n - 1 - slot) // R + 1
                deps.append(("%s_%d" % (q, slot), 16 * cnt))
        for e in CE:
            if self.seq[e]:
                deps.append((e, self.seq[e]))
        return deps

    def barrier(self):
        deps = self._all_idents()
        for stream in self.ops:
            d = [(s, v) for (s, v) in deps if s != stream]
            waits = self._waits(stream, d)
            if waits:
                self.ops[stream].append((waits, None, None))
        self.last_w = {}
        self.readers = {}

    def drain(self):
        waits = self._waits("sp", self._all_idents())
        self.ops["sp"].append((waits, None, None))

    def check(self):
        sem = {}
        ptr = {k: 0 for k in self.ops}
        progress = True
        while progress:
            progress = False
            for k, ops in self.ops.items():
                while ptr[k] < len(ops):
                    waits, fn, inc = ops[ptr[k]]
                    if any(sem.get(s_, 0) < v for s_, v in waits):
                        break
                    if inc is not None:
                        sem[inc[0]] = sem.get(inc[0], 0) + inc[1]
                    ptr[k] += 1
                    progress = True
        stuck = {k: (ptr[k], len(v)) for k, v in self.ops.items() if ptr[k] < len(v)}
        if stuck:
            msg = []
            for k, (p, n) in stuck.items():
                waits = self.ops[k][p][0]
                msg.append("%s@%d/%d waits %s have %s" % (k, p, n, waits, [(s_, sem.get(s_, 0)) for s_, _ in waits]))
            raise RuntimeError("DEADLOCK: " + " | ".join(msg))
        return max(sem.values()) if sem else 0

    def emit(self):
        self.check()
        nc = self.nc
        names = list(CE) + ["%s_%d" % (q, i) for q, R in DQ.items() for i in range(R)]
        sems = {}
        with contextlib.ExitStack() as st:
            for nme in names:
                sems[nme] = st.enter_context(nc.semaphore("s_" + nme))
            block = st.enter_context(nc.Block())

            def run(eng, stream):
                for waits, fn, inc in self.ops[stream]:
                    for s, v in waits:
                        eng.wait_ge(sems[s], v)
                    if fn is None:
                        continue
                    ins = fn(eng)
                    if inc is not None:
                        ins.then_inc(sems[inc[0]], inc[1])

            @block.tensor
            def _(e):
                run(e, "pe")

            @block.scalar
            def _(e):
                run(e, "act")

            @block.vector
            def _(e):
                run(e, "dve")

            @block.gpsimd
            def _(e):
                run(e, "pool")

            @block.sync
            def _(e):
                run(e, "sp")


def _rope_tables(dim):
    rows = SEQ // 64
    row = np.repeat(np.arange(rows, dtype=np.float32), 64)
    col = np.tile(np.arange(64, dtype=np.float32), rows)
    nf = dim // 4
    inv = (np.float32(10000.0) ** (-np.arange(nf, dtype=np.float32) / np.float32(nf))).astype(np.float32)
    ang = np.concatenate([row[:, None] * inv, col[:, None] * inv], axis=-1).astype(np.float32)
    return np.cos(ang).astype(np.float32), np.sin(ang).astype(np.float32)


def _host_consts():
    c = {}
    cos64, sin64 = _rope_tables(64)
    cos32, sin32 = _rope_tables(32)
    ct = np.ones((128, T), np.float32)
    st = np.zeros((128, T), np.float32)
    for p in range(128):
        d = p % 64
        ct[p, :SEQ] = cos64[:, d % 32]
        st[p, :SEQ] = -sin64[:, d] if d < 32 else sin64[:, d - 32]
    c["cos64"], c["sin64"] = ct, st
    cm = np.ones((96, T), np.float32)
    sm = np.zeros((96, T), np.float32)
    for p in range(64, 96):
        r = p - 64
        cm[p, :SEQ] = cos32[:, r % 16]
        sm[p, :SEQ] = -sin32[:, r] if r < 16 else sin32[:, r - 16]
    c["cosM"], c["sinM"] = cm, sm
    c["cosR"], c["sinR"] = np.ascontiguousarray(cm[64:96]), np.ascontiguousarray(sm[64:96])
    P64 = np.zeros((128, 128), np.float32)
    for m in range(128):
        k = m + 32 if (m % 64) < 32 else m - 32
        P64[k, m] = 1
    PM = np.zeros((96, 96), np.float32)
    for m in range(64, 96):
        r = m - 64
        k = 64 + (r + 16 if r < 16 else r - 16)
        PM[k, m] = 1
    PR = np.ascontiguousarray(PM[64:96, 64:96])
    bf = ml_dtypes.bfloat16
    c["P64"], c["PM"], c["PR"] = P64.astype(bf), PM.astype(bf), PR.astype(bf)
    c["identb"] = np.eye(128, dtype=np.float32).astype(bf)
    c["identf"] = np.eye(128, dtype=np.float32)
    c["onesb"] = np.ones((128, 128), np.float32).astype(bf)
    tri = np.zeros((128, 128), np.float32)
    for k in range(128):
        tri[k, k + 1:] = 1
    c["triU"] = tri.astype(bf)
    kr = np.arange(128)[:, None]
    qr = np.arange(128)[None, :]
    c["mprev"] = np.tile((qr <= kr).astype(np.float32), (1, 4)).astype(bf)
    c["mnext"] = np.tile((kr <= qr).astype(np.float32), (1, 4)).astype(bf)
    c["sbstart"] = np.tile((np.arange(NSB, dtype=np.float32) * SBW)[None, :, None], (128, 1, NE)).astype(np.float32)
    c["pidx"] = np.arange(128, dtype=np.float32).reshape(128, 1)
    return c


CONST_SPECS = {
    "cos64": ([128, T], F32), "sin64": ([128, T], F32), "cosM": ([96, T], F32), "sinM": ([96, T], F32),
    "cosR": ([32, T], F32), "sinR": ([32, T], F32), "P64": ([128, 128], BF16), "PM": ([96, 96], BF16),
    "PR": ([32, 32], BF16), "identb": ([128, 128], BF16), "identf": ([128, 128], F32),
    "onesb": ([128, 128], BF16), "triU": ([128, 128], BF16), "mprev": ([128, 512], BF16),
    "mnext": ([128, 512], BF16), "sbstart": ([128, NSB, NE], F32), "pidx": ([128, 1], F32),
}


def _layer_specs(l):
    j = l // 2
    s = {
        "ada_w_%d" % l: [D, 6 * D], "ada_b_%d" % l: [6 * D], "norm_mix_%d" % l: [D], "norm_ffn_%d" % l: [D],
        "router_w_%d" % l: [D, 36], "router_b_%d" % l: [36],
        "w1_%d" % l: [NE * 128, 4096], "w3_%d" % l: [NE * 128, 4096], "w2_%d" % l: [NE * 128, 4096],
    }
    if l % 2 == 0:
        s.update({"ab_w_in_%d" % l: [D, 1184], "mla_q_norm_%d" % l: [256], "mla_w_uq_%d" % l: [256, 768],
                  "mla_kv_norm_%d" % l: [128], "wukv_k_%d" % l: [128, 512], "wukv_v_%d" % l: [128, 512],
                  "swa_sink_%d" % l: [8], "w_out_%d" % l: [D, D]})
    else:
        s.update({"diff_w_in_%d" % l: [D, 3072], "lq1_%d" % l: [64], "lk1_%d" % l: [64], "lq2_%d" % l: [64],
                  "lk2_%d" % l: [64], "subln_%d" % l: [128], "w_out_%d" % l: [D, D]})
    return s


def _layer_arrays(inp, l):
    j = l // 2
    a = {
        "ada_w_%d" % l: inp["ada_w"][l], "ada_b_%d" % l: inp["ada_b"][l], "norm_mix_%d" % l: inp["norm_mix"][l],
        "norm_ffn_%d" % l: inp["norm_ffn"][l],
        "router_w_%d" % l: np.ascontiguousarray(np.concatenate([inp["router_group_w"][l], inp["router_expert_w"][l]], axis=1)),
        "router_b_%d" % l: np.ascontiguousarray(np.concatenate([inp["router_group_b"][l], inp["router_expert_b"][l]], axis=0)),
        "w1_%d" % l: inp["expert_w1"][l].reshape(NE, 8, 128, 512).transpose(0, 2, 1, 3).reshape(NE * 128, 4096),
        "w3_%d" % l: inp["expert_w3"][l].reshape(NE, 8, 128, 512).transpose(0, 2, 1, 3).reshape(NE * 128, 4096),
        "w2_%d" % l: inp["expert_w2"][l].reshape(NE, 4, 128, D).transpose(0, 2, 1, 3).reshape(NE * 128, 4096),
    }
    if l % 2 == 0:
        ukv = inp["mla_w_ukv"][j].reshape(128, 8, 128)
        a.update({"ab_w_in_%d" % l: inp["ab_w_in"][j], "mla_q_norm_%d" % l: inp["mla_q_norm"][j],
                  "mla_w_uq_%d" % l: inp["mla_w_uq"][j], "mla_kv_norm_%d" % l: inp["mla_kv_norm"][j],
                  "wukv_k_%d" % l: np.ascontiguousarray(ukv[:, :, :64].reshape(128, 512)),
                  "wukv_v_%d" % l: np.ascontiguousarray(ukv[:, :, 64:].reshape(128, 512)),
                  "swa_sink_%d" % l: inp["swa_sink"][j], "w_out_%d" % l: inp["ab_w_out"][j]})
    else:
        a.update({"diff_w_in_%d" % l: inp["diff_w_in"][j], "lq1_%d" % l: inp["diff_lambda_q1"][j],
                  "lk1_%d" % l: inp["diff_lambda_k1"][j], "lq2_%d" % l: inp["diff_lambda_q2"][j],
                  "lk2_%d" % l: inp["diff_lambda_k2"][j], "subln_%d" % l: inp["diff_subln"][j],
                  "w_out_%d" % l: inp["diff_w_out"][j]})
    return {k: np.ascontiguousarray(v, dtype=np.float32) for k, v in a.items()}


class MK:
    def __init__(self, layers, final=True, debug_outs=()):
        self.layers = list(layers)
        self.final = final
        self.nc = nc = bass.Bass("TRN2", target_bir_lowering=False)
        self.S = Sched(nc)
        self.din = {}
        dt = nc.dram_tensor
        self.din["x_in"] = dt("x_in", [NB, SEQ, D], F32, kind="ExternalInput").ap()
        self.din["ctx_in"] = dt("ctx_in", [NB, CTX, D], F32, kind="ExternalInput").ap()
        self.din["ccT"] = dt("ccT", [128, 8, 3], F32, kind="ExternalInput").ap()
        self.din["final_norm"] = dt("final_norm", [D], F32, kind="ExternalInput").ap()
        for k, (shp, dty) in CONST_SPECS.items():
            self.din[k] = dt(k, shp, dty, kind="ExternalInput").ap()
        for l in self.layers:
            for k, shp in _layer_specs(l).items():
                self.din[k] = dt(k, shp, F32, kind="ExternalInput").ap()
        self.out = dt("out", [NB, SEQ, D], F32, kind="ExternalOutput").ap()

        def scr(name, shape, dty):
            kind = "ExternalOutput" if name in debug_outs else "Internal"
            return dt(name, shape, dty, kind=kind).ap()
        self.xs = scr("xs", [NB, T, D], F32)
        self.modD = scr("modD", [DEPTH, 3, 6 * D], F32)
        self.QA = scr("QA", [NB, 512, T], BF16)
        self.KA = scr("KA", [NB, 128, T], BF16)
        self.VA = scr("VA", [NB, T, 128], BF16)
        self.QM = scr("QM", [NB, 8, 96, T], BF16)
        self.KN = scr("KN", [NB, 512, T], BF16)
        self.KR = scr("KR", [NB, 32, T], BF16)
        self.VM = scr("VM", [NB, T, 512], BF16)
        self.QD = scr("QD", [NB, 8, 128, T], BF16)
        self.KD = scr("KD", [NB, 8, 128, T], BF16)
        self.VD = scr("VD", [NB, T, D], BF16)
        self.OT = scr("OT", [NB, D, T], BF16)
        self.XB = scr("XB", [NSLOT, D], BF16)
        self.H2 = scr("H2", [NB, T, D], BF16)
        self.YB = scr("YB", [NSLOT + 128, D], F32)

        self.arena = nc.alloc_sbuf_tensor("arena", [128, 200 * 1024], U8)
        self.aoff = 0
        self.pbw = [nc.alloc_psum_tensor("pbw%d" % i, [128, 1024], F32) for i in range(4)]
        self.pb = [self.pbw[i // 2][:, (i % 2) * 512:(i % 2 + 1) * 512] for i in range(8)]
        self.pbi = 0
        self.uid = 0

    def reset(self, keep=0):
        self.aoff = keep

    def tile(self, shape, dty, name=None):
        esz = {F32: 4, BF16: 2, I32: 4, U8: 1}[dty]
        n = 1
        for s in shape[1:]:
            n *= s
        nbytes = (n * esz + 31) // 32 * 32
        assert self.aoff + nbytes <= 200 * 1024, ("SBUF arena overflow", name, self.aoff, nbytes)
        ap = self.arena[:, self.aoff:self.aoff + n * esz].bitcast(dty)
        self.aoff += nbytes
        if len(shape) == 3:
            ap = ap.rearrange("p (a b) -> p a b", a=shape[1])
        elif len(shape) == 4:
            ap = ap.rearrange("p (a b c) -> p a b c", a=shape[1], b=shape[2])
        if shape[0] < 128:
            ap = ap[0:shape[0]]
        self.uid += 1
        return ap, "%s#%d" % (name or "t", self.uid)

    def bank(self):
        i = self.pbi % 8
        self.pbi += 1
        return self.pb[i], "pb%d" % i

    def mm(self, out, lhsT, rhs, start, stop, r, w, sig=None):
        if sig is None:
            sig = stop
        self.S.pe_noself = not start
        self.S.op("pe", lambda e: e.matmul(out=out, lhsT=lhsT, rhs=rhs, start=start, stop=stop), r, w, sig)
        self.S.pe_noself = False

    def tr(self, out, in_, ident, r, w, sig=True):
        self.S.op("pe", lambda e: e.transpose(out=out, in_=in_, identity=ident), r, w, sig)

    def act(self, out, in_, func, r, w, **kw):
        self.S.op("act", lambda e: e.activation(out=out, in_=in_, func=func, **kw), r, w)

    def tt(self, eng, out, in0, in1, op, r, w):
        self.S.op(eng, lambda e: e.tensor_tensor(out=out, in0=in0, in1=in1, op=op), r, w)

    def ts(self, eng, out, in0, s1, s2, op0, op1, r, w, **kw):
        if s2 is None:
            self.S.op(eng, lambda e: e.tensor_scalar(out=out, in0=in0, scalar1=s1, scalar2=None, op0=op0, **kw), r, w)
        else:
            self.S.op(eng, lambda e: e.tensor_scalar(out=out, in0=in0, scalar1=s1, scalar2=s2, op0=op0, op1=op1, **kw), r, w)

    def stt(self, eng, out, in0, scalar, in1, op0, op1, r, w):
        self.S.op(eng, lambda e: e.scalar_tensor_tensor(out=out, in0=in0, scalar=scalar, in1=in1, op0=op0, op1=op1), r, w)

    def cp(self, eng, out, in_, r, w):
        if eng == "act":
            self.S.op("act", lambda e: e.activation(out=out, in_=in_, func=AF.Copy), r, w)
        else:
            self.S.op(eng, lambda e: e.tensor_copy(out=out, in_=in_), r, w)

    def memset(self, eng, ap, val, w):
        self.S.op(eng, lambda e: e.memset(ap, val), (), w)

    def recip(self, out, in_, r, w):
        self.S.op("dve", lambda e: e.reciprocal(out=out, in_=in_), r, w)

    def dma(self, out, in_, r, w, q="sp", slow=False):
        if slow:
            self.S.dma(q, lambda e: e.dma_start(out=out, in_=in_, allow_slow_non_contiguous=True), r, w)
        else:
            self.S.dma(q, lambda e: e.dma_start(out=out, in_=in_), r, w)

    def colvec(self, dst, dkey, src1d, r=()):
        self.dma(dst, src1d.rearrange("(c p) -> p c", p=128), r, [dkey], slow=True)

    def bcast(self, dst, dkey, src1d, r=(), parts=128):
        self.dma(dst, src1d.partition_broadcast(parts), r, [dkey])

    def load_bf16(self, dst, dkey, src, stg, eng="pool", r=()):
        sap, skey = stg
        P, n = src.shape[0], src.shape[1]
        self.dma(sap[0:P, 0:n], src, r, [skey])
        self.cp(eng, dst, sap[0:P, 0:n], [skey], [dkey])

    def setup_consts(self):
        self.reset(0)
        C = {}
        for k in ("P64", "PM", "PR", "identb", "identf", "onesb", "triU", "mprev", "mnext", "pidx"):
            shp, dty = CONST_SPECS[k]
            ap, key = self.tile(shp, dty, k)
            self.dma(ap, self.din[k], (), [key])
            C[k] = (ap, key)
        C["rt_slot"] = self.tile([128, NTILE, 2], I32, "rt_slot")
        C["rt_pos"] = self.tile([128, NTILE, 2], F32, "rt_pos")
        C["rt_oh"] = self.tile([128, NTILE, 2, NE], F32, "rt_oh")
        C["widx"] = self.tile([128, NSB], I32, "widx")
        C["rt_gate"] = self.tile([128, NTILE, 2], F32, "rt_gate")
        C["sc"] = self.tile([128, 8, 3], F32, "sc")
        C["base"] = self.tile([128, NE], F32, "base")
        self.C = C
        self.keep = self.aoff
        ap, key = C["sc"]
        self.dma(ap, self.din["ccT"], (), [key])
        self.act(ap, ap, AF.Silu, [key], [key])

    def p0_mod(self, l):
        self.reset(self.keep)
        S = self.S
        sc, sck = self.C["sc"]
        wts = [self.tile([128, 8, 512], F32, "adaw") for _ in range(2)]
        bts = [self.tile([3, 512], F32, "adab") for _ in range(2)]
        ots = [self.tile([3, 512], F32, "modo") for _ in range(2)]
        aw = self.din["ada_w_%d" % l]
        ab = self.din["ada_b_%d" % l]
        for ct in range(12):
            wt, wk = wts[ct % 2]
            bt, bk = bts[ct % 2]
            ot, ok = ots[ct % 2]
            self.dma(wt, aw[:, ct * 512:(ct + 1) * 512].rearrange("(c p) n -> p c n", p=128), (), [wk])
            self.bcast(bt, bk, ab[ct * 512:(ct + 1) * 512], parts=3)
            ps, pk = self.bank()
            for c in range(8):
                self.mm(ps[0:3, :], sc[:, c, :], wt[:, c, :], c == 0, c == 7, [sck, wk], [pk])
            self.tt("dve", ot, ps[0:3, :], bt, ALU.add, [pk, bk], [ok])
            self.dma(self.modD[l, :, ct * 512:(ct + 1) * 512], ot, [ok], ["modD"])
        S.barrier()

    def modv(self, l, r, k):
        return self.modD[l, r, k * D:(k + 1) * D]

    def norm_stats(self, src, Tw, tiles):
        (xt, xk), (junk, jk), (ssq, sk), (rstd, rk), (xn, xnk) = tiles
        ns = Tw // 128
        self.dma(xt[:, 0:ns, :], src.rearrange("(s p) d -> p s d", p=128), (), [xk])
        self.memset("pool", ssq, 0.0, [sk])
        for s in range(ns):
            self.act(junk, xt[:, s, :], AF.Square, [xk], [jk, sk], accum_out=ssq[:, s:s + 1])
        self.act(rstd[:, 0:ns], ssq[:, 0:ns], AF.Sqrt, [sk], [rk], scale=1.0 / D, bias=self.epsap(EPS))
        self.recip(rstd[:, 0:ns], rstd[:, 0:ns], [rk], [rk])
        for s in range(ns):
            self.ts("dve" if s % 2 == 0 else "pool", xn[:, s, :], xt[:, s, :], rstd[:, s:s + 1], None, ALU.mult, None, [xk, rk], [xnk + str(s)])

    def norm_trans(self, Tw, gcol, scol, tiles, hT):
        (xt, xk), (junk, jk), (ssq, sk), (rstd, rk), (xn, xnk) = tiles
        identb, ik = self.C["identb"]
        ns = Tw // 128
        hTa, hk = hT
        for cp_ in range(4):
            ps, pk = self.bank()
            psb = ps[:].bitcast(BF16)
            for cc in range(2):
                c = cp_ * 2 + cc
                for s in range(ns):
                    self.tr(psb[:, cc * 512 + s * 128: cc * 512 + (s + 1) * 128], xn[:, s, c * 128:(c + 1) * 128], identb,
                            [xnk + str(s), ik], [pk], sig=(cc == 1 and s == ns - 1))
            for cc in range(2):
                c = cp_ * 2 + cc
                if cp_ % 2 == 0:
                    self.act(hTa[:, c, 0:Tw], psb[:, cc * 512: cc * 512 + Tw], AF.Identity, [pk, gcol[1], scol[1]], [hk + str(c)],
                             scale=gcol[0][:, c:c + 1], bias=scol[0][:, c:c + 1])
                else:
                    self.ts("dve", hTa[:, c, 0:Tw], psb[:, cc * 512: cc * 512 + Tw], gcol[0][:, c:c + 1], scol[0][:, c:c + 1],
                            ALU.mult, ALU.add, [pk, gcol[1], scol[1]], [hk + str(c)])

    def epsap(self, val):
        if not hasattr(self, "_eps"):
            self._eps = {}
        if val not in self._eps:
            raise KeyError(val)
        return self._eps[val][0]

    def setup_regs(self):
        self.regs = {}

        def mk(name, val):
            def fn(e):
                r = e.alloc_register(name)
                self.regs[name] = r
                return e.reg_mov(r, val)
            return fn
        self.S.op("pool", mk("bslot", NSLOT - 1), (), (), sig=False)
        self.S.op("pool", mk("bw", NE * 128 - 1), (), (), sig=False)

    def setup_eps(self):
        self._eps = {}
        for val in (EPS, 1e-5):
            ap, key = self.tile([128, 1], F32, "eps")
            self.memset("pool", ap, float(val), [key])
            self._eps[val] = (ap, key)
        self.keep = self.aoff

    def mod_cols(self, l, kshift, kscale, normname):
        nm, nmk = self.tile([128, 8], F32, "nmcol")
        self.colvec(nm, nmk, self.din["%s_%d" % (normname, l)])
        res = []
        for r in range(3):
            g, gk = self.tile([128, 8], F32, "gcol")
            s, sk = self.tile([128, 8], F32, "scol")
            self.colvec(g, gk, self.modv(l, r, kscale), r=["modD"])
            self.colvec(s, sk, self.modv(l, r, kshift), r=["modD"])
            self.stt("dve", g, g, 1.0, nm, ALU.add, ALU.mult, [gk, nmk], [gk])
            res.append(((g, gk), (s, sk)))
        return res

    def xsrc(self, l, bl, tok0, n):
        if l == self.layers[0]:
            if tok0 < SEQ:
                return self.din["x_in"][bl, tok0:tok0 + n, :]
            return self.din["ctx_in"][bl, tok0 - SEQ:tok0 - SEQ + n, :]
        return self.xs[bl, tok0:tok0 + n, :]

    def ttiles(self):
        return [(i * 512, 512) for i in range(8)] + [(SEQ, 256)]

    def rope(self, psA, P, Tw, tok0, asb, perm, cosd, sind, tabs, out, okey, rkeys):
        (a_sb, ak), (t1, t1k), (t2, t2k) = asb
        (ct, ctk), (stt_, stk) = tabs
        self.cp("act", a_sb[0:P, 0:Tw], psA[0:P, 0:Tw], rkeys, [ak])

        def tail():
            psB, pbk = self.bank()
            self.mm(psB[0:P, 0:Tw], perm[0][0:P, 0:P], a_sb[0:P, 0:Tw], True, True, [ak, perm[1]], [pbk])
            self.tt("dve", t1[0:P, 0:Tw], psA[0:P, 0:Tw], ct[0:P, 0:Tw], ALU.mult, rkeys + [ctk], [t1k])
            self.tt("dve", t2[0:P, 0:Tw], psB[0:P, 0:Tw], stt_[0:P, 0:Tw], ALU.mult, [pbk, stk], [t2k])
            self.tt("pool", out, t1[0:P, 0:Tw], t2[0:P, 0:Tw], ALU.add, [t1k, t2k], [okey])
        return tail

    def p1_even(self, l, bl):
        self.reset(self.keep)
        S = self.S
        C = self.C
        cols = self.mod_cols(l, 0, 1, "norm_mix")
        stg = self.tile([128, 4096], F32, "stg")
        win, wk = self.tile([128, 8, 1184], BF16, "win")
        wsrc = self.din["ab_w_in_%d" % l]
        for c in range(8):
            self.load_bf16(win[:, c, :], wk, wsrc[c * 128:(c + 1) * 128, :], stg)
        wuq, wuqk = self.tile([128, 2, 768], BF16, "wuq")
        for c in range(2):
            self.load_bf16(wuq[:, c, :], wuqk, self.din["mla_w_uq_%d" % l][c * 128:(c + 1) * 128, :], stg)
        wkk, wkkk = self.tile([128, 512], BF16, "wukvk")
        self.load_bf16(wkk, wkkk, self.din["wukv_k_%d" % l], stg)
        wkv, wkvk = self.tile([128, 512], BF16, "wukvv")
        self.load_bf16(wkv, wkvk, self.din["wukv_v_%d" % l], stg)
        qn, qnk = self.tile([128, 2], F32, "qn")
        self.dma(qn, self.din["mla_q_norm_%d" % l].rearrange("(c p) -> p c", p=128), (), [qnk], slow=True)
        kvn, kvnk = self.tile([128, 1], F32, "kvn")
        self.dma(kvn, self.din["mla_kv_norm_%d" % l].rearrange("(c p) -> p c", p=128), (), [kvnk], slow=True)
        nts = [(self.tile([128, 4, D], F32, "xt"), self.tile([128, D], BF16, "junk"), self.tile([128, 4], F32, "ssq"),
                self.tile([128, 4], F32, "rstd"), self.tile([128, 4, D], BF16, "xn")) for _ in range(2)]
        hTs = [self.tile([128, 8, 512], BF16, "hT") for _ in range(2)]
        asbs = [(self.tile([128, 512], BF16, "asb"), self.tile([128, 512], F32, "t1"), self.tile([128, 512], F32, "t2")) for _ in range(2)]
        c64 = self.tile([128, 512], F32, "c64")
        s64 = self.tile([128, 512], F32, "s64")
        cM = self.tile([96, 512], F32, "cM")
        sM = self.tile([96, 512], F32, "sM")
        cR = self.tile([32, 512], F32, "cR")
        sR = self.tile([32, 512], F32, "sR")
        qa_o = self.tile([128, 4, 512], BF16, "qa_o")
        ka_o = self.tile([128, 512], BF16, "ka_o")
        va_o = self.tile([128, 4, 128], BF16, "va_o")
        qm_o = self.tile([96, 8, 512], BF16, "qm_o")
        kn_o = self.tile([128, 4, 512], BF16, "kn_o")
        vm_o = self.tile([128, 4, 512], BF16, "vm_o")
        kr_o = self.tile([32, 512], BF16, "kr_o")
        sq = self.tile([128, 512], BF16, "sq")
        rs = self.tile([128, 512], F32, "rs")
        cqn = self.tile([128, 2, 512], BF16, "cqn")
        ckvn = self.tile([128, 512], BF16, "ckvn")
        ones, onk = C["onesb"]
        tts = self.ttiles()
        stx = {"ri": 0, "pend": None}

        def defer(tail):
            if stx["pend"] is not None:
                stx["pend"]()
            stx["pend"] = tail

        def flush():
            if stx["pend"] is not None:
                stx["pend"]()
                stx["pend"] = None

        def next_asb():
            stx["ri"] += 1
            return asbs[stx["ri"] % 2]

        def stats(t):
            tok0, Tw = tts[t]
            self.norm_stats(self.xsrc(l, bl, tok0, Tw), Tw, nts[t % 2])

        def trans(t):
            tok0, Tw = tts[t]
            r = bl if tok0 < SEQ else 2
            self.norm_trans(Tw, cols[r][0], cols[r][1], nts[t % 2], hTs[t % 2])

        def projall(t):
            tok0, Tw = tts[t]
            ns = Tw // 128
            hTa, hk = hTs[t % 2]
            for (tab, nm_) in ((c64, "cos64"), (s64, "sin64"), (cM, "cosM"), (sM, "sinM"), (cR, "cosR"), (sR, "sinR")):
                P = self.din[nm_].shape[0]
                self.dma(tab[0][0:P, 0:Tw], self.din[nm_][:, tok0:tok0 + Tw], (), [tab[1]])

            def proj(col0, M, ps, pk, rhs_ap=None, rkeys=None, lw=None):
                for c in range(8):
                    self.mm(ps[0:M, 0:Tw], win[:, c, col0:col0 + M], hTa[:, c, 0:Tw], c == 0, c == 7, [wk, hk + str(c)], [pk])
            for m in range(4):
                ps, pk = self.bank()
                proj(m * 128, 128, ps, pk)
                defer(self.rope(ps, 128, Tw, tok0, next_asb(), C["P64"], None, None, (c64, s64), qa_o[0][:, m, 0:Tw], qa_o[1], [pk]))
            flush()
            self.dma(self.QA[bl, :, tok0:tok0 + Tw].rearrange("(m p) t -> p m t", p=128), qa_o[0][:, :, 0:Tw], [qa_o[1]], ["QA"])
            ps, pk = self.bank()
            proj(512, 128, ps, pk)
            defer(self.rope(ps, 128, Tw, tok0, next_asb(), C["P64"], None, None, (c64, s64), ka_o[0][:, 0:Tw], ka_o[1], [pk]))
            flush()
            self.dma(self.KA[bl, :, tok0:tok0 + Tw], ka_o[0][:, 0:Tw], [ka_o[1]], ["KA"])
            ps, pk = self.bank()
            for s in range(ns):
                for c in range(8):
                    self.mm(ps[:, s * 128:(s + 1) * 128], hTa[:, c, s * 128:(s + 1) * 128], win[:, c, 640:768], c == 0, c == 7,
                            [wk, hk + str(c)], [pk])
            self.cp("act", va_o[0][:, 0:ns, :], ps[:, 0:ns * 128].rearrange("p (s n) -> p s n", s=ns), [pk], [va_o[1]])
            self.dma(self.VA[bl, tok0:tok0 + Tw, :].rearrange("(s p) n -> p s n", p=128), va_o[0][:, 0:ns, :], [va_o[1]], ["VA"])
            pcq = []
            psN, pnk = self.bank()
            for j in range(2):
                ps, pk = self.bank()
                proj(768 + j * 128, 128, ps, pk)
                pcq.append((ps, pk))
                self.act(sq[0][:, 0:Tw], ps[:, 0:Tw], AF.Square, [pk], [sq[1]])
                self.mm(psN[:, 0:Tw], ones, sq[0][:, 0:Tw], j == 0, j == 1, [onk, sq[1]], [pnk])
            self.act(rs[0][:, 0:Tw], psN[:, 0:Tw], AF.Sqrt, [pnk], [rs[1]], scale=1.0 / 256, bias=self.epsap(EPS))
            self.recip(rs[0][:, 0:Tw], rs[0][:, 0:Tw], [rs[1]], [rs[1]])
            for j in range(2):
                self.stt("dve", cqn[0][:, j, 0:Tw], pcq[j][0][:, 0:Tw], qn[:, j:j + 1], rs[0][:, 0:Tw], ALU.mult, ALU.mult,
                         [pcq[j][1], qnk, rs[1]], [cqn[1]])
            for h in range(8):
                ps, pk = self.bank()
                for j in range(2):
                    self.mm(ps[0:96, 0:Tw], wuq[:, j, h * 96:(h + 1) * 96], cqn[0][:, j, 0:Tw], j == 0, j == 1, [wuqk, cqn[1]], [pk])
                defer(self.rope(ps, 96, Tw, tok0, next_asb(), C["PM"], None, None, (cM, sM), qm_o[0][:, h, 0:Tw], qm_o[1], [pk]))
            flush()
            self.dma(self.QM[bl, :, :, tok0:tok0 + Tw].rearrange("h p t -> p h t"), qm_o[0][:, :, 0:Tw], [qm_o[1]], ["QM"])
            ps, pk = self.bank()
            proj(1024, 128, ps, pk)
            self.act(sq[0][:, 0:Tw], ps[:, 0:Tw], AF.Square, [pk], [sq[1]])
            psN, pnk = self.bank()
            self.mm(psN[:, 0:Tw], ones, sq[0][:, 0:Tw], True, True, [onk, sq[1]], [pnk])
            self.act(rs[0][:, 0:Tw], psN[:, 0:Tw], AF.Sqrt, [pnk], [rs[1]], scale=1.0 / 128, bias=self.epsap(EPS))
            self.recip(rs[0][:, 0:Tw], rs[0][:, 0:Tw], [rs[1]], [rs[1]])
            self.stt("dve", ckvn[0][:, 0:Tw], ps[:, 0:Tw], kvn[:, 0:1], rs[0][:, 0:Tw], ALU.mult, ALU.mult, [pk, kvnk, rs[1]], [ckvn[1]])
            for m in range(4):
                ps, pk = self.bank()
                self.mm(ps[:, 0:Tw], wkk[:, m * 128:(m + 1) * 128], ckvn[0][:, 0:Tw], True, True, [wkkk, ckvn[1]], [pk])
                self.cp("act" if m % 2 == 0 else "dve", kn_o[0][:, m, 0:Tw], ps[:, 0:Tw], [pk], [kn_o[1]])
            self.dma(self.KN[bl, :, tok0:tok0 + Tw].rearrange("(m p) t -> p m t", p=128), kn_o[0][:, :, 0:Tw], [kn_o[1]], ["KN"])
            for s in range(ns):
                ps, pk = self.bank()
                self.mm(ps[:, :], ckvn[0][:, s * 128:(s + 1) * 128], wkv[:, :], True, True, [wkvk, ckvn[1]], [pk])
                self.cp("act" if s % 2 == 0 else "dve", vm_o[0][:, s, :], ps[:, :], [pk], [vm_o[1]])
            self.dma(self.VM[bl, tok0:tok0 + Tw, :].rearrange("(s p) n -> p s n", p=128), vm_o[0][:, 0:ns, :], [vm_o[1]], ["VM"])
            ps, pk = self.bank()
            proj(1152, 32, ps, pk)
            defer(self.rope(ps, 32, Tw, tok0, next_asb(), C["PR"], None, None, (cR, sR), kr_o[0][:, 0:Tw], kr_o[1], [pk]))
            flush()
            self.dma(self.KR[bl, :, tok0:tok0 + Tw], kr_o[0][:, 0:Tw], [kr_o[1]], ["KR"])

        stats(0)
        trans(0)
        for t in range(len(tts)):
            if t + 1 < len(tts):
                stats(t + 1)
            projall(t)
            if t + 1 < len(tts):
                trans(t + 1)
        S.barrier()

    def attn_block(self, Kt, Kkey, Krows, qrhs, Qkey, N, chunks, Vt, Vkey, scale, ptiles, acc, acck):
        n = len(chunks)
        for i, (kc, mask) in enumerate(chunks):
            ps, pk = self.bank_s()
            self.mm(ps[:, 0:N], Kt[0:Krows, kc * 128:(kc + 1) * 128], qrhs, True, True, [Kkey, Qkey], [pk])
            pt, ptk = ptiles[self.pti % len(ptiles)]
            self.pti += 1
            self.act(pt[:, 0:N], ps[:, 0:N], AF.Exp, [pk], [ptk], scale=scale)
            if mask is not None:
                self.tt("pool", pt[:, 0:N], pt[:, 0:N], mask[0][:, 0:N], ALU.mult, [ptk, mask[1]], [ptk])
            self.mm(acc[:, 0:N], Vt[:, kc, :], pt[:, 0:N], i == 0, i == n - 1, [Vkey, ptk], [acck])

    def bank_s(self):
        i = self.sbanks[self.sbi % len(self.sbanks)]
        self.sbi += 1
        return self.pb[i], "pb%d" % i

    def finish_div(self, acc, acck, N, ep, out, okey, sinkadd=None):
        (accs, ak), (rd0, rk) = ep
        if sinkadd is not None:
            self.tt("dve", accs[:, 0:N], acc[:, 0:N], sinkadd[0][:, 0:N], ALU.add, [acck, sinkadd[1]], [ak])
        else:
            self.cp("dve", accs[:, 0:N], acc[:, 0:N], [acck], [ak])
        self.recip(accs[64:128, 0:N], accs[64:128, 0:N], [ak], [ak])
        self.cp("pool", rd0[0:64, 0:N], accs[64:128, 0:N], [ak], [rk])
        self.tt("dve", out, accs[0:64, 0:N], rd0[0:64, 0:N], ALU.mult, [ak, rk], [okey])

    def pipeline(self, items, LA):
        n = len(items)
        for i in range(n + LA):
            if i < n:
                items[i][0]()
            if i >= LA:
                items[i - LA][1]()

    def p2_even(self, l, bl, ctx_out):
        self.reset(self.keep)
        S = self.S
        C = self.C
        LA = 3
        self.sbanks = [0, 1, 2, 3, 4]
        self.sbi = 0
        self.pti = 0
        accb = [(self.pb[5], "pb5"), (self.pb[6], "pb6"), (self.pb[7], "pb7")]
        ptiles = [self.tile([128, 512], BF16, "pt") for _ in range(LA + 3)]
        eps_ = [(self.tile([128, 512], F32, "accs"), self.tile([64, 512], F32, "rd0")) for _ in range(2)]
        otl = [self.tile([64, 512], BF16, "otile") for _ in range(3)]
        st = {"acci": 0, "oti": 0, "epi": 0}
        snk, snkk = self.tile([128, 8], F32, "snk")
        self.bcast(snk, snkk, self.din["swa_sink_%d" % l])
        self.act(snk, snk, AF.Exp, [snkk], [snkk])
        sinkadd = []
        for g in range(2):
            sa, sak = self.tile([128, 512], F32, "sinkadd")
            self.memset("pool", sa, 0.0, [sak])
            for j in range(4):
                self.ts("dve", sa[64:128, j * 128:(j + 1) * 128], sa[64:128, j * 128:(j + 1) * 128], snk[64:128, g * 4 + j:g * 4 + j + 1], None,
                        ALU.add, None, [sak, snkk], [sak])
            sinkadd.append((sa, sak))
        Qg = [self.tile([64, 4, T], BF16, "Qg") for _ in range(2)]
        Kg = [self.tile([96, T], BF16, "Kg") for _ in range(2)]
        Vg = [self.tile([128, NCH, 128], BF16, "Vg") for _ in range(2)]
        Qh = [self.tile([96, T], BF16, "Qh") for _ in range(2)]
        for v, vk in Vg:
            self.memset("pool", v[:, :, 64:128], 1.0, [vk + "o"])
        mprev, mnext = C["mprev"], C["mnext"]
        mscale = 96.0 ** -0.5
        items = []

        def make_items(kt, kkeys, krows, qrhs, qk, N, chunks, vt, vkeys, scale, fin):
            accs = {}
            n = len(chunks)
            for i, (kc, mask) in enumerate(chunks):
                cell = {}

                def A(kc=kc, mask=mask, cell=cell):
                    ps, pk = self.bank_s()
                    self.mm(ps[:, 0:N], kt[0:krows, kc * 128:(kc + 1) * 128], qrhs, True, True, list(kkeys) + [qk], [pk])
                    pt, ptk = ptiles[self.pti % len(ptiles)]
                    self.pti += 1
                    self.act(pt[:, 0:N], ps[:, 0:N], AF.Exp, [pk], [ptk], scale=scale)
                    if mask is not None:
                        self.tt("pool", pt[:, 0:N], pt[:, 0:N], mask[0][:, 0:N], ALU.mult, [ptk, mask[1]], [ptk])
                    cell["pt"] = (pt, ptk)

                def B(i=i, kc=kc, cell=cell):
                    if i == 0:
                        accs["a"] = accb[st["acci"] % 3]
                        st["acci"] += 1
                    acc, acck = accs["a"]
                    pt, ptk = cell["pt"]
                    self.mm(acc[:, 0:N], vt[:, kc, :], pt[:, 0:N], i == 0, i == n - 1, list(vkeys) + [ptk], [acck])
                    if i == n - 1:
                        fin(acc, acck)
                items.append((A, B))

        pt2s = [self.tile([128, 2, 512], BF16, "pt2") for _ in range(LA + 2)]
        st["wi"] = 0
        st["p2i"] = 0

        def make_pair_items(kt, kkeys, krows, qrhs, qk, N, chunks, vt, vkeys, scale, fin):
            accs = {}
            npair = len(chunks) // 2
            for i in range(npair):
                kc0, kc1 = chunks[2 * i], chunks[2 * i + 1]
                cell = {}

                def A(kc0=kc0, kc1=kc1, cell=cell):
                    j = st["wi"] % 2
                    st["wi"] += 1
                    pw, pwk = self.pbw[j], ["pb%d" % (2 * j), "pb%d" % (2 * j + 1)]
                    for m_, kc in enumerate((kc0, kc1)):
                        self.mm(pw[:, m_ * 512: m_ * 512 + N], kt[0:krows, kc * 128:(kc + 1) * 128], qrhs, True, True, list(kkeys) + [qk], [pwk[m_]])
                    pt, ptk = pt2s[st["p2i"] % len(pt2s)]
                    st["p2i"] += 1
                    pin = pw[:, :].rearrange("p (m n) -> p m n", m=2)[:, :, 0:N]
                    self.act(pt[:, :, 0:N], pin, AF.Exp, pwk, [ptk], scale=scale)
                    cell["pt"] = (pt, ptk)

                def B(i=i, kc0=kc0, kc1=kc1, cell=cell):
                    if i == 0:
                        accs["a"] = accb[st["acci"] % 3]
                        st["acci"] += 1
                    acc, acck = accs["a"]
                    pt, ptk = cell["pt"]
                    for m_, kc in enumerate((kc0, kc1)):
                        self.mm(acc[:, 0:N], vt[:, kc, :], pt[:, m_, 0:N], (i == 0 and m_ == 0), (i == npair - 1 and m_ == 1),
                                list(vkeys) + [ptk], [acck])
                    if i == npair - 1:
                        fin(acc, acck)
                items.append((A, B))

        def loads_swa(g):
            q, qk = Qg[g % 2]
            k, kk = Kg[g % 2]
            v, vk = Vg[g % 2]
            self.dma(q, self.QA[bl, g * 256:(g + 1) * 256, :].rearrange("(j p) t -> p j t", p=64), ["QA"], [qk])
            self.dma(k[0:64, :], self.KA[bl, g * 64:(g + 1) * 64, :], ["KA"], [kk])
            self.dma(v[:, :, 0:64], self.VA[bl, :, g * 64:(g + 1) * 64].rearrange("(c p) n -> p c n", p=128), ["VA"], [vk])

        def loads_mla(h):
            q, qk = Qh[h % 2]
            k, kk = Kg[h % 2]
            v, vk = Vg[h % 2]
            self.dma(q, self.QM[bl, h, :, :], ["QM"], [qk])
            self.dma(k[0:64, :], self.KN[bl, h * 64:(h + 1) * 64, :], ["KN"], [kk])
            self.dma(k[64:96, :], self.KR[bl, :, :], ["KR"], [kk + "r"])
            self.dma(v[:, :, 0:64], self.VM[bl, :, h * 64:(h + 1) * 64].rearrange("(c p) n -> p c n", p=128), ["VM"], [vk])

        def fin_swa(g, i):
            def f(acc, acck):
                ot, otk = otl[st["oti"] % 3]
                st["oti"] += 1
                ep = eps_[st["epi"] % 2]
                st["epi"] += 1
                self.finish_div(acc, acck, 512, ep, ot[:, :], otk, sinkadd=sinkadd[g])
                self.dma(self.OT[bl, g * 256:(g + 1) * 256, i * 128:(i + 1) * 128].rearrange("(j p) t -> p j t", p=64),
                         ot.rearrange("p (j t) -> p j t", j=4), [otk], ["OT"], q="poolq")
            return f

        def fin_mla(h, q0, N):
            def f(acc, acck):
                ot, otk = otl[st["oti"] % 3]
                st["oti"] += 1
                ep = eps_[st["epi"] % 2]
                st["epi"] += 1
                self.finish_div(acc, acck, N, ep, ot[:, 0:N], otk)
                self.dma(self.OT[bl, 512 + h * 64: 512 + (h + 1) * 64, q0:q0 + N], ot[:, 0:N], [otk], ["OT"], q="poolq")
            return f

        units = [("swa", 0), ("swa", 1)] + [("mla", h) for h in range(8)]

        def emit_loads(u):
            kind, idx = units[u]
            (loads_swa if kind == "swa" else loads_mla)(idx)

        emit_loads(0)
        for u, (kind, idx) in enumerate(units):
            first = len(items)
            if kind == "swa":
                g = idx
                q, qk = Qg[g % 2]
                k, kk = Kg[g % 2]
                v, vk = Vg[g % 2]
                for i in range(34 if ctx_out else 32):
                    if i < 32:
                        chunks = []
                        if i > 0:
                            chunks.append((i - 1, mprev))
                        chunks.append((i, None))
                        if i < 31:
                            chunks.append((i + 1, mnext))
                        chunks += [(32, None), (33, None)]
                    else:
                        chunks = [(32, None), (33, None)]
                    make_items(k, [kk], 64, q[:, :, i * 128:(i + 1) * 128], qk, 512, chunks, v, [vk, vk + "o"], 0.125, fin_swa(g, i))
            else:
                h = idx
                q, qk = Qh[h % 2]
                k, kk = Kg[h % 2]
                v, vk = Vg[h % 2]
                qts = [(i * 512, 512, list(range(NCH))) for i in range(8)]
                if ctx_out:
                    qts.append((SEQ, 256, [32, 33]))
                for (q0, N, chunks) in qts:
                    make_pair_items(k, [kk, kk + "r"], 96, q[:, q0:q0 + N], qk, N, chunks, v, [vk, vk + "o"], mscale, fin_mla(h, q0, N))
            if u + 1 < len(units):
                A0, B0 = items[first + LA]

                def A0w(A0=A0, u=u):
                    emit_loads(u + 1)
                    A0()
                items[first + LA] = (A0w, B0)
        self.pipeline(items, LA)
        S.barrier()

    def attn_block2(self, Kt, Kkeys, Krows, qrhs, Qkey, N, chunks, Vt, Vkeys, scale, ptiles, acc, acck):
        n = len(chunks)
        for i, (kc, mask) in enumerate(chunks):
            ps, pk = self.bank_s()
            self.mm(ps[:, 0:N], Kt[0:Krows, kc * 128:(kc + 1) * 128], qrhs, True, True, list(Kkeys) + [Qkey], [pk])
            pt, ptk = ptiles[self.pti % len(ptiles)]
            self.pti += 1
            self.act(pt[:, 0:N], ps[:, 0:N], AF.Exp, [pk], [ptk], scale=scale)
            if mask is not None:
                self.tt("pool", pt[:, 0:N], pt[:, 0:N], mask[0][:, 0:N], ALU.mult, [ptk, mask[1]], [ptk])
            self.mm(acc[:, 0:N], Vt[:, kc, :], pt[:, 0:N], i == 0, i == n - 1, list(Vkeys) + [ptk], [acck])

    def p1_diff(self, l, bl):
        self.reset(self.keep)
        S = self.S
        C = self.C
        cols = self.mod_cols(l, 0, 1, "norm_mix")
        stg = self.tile([128, 4096], F32, "stg")
        win, wk = self.tile([128, 8, 3072], BF16, "win")
        wsrc = self.din["diff_w_in_%d" % l]
        for c in range(8):
            self.load_bf16(win[:, c, :], wk, wsrc[c * 128:(c + 1) * 128, :], stg, eng="pool" if c % 2 == 0 else "dve")
        nts = [(self.tile([128, 4, D], F32, "xt"), self.tile([128, D], BF16, "junk"), self.tile([128, 4], F32, "ssq"),
                self.tile([128, 4], F32, "rstd"), self.tile([128, 4, D], BF16, "xn")) for _ in range(2)]
        hTs = [self.tile([128, 8, 512], BF16, "hT") for _ in range(2)]
        asbs = [(self.tile([128, 512], BF16, "asb"), self.tile([128, 512], F32, "t1"), self.tile([128, 512], F32, "t2")) for _ in range(2)]
        c64 = self.tile([128, 512], F32, "c64")
        s64 = self.tile([128, 512], F32, "s64")
        q_o = self.tile([128, 8, 512], BF16, "q_o")
        k_o = self.tile([128, 8, 512], BF16, "k_o")
        v_o = self.tile([128, 4, D], BF16, "v_o")
        tts = self.ttiles()
        stx = {"ri": 0, "pend": None}

        def defer(tail):
            if stx["pend"] is not None:
                stx["pend"]()
            stx["pend"] = tail

        def flush():
            if stx["pend"] is not None:
                stx["pend"]()
                stx["pend"] = None

        def next_asb():
            stx["ri"] += 1
            return asbs[stx["ri"] % 2]

        def stats(t):
            tok0, Tw = tts[t]
            self.norm_stats(self.xsrc(l, bl, tok0, Tw), Tw, nts[t % 2])

        def trans(t):
            tok0, Tw = tts[t]
            r = bl if tok0 < SEQ else 2
            self.norm_trans(Tw, cols[r][0], cols[r][1], nts[t % 2], hTs[t % 2])

        def projall(t):
            tok0, Tw = tts[t]
            ns = Tw // 128
            hTa, hk = hTs[t % 2]
            for (tab, nm_) in ((c64, "cos64"), (s64, "sin64")):
                self.dma(tab[0][:, 0:Tw], self.din[nm_][:, tok0:tok0 + Tw], (), [tab[1]])
            for typ, (o_t, dst) in enumerate(((q_o, self.QD), (k_o, self.KD))):
                for m in range(8):
                    ps, pk = self.bank()
                    col0 = typ * 1024 + m * 128
                    for c in range(8):
                        self.mm(ps[:, 0:Tw], win[:, c, col0:col0 + 128], hTa[:, c, 0:Tw], c == 0, c == 7, [wk, hk + str(c)], [pk])
                    defer(self.rope(ps, 128, Tw, tok0, next_asb(), C["P64"], None, None, (c64, s64), o_t[0][:, m, 0:Tw], o_t[1], [pk]))
                flush()
                self.dma(dst[bl, :, :, tok0:tok0 + Tw].rearrange("h p t -> p h t"), o_t[0][:, :, 0:Tw], [o_t[1]], ["QKD%d" % typ])
            for s_ in range(ns):
                for half in range(2):
                    ps, pk = self.bank()
                    for c in range(8):
                        self.mm(ps[:, :], hTa[:, c, s_ * 128:(s_ + 1) * 128], win[:, c, 2048 + half * 512: 2048 + (half + 1) * 512],
                                c == 0, c == 7, [wk, hk + str(c)], [pk])
                    self.cp("act" if half == 0 else "dve", v_o[0][:, s_, half * 512:(half + 1) * 512], ps[:, :], [pk], [v_o[1]])
            self.dma(self.VD[bl, tok0:tok0 + Tw, :].rearrange("(s p) n -> p s n", p=128), v_o[0][:, 0:ns, :], [v_o[1]], ["VD"])

        stats(0)
        trans(0)
        for t in range(len(tts)):
            if t + 1 < len(tts):
                stats(t + 1)
            projall(t)
            if t + 1 < len(tts):
                trans(t + 1)
        S.barrier()

    def p2_diff(self, l, bl, ctx_out):
        self.reset(self.keep)
        S = self.S
        C = self.C
        lam_init = 0.8 - 0.6 * math.exp(-0.3 * l)
        ones, onk = C["onesb"]
        lt = [self.tile([128, 64], F32, "lam") for _ in range(4)]
        for (t_, nm_) in zip(lt, ("lq1", "lk1", "lq2", "lk2")):
            self.bcast(t_[0], t_[1], self.din["%s_%d" % (nm_, l)])
        ls = self.tile([128, 2], F32, "ls")
        self.memset("pool", ls[0], 0.0, [ls[1]])
        lj = self.tile([128, 64], F32, "lj")
        for i in range(2):
            self.tt("dve", lj[0], lt[2 * i][0], lt[2 * i + 1][0], ALU.mult, [lt[2 * i][1], lt[2 * i + 1][1]], [lj[1]])
            self.S.op("dve", (lambda a, b: (lambda e: e.reduce_sum(out=a, in_=b, axis=AX.X)))(ls[0][:, i:i + 1], lj[0]), [lj[1]], [ls[1]])
        self.act(ls[0], ls[0], AF.Exp, [ls[1]], [ls[1]])
        nlam = self.tile([128, 1], F32, "nlam")
        self.tt("dve", nlam[0], ls[0][:, 1:2], ls[0][:, 0:1], ALU.subtract, [ls[1]], [nlam[1]])
        self.ts("dve", nlam[0], nlam[0], -lam_init, None, ALU.add, None, [nlam[1]], [nlam[1]])
        sub = self.tile([128, 1], F32, "sub")
        self.dma(sub[0], self.din["subln_%d" % l].rearrange("(c p) -> p c", p=128), (), [sub[1]], slow=True)
        self.ts("dve", sub[0], sub[0], 1.0 - lam_init, None, ALU.mult, None, [sub[1]], [sub[1]])

        LA = 2
        self.sbanks = [0, 1, 2, 3]
        self.sbi = 0
        self.pti = 0
        ptiles = [self.tile([128, 512], BF16, "pt") for _ in range(2 * (LA + 2))]
        Qh = [self.tile([128, T], BF16, "Qh") for _ in range(2)]
        Kh = [self.tile([128, T], BF16, "Kh") for _ in range(2)]
        Vh = [self.tile([128, NCH, 128], BF16, "Vh") for _ in range(2)]
        r1 = self.tile([128, 512], F32, "r1")
        r2 = self.tile([128, 512], F32, "r2")
        o1 = self.tile([128, 512], F32, "o1")
        o2 = self.tile([128, 512], F32, "o2")
        sq = self.tile([128, 512], BF16, "sq")
        otl = [self.tile([128, 512], BF16, "otile") for _ in range(2)]
        st = {"oti": 0}
        accs = [(self.pb[4], "pb4"), (self.pb[5], "pb5"), (self.pb[6], "pb6"), (self.pb[7], "pb7")]
        items = []

        def loads(h):
            q, qk = Qh[h % 2]
            k, kk = Kh[h % 2]
            v, vk = Vh[h % 2]
            self.dma(q, self.QD[bl, h, :, :], ["QKD0"], [qk])
            self.dma(k, self.KD[bl, h, :, :], ["QKD1"], [kk])
            self.dma(v, self.VD[bl, :, h * 128:(h + 1) * 128].rearrange("(c p) n -> p c n", p=128), ["VD"], [vk])

        def epilogue(h, q0, N):
            self.recip(r1[0][:, 0:N], accs[1][0][:, 0:N], [accs[1][1]], [r1[1]])
            self.recip(r2[0][:, 0:N], accs[3][0][:, 0:N], [accs[3][1]], [r2[1]])
            self.tt("dve", o1[0][:, 0:N], accs[0][0][:, 0:N], r1[0][:, 0:N], ALU.mult, [accs[0][1], r1[1]], [o1[1]])
            self.tt("dve", o2[0][:, 0:N], accs[2][0][:, 0:N], r2[0][:, 0:N], ALU.mult, [accs[2][1], r2[1]], [o2[1]])
            self.stt("dve", o1[0][:, 0:N], o2[0][:, 0:N], nlam[0][:, 0:1], o1[0][:, 0:N], ALU.mult, ALU.add, [o2[1], nlam[1], o1[1]], [o1[1]])
            self.act(sq[0][:, 0:N], o1[0][:, 0:N], AF.Square, [o1[1]], [sq[1]])
            psN, pnk = self.bank_s()
            self.mm(psN[:, 0:N], ones, sq[0][:, 0:N], True, True, [onk, sq[1]], [pnk])
            self.act(r1[0][:, 0:N], psN[:, 0:N], AF.Sqrt, [pnk], [r1[1]], scale=1.0 / 128, bias=self.epsap(1e-5))
            self.recip(r1[0][:, 0:N], r1[0][:, 0:N], [r1[1]], [r1[1]])
            ot, otk = otl[st["oti"] % 2]
            st["oti"] += 1
            self.stt("dve", ot[:, 0:N], o1[0][:, 0:N], sub[0][:, 0:1], r1[0][:, 0:N], ALU.mult, ALU.mult, [o1[1], sub[1], r1[1]], [otk])
            self.dma(self.OT[bl, h * 128:(h + 1) * 128, q0:q0 + N], ot[:, 0:N], [otk], ["OT"], q="poolq")

        loads(0)
        for h in range(8):
            q, qk = Qh[h % 2]
            k, kk = Kh[h % 2]
            v, vk = Vh[h % 2]
            first = len(items)
            qts = [(i * 512, 512, list(range(NCH))) for i in range(8)]
            if ctx_out:
                qts.append((SEQ, 256, [32, 33]))
            for (q0, N, chunks) in qts:
                n = len(chunks)
                for i, kc in enumerate(chunks):
                    cell = {}

                    def A(kc=kc, cell=cell, q=q, k=k, qk=qk, kk=kk, q0=q0, N=N):
                        pts = []
                        for mp in range(2):
                            ps, pk = self.bank_s()
                            self.mm(ps[:, 0:N], k[mp * 64:(mp + 1) * 64, kc * 128:(kc + 1) * 128], q[mp * 64:(mp + 1) * 64, q0:q0 + N], True, True,
                                    [kk, qk], [pk])
                            pt, ptk = ptiles[self.pti % len(ptiles)]
                            self.pti += 1
                            self.act(pt[:, 0:N], ps[:, 0:N], AF.Exp, [pk], [ptk], scale=0.125)
                            pts.append((pt, ptk))
                        cell["pts"] = pts

                    def B(i=i, n=n, kc=kc, cell=cell, v=v, vk=vk, h=h, q0=q0, N=N):
                        for mp in range(2):
                            pt, ptk = cell["pts"][mp]
                            self.mm(accs[2 * mp][0][:, 0:N], v[:, kc, :], pt[:, 0:N], i == 0, i == n - 1, [vk, ptk], [accs[2 * mp][1]])
                            self.mm(accs[2 * mp + 1][0][:, 0:N], ones, pt[:, 0:N], i == 0, i == n - 1, [onk, ptk], [accs[2 * mp + 1][1]])
                        if i == n - 1:
                            epilogue(h, q0, N)
                    items.append((A, B))
            if h + 1 < 8:
                A0, B0 = items[first + LA]

                def A0w(A0=A0, h=h):
                    loads(h + 1)
                    A0()
                items[first + LA] = (A0w, B0)
        self.pipeline(items, LA)
        S.barrier()

    def p3(self, l, bl, last):
        self.reset(self.keep)
        S = self.S
        C = self.C
        stg = self.tile([128, 4096], F32, "stg")
        nf, nfk = self.tile([128, D], F32, "nf")
        self.bcast(nf, nfk, self.din["norm_ffn_%d" % l])
        wo = {}
        g2 = {}
        s2 = {}
        wsrc = self.din["w_out_%d" % l]
        for r in ((bl,) if last else (bl, 2)):
            gt, gk = self.tile([128, D], F32, "gate")
            self.bcast(gt, gk, self.modv(l, r, 2))
            w_, wk_ = self.tile([128, 8, D], BF16, "wo")
            for c in range(8):
                sap, skey = stg
                self.dma(sap[:, 0:D], wsrc[c * 128:(c + 1) * 128, :], (), [skey])
                self.tt("dve" if c % 2 == 0 else "pool", w_[:, c, :], sap[:, 0:D], gt, ALU.mult, [skey, gk], [wk_])
            wo[r] = (w_, wk_)
            a, ak = self.tile([128, D], F32, "gs2")
            self.bcast(a, ak, self.modv(l, r, 4))
            self.stt("dve", a, a, 1.0, nf, ALU.add, ALU.mult, [ak, nfk], [ak])
            g2[r] = (a, ak)
            b, bk = self.tile([128, D], F32, "sh2")
            self.bcast(b, bk, self.modv(l, r, 3))
            s2[r] = (b, bk)
        wr, wrk = self.tile([128, 8, 36], F32, "wr")
        self.dma(wr, self.din["router_w_%d" % l].rearrange("(c p) n -> p c n", p=128), (), [wrk])
        rb, rbk = self.tile([128, 36], F32, "rb")
        self.bcast(rb, rbk, self.din["router_b_%d" % l])
        base, bsk = C["base"]
        if bl == 0:
            self.memset("pool", base, 0.0, [bsk])
        ots = [self.tile([128, 8, 128], BF16, "ot") for _ in range(3)]
        xts = [self.tile([128, D], F32, "xt") for _ in range(3)]
        junk = self.tile([128, D], BF16, "junk")
        h2s = [self.tile([128, D], F32, "h2") for _ in range(3)]
        h2b = [self.tile([128, D], BF16, "h2b") for _ in range(2)]
        h2Ts = [self.tile([128, 8, 128], F32, "h2T") for _ in range(2)]
        sms = [{nm_: self.tile([128, w_], F32, nm_) for nm_, w_ in (("lg", 36), ("gmax", 1), ("ngmax", 1), ("ohg", 4),
                                                                    ("gsum", 1), ("pgrp", 1), ("negm", 32), ("elm", 32), ("top8", 8),
                                                                    ("ex", 1), ("den", 1), ("A", 32), ("posv", 32),
                                                                    ("junk32", 32), ("junk32b", 32), ("nm1", 1))} for _ in range(2)]
        ssqs = [self.tile([128, 1], F32, "ssq") for _ in range(3)]
        rstds = [self.tile([128, 1], F32, "rstd") for _ in range(3)]
        Abs = [self.tile([128, NE], BF16, "Ab") for _ in range(3)]
        identf, ifk = C["identf"]
        triU, tuk = C["triU"]
        ones, onk = C["onesb"]
        rt_gate, rgk = C["rt_gate"]
        rt_oh, rohk = C["rt_oh"]
        rt_pos, rpk = C["rt_pos"]
        ntile = 32 if last else NCH
        cells = {}

        def S1a(ti):
            tok0 = ti * 128
            r = bl if tok0 < SEQ else 2
            ot, otk = ots[ti % 3]
            xt, xk = xts[ti % 3]
            self.dma(ot, self.OT[bl, :, tok0:tok0 + 128].rearrange("(c p) t -> p c t", p=128), ["OT"], [otk])
            self.dma(xt, self.xsrc(l, bl, tok0, 128), (), [xk])
            w_, wk_ = wo[r]
            for half in range(2):
                ps, pk = self.bank()
                for c in range(8):
                    self.mm(ps[:, :], ot[:, c, :], w_[:, c, half * 512:(half + 1) * 512], c == 0, c == 7, [otk, wk_], [pk])
                self.tt("dve", xt[:, half * 512:(half + 1) * 512], xt[:, half * 512:(half + 1) * 512], ps[:, :], ALU.add, [xk, pk], [xk])
            self.dma(self.xs[bl, tok0:tok0 + 128, :], xt, [xk], ["xs"], q="poolq")
            ssq, ssk = ssqs[ti % 3]
            rstd, rsk_ = rstds[ti % 3]
            self.memset("pool", ssq, 0.0, [ssk])
            self.act(junk[0], xt, AF.Square, [xk], [junk[1], ssk], accum_out=ssq)
            self.act(rstd, ssq, AF.Sqrt, [ssk], [rsk_], scale=1.0 / D, bias=self.epsap(EPS))

        def S1b(ti):
            tok0 = ti * 128
            r = bl if tok0 < SEQ else 2
            xt, xk = xts[ti % 3]
            rstd, rsk_ = rstds[ti % 3]
            self.recip(rstd, rstd, [rsk_], [rsk_])
            h2, h2k = h2s[ti % 3]
            self.stt("dve", h2, xt, rstd[:, 0:1], g2[r][0], ALU.mult, ALU.mult, [xk, rsk_, g2[r][1]], [h2k])
            self.tt("pool", h2, h2, s2[r][0], ALU.add, [h2k, s2[r][1]], [h2k])
            hb, hbk = h2b[ti % 2]
            self.cp("act", hb, h2, [h2k], [hbk])
            self.dma(self.H2[bl, tok0:tok0 + 128, :], hb, [hbk], ["H2"], q="poolq")

        def S2a(ti):
            h2, h2k = h2s[ti % 3]
            h2T = h2Ts[ti % 2]
            for half in range(2):
                ps, pk = self.bank()
                for cc in range(4):
                    c = half * 4 + cc
                    self.tr(ps[:, cc * 128:(cc + 1) * 128], h2[:, c * 128:(c + 1) * 128], identf, [h2k, ifk], [pk], sig=(cc == 3))
                self.cp("act" if half == 0 else "dve", h2T[0][:, half * 4:(half + 1) * 4, :], ps[:, :].rearrange("p (c t) -> p c t", c=4), [pk],
                        [h2T[1] + str(half)])
            ps, pk = self.bank()
            for c in range(8):
                self.mm(ps[:, 0:36], h2T[0][:, c, :], wr[:, c, :], c == 0, c == 7, [h2T[1] + str(c // 4), wrk], [pk])
            cells[ti] = (ps, pk)

        def S2b(ti):
            gti = bl * NCH + ti
            sm = sms[ti % 2]
            t = lambda n_: sm[n_][0]
            k_ = lambda n_: sm[n_][1]
            ps, pk = cells.pop(ti)
            self.tt("dve", t("lg"), ps[:, 0:36], rb, ALU.add, [pk, rbk], [k_("lg")])
            self.S.op("dve", (lambda a, b: (lambda e: e.reduce_max(out=a, in_=b, axis=AX.X)))(t("gmax"), t("lg")[:, 0:4]), [k_("lg")], [k_("gmax")])
            self.ts("dve", t("ohg"), t("lg")[:, 0:4], t("gmax")[:, 0:1], None, ALU.is_equal, None, [k_("lg"), k_("gmax")], [k_("ohg")])
            self.ts("dve", t("ngmax"), t("gmax"), -1.0, None, ALU.mult, None, [k_("gmax")], [k_("ngmax")])
            self.memset("pool", t("gsum"), 0.0, [k_("gsum")])
            self.act(t("junk32")[:, 0:4], t("lg")[:, 0:4], AF.Exp, [k_("lg"), k_("ngmax")], [k_("junk32"), k_("gsum")], bias=t("ngmax")[:, 0:1],
                     accum_out=t("gsum"))
            self.ts("dve", t("negm").rearrange("p (g e) -> p g e", g=4), t("ohg").unsqueeze(2).to_broadcast([128, 4, 8]), -1.0, 1e30,
                    ALU.add, ALU.mult, [k_("ohg")], [k_("negm")])
            self.tt("dve", t("elm"), t("lg")[:, 4:36], t("negm"), ALU.add, [k_("lg"), k_("negm")], [k_("elm")])
            self.S.op("dve", (lambda a, b: (lambda e: e.max(out=a, in_=b)))(t("top8"), t("elm")), [k_("elm")], [k_("top8")])
            oh1 = rt_oh[:, gti, 0, :]
            oh2 = rt_oh[:, gti, 1, :]
            ohk = rohk + str(gti)
            self.ts("dve", oh1, t("elm"), t("top8")[:, 0:1], None, ALU.is_equal, None, [k_("elm"), k_("top8")], [ohk + "a"])
            self.ts("dve", oh2, t("elm"), t("top8")[:, 1:2], None, ALU.is_equal, None, [k_("elm"), k_("top8")], [ohk + "b"])
            self.ts("dve", t("nm1"), t("top8")[:, 0:1], -1.0, None, ALU.mult, None, [k_("top8")], [k_("nm1")])
            self.act(t("ex"), t("top8")[:, 1:2], AF.Exp, [k_("top8"), k_("nm1")], [k_("ex")], bias=t("nm1")[:, 0:1])

        def S2c(ti):
            gti = bl * NCH + ti
            sm = sms[ti % 2]
            t = lambda n_: sm[n_][0]
            k_ = lambda n_: sm[n_][1]
            oh1 = rt_oh[:, gti, 0, :]
            oh2 = rt_oh[:, gti, 1, :]
            ohk = rohk + str(gti)
            self.recip(t("pgrp"), t("gsum"), [k_("gsum")], [k_("pgrp")])
            self.ts("dve", t("den"), t("ex"), 1.0, None, ALU.add, None, [k_("ex")], [k_("den")])
            self.recip(t("den"), t("den"), [k_("den")], [k_("den")])
            self.tt("dve", rt_gate[:, gti, 0:1], t("pgrp"), t("den"), ALU.mult, [k_("pgrp"), k_("den")], [rgk + "a%d" % gti])
            self.tt("dve", rt_gate[:, gti, 1:2], rt_gate[:, gti, 0:1], t("ex"), ALU.mult, [rgk + "a%d" % gti, k_("ex")], [rgk + "b%d" % gti])
            self.tt("dve", t("A"), oh1, oh2, ALU.add, [ohk + "a", ohk + "b"], [k_("A")])
            Ab = Abs[ti % 3]
            self.cp("dve", Ab[0], t("A"), [k_("A")], [Ab[1]])

        def S3(ti):
            gti = bl * NCH + ti
            sm = sms[ti % 2]
            t = lambda n_: sm[n_][0]
            k_ = lambda n_: sm[n_][1]
            Ab = Abs[ti % 3]
            oh1 = rt_oh[:, gti, 0, :]
            oh2 = rt_oh[:, gti, 1, :]
            ohk = rohk + str(gti)
            psr, prk = self.bank()
            self.mm(psr[:, 0:NE], triU, Ab[0], True, True, [tuk, Ab[1]], [prk])
            psc, pck = self.bank()
            self.mm(psc[:, 0:NE], ones, Ab[0], True, True, [onk, Ab[1]], [pck])
            self.tt("dve", t("posv"), psr[:, 0:NE], base, ALU.add, [prk, bsk], [k_("posv")])
            self.tt("dve", base, base, psc[:, 0:NE], ALU.add, [bsk, pck], [bsk])
            for kk_, (ohx, okx) in enumerate(((oh1, ohk + "a"), (oh2, ohk + "b"))):
                self.tt("dve", t("junk32b"), ohx, t("posv"), ALU.mult, [okx, k_("posv")], [k_("junk32b")])
                self.S.op("dve", (lambda a, b: (lambda e: e.reduce_sum(out=a, in_=b, axis=AX.X)))(rt_pos[:, gti, kk_:kk_ + 1], t("junk32b")),
                          [k_("junk32b")], [rpk + "%d_%d" % (gti, kk_)])

        stages = [S1a, S1b, S2a, S2b, S2c, S3]
        for i in range(ntile + len(stages) - 1):
            for d, fn in enumerate(stages):
                if 0 <= i - d < ntile:
                    fn(i - d)
        S.barrier()

    def p3b(self, l, last):
        self.reset(self.keep)
        S = self.S
        C = self.C
        base, bsk = C["base"]
        rt_oh, rohk = C["rt_oh"]
        rt_pos, rpk = C["rt_pos"]
        rt_slot, rsk = C["rt_slot"]
        widx, wik = C["widx"]
        pidx, pik = C["pidx"]
        padded = self.tile([128, NE], F32, "padded")
        tmp = self.tile([128, NE], F32, "tmp")
        pstart = self.tile([128, NE], F32, "pstart")
        pend = self.tile([128, NE], F32, "pend")
        sbs = self.tile([128, NSB, NE], F32, "sbs")
        self.dma(sbs[0], self.din["sbstart"], (), [sbs[1]])
        cmp_ = self.tile([128, NSB, NE], F32, "cmp")
        self.tt("dve", cmp_[0], base.unsqueeze(1).to_broadcast([128, NSB, NE]), sbs[0], ALU.is_gt, [bsk, sbs[1]], [cmp_[1]])
        self.S.op("dve", (lambda a, b: (lambda e: e.reduce_sum(out=a, in_=b, axis=AX.X)))(tmp[0], cmp_[0].rearrange("p s e -> p e s")),
                  [cmp_[1]], [tmp[1]])
        self.ts("dve", padded[0], tmp[0], float(SBW), None, ALU.mult, None, [tmp[1]], [padded[1]])
        self.memset("pool", pstart[0], 0.0, [pstart[1]])
        for e in range(1, NE):
            self.tt("dve", pstart[0][:, e:e + 1], pstart[0][:, e - 1:e], padded[0][:, e - 1:e], ALU.add, [pstart[1], padded[1]], [pstart[1]])
        self.tt("dve", pend[0], pstart[0], padded[0], ALU.add, [pstart[1], padded[1]], [pend[1]])
        self.tt("dve", cmp_[0], pend[0].unsqueeze(1).to_broadcast([128, NSB, NE]), sbs[0], ALU.is_le, [pend[1], sbs[1]], [cmp_[1]])
        be = self.tile([128, NSB], F32, "be")
        self.S.op("dve", (lambda a, b: (lambda e: e.reduce_sum(out=a, in_=b, axis=AX.X)))(be[0], cmp_[0]), [cmp_[1]], [be[1]])
        self.ts("dve", be[0], be[0], float(NE - 1), None, ALU.min, None, [be[1]], [be[1]])
        self.ts("dve", be[0], be[0], 128.0, None, ALU.mult, None, [be[1]], [be[1]])
        self.ts("dve", be[0], be[0], pidx[:, 0:1], None, ALU.add, None, [be[1], pik], [be[1]])
        self.cp("dve", widx, be[0], [be[1]], [wik])
        sl = self.tile([128, 2], F32, "sl")
        j32 = self.tile([128, NE], F32, "j32")
        hbs = [self.tile([128, D], BF16, "hb") for _ in range(3)]
        it = 0
        for bl in range(NB):
            for ti in range(32 if last else NCH):
                gti = bl * NCH + ti
                hb, hbk = hbs[it % 3]
                it += 1
                self.dma(hb, self.H2[bl, ti * 128:(ti + 1) * 128, :], ["H2"], [hbk])
                for kk_ in range(2):
                    self.tt("dve", j32[0], rt_oh[:, gti, kk_, :], pstart[0], ALU.mult, [pstart[1]], [j32[1]])
                    self.S.op("dve", (lambda a, b: (lambda e: e.reduce_sum(out=a, in_=b, axis=AX.X)))(sl[0][:, kk_:kk_ + 1], j32[0]),
                              [j32[1]], [sl[1]])
                self.tt("dve", sl[0], sl[0], rt_pos[:, gti, :], ALU.add, [sl[1]], [sl[1]])
                self.cp("dve", rt_slot[:, gti, :], sl[0], [sl[1]], [rsk + str(gti)])
                for kk_ in range(2):
                    self.S.dma("poolq", (lambda o_, i_, s_: (lambda e: e.indirect_dma_start(
                        out=o_, out_offset=bass.IndirectOffsetOnAxis(ap=s_, axis=0), in_=i_, in_offset=None,
                        bounds_check=self.regs["bslot"], oob_is_err=False)))(self.XB, hb, rt_slot[:, gti, kk_:kk_ + 1]),
                        [hbk, rsk + str(gti)], ["XB"])
        S.barrier()

    def p4(self, l):
        self.reset(self.keep)
        S = self.S
        C = self.C
        identb, ik = C["identb"]
        widx, wik = C["widx"]
        w1b = [self.tile([128, 8, 512], BF16, "w1b") for _ in range(2)]
        w3b = [self.tile([128, 8, 512], BF16, "w3b") for _ in range(2)]
        w2b = [self.tile([128, 4, D], BF16, "w2b") for _ in range(2)]
        xbs = [self.tile([128, 4, D], BF16, "xb") for _ in range(2)]
        xbTs = [self.tile([128, 8, 512], BF16, "xbT") for _ in range(2)]
        gTs = [self.tile([128, 4, 512], BF16, "gT") for _ in range(2)]
        s1s = [self.tile([128, 512], F32, "s1") for _ in range(2)]
        youts = [self.tile([128, 4, D], F32, "yout") for _ in range(2)]
        W1, W3, W2 = self.din["w1_%d" % l], self.din["w3_%d" % l], self.din["w2_%d" % l]

        def LW(sb):
            for (dstt, src) in ((w1b[sb % 2], W1), (w3b[sb % 2], W3), (w2b[sb % 2], W2)):
                dst, dk = dstt
                dflat = dst.rearrange("p a b -> p (a b)")
                self.S.dma("poolq", (lambda o_, i_, s_: (lambda e: e.indirect_dma_start(
                    out=o_, out_offset=None, in_=i_, in_offset=bass.IndirectOffsetOnAxis(ap=s_, axis=0),
                    bounds_check=self.regs["bw"], oob_is_err=False)))(dflat, src, widx[:, sb:sb + 1]), [wik], [dk])

        def LX(sb):
            xb, xbk = xbs[sb % 2]
            self.dma(xb, self.XB[sb * SBW:(sb + 1) * SBW, :].rearrange("(s p) d -> p s d", p=128), ["XB"], [xbk])

        def Tr(sb):
            xb, xbk = xbs[sb % 2]
            xbT = xbTs[sb % 2]
            for cp_ in range(4):
                ps, pk = self.bank()
                psb = ps[:].bitcast(BF16)
                for cc in range(2):
                    c = cp_ * 2 + cc
                    for s_ in range(4):
                        self.tr(psb[:, cc * 512 + s_ * 128: cc * 512 + (s_ + 1) * 128], xb[:, s_, c * 128:(c + 1) * 128], identb, [xbk, ik], [pk],
                                sig=(cc == 1 and s_ == 3))
                ev = "act" if cp_ % 2 == 0 else "dve"
                self.cp(ev, xbT[0][:, cp_ * 2, :], psb[:, 0:512], [pk], [xbT[1] + str(cp_ * 2)])
                self.cp(ev, xbT[0][:, cp_ * 2 + 1, :], psb[:, 512:1024], [pk], [xbT[1] + str(cp_ * 2 + 1)])

        def H(sb):
            a1, k1 = w1b[sb % 2]
            a3, k3 = w3b[sb % 2]
            xbT = xbTs[sb % 2]
            gT = gTs[sb % 2]
            for f in range(4):
                ps1, pk1 = self.bank()
                for c in range(8):
                    self.mm(ps1[:, :], a1[:, c, f * 128:(f + 1) * 128], xbT[0][:, c, :], c == 0, c == 7, [k1, xbT[1] + str(c)], [pk1])
                ps3, pk3 = self.bank()
                for c in range(8):
                    self.mm(ps3[:, :], a3[:, c, f * 128:(f + 1) * 128], xbT[0][:, c, :], c == 0, c == 7, [k3, xbT[1] + str(c)], [pk3])
                s1, s1k = s1s[f % 2]
                self.act(s1, ps1[:, :], AF.Silu, [pk1], [s1k])
                self.tt("dve", gT[0][:, f, :], s1, ps3[:, :], ALU.mult, [s1k, pk3], [gT[1] + str(f)])

        def Y(sb):
            a2, k2 = w2b[sb % 2]
            gT = gTs[sb % 2]
            yo, yok = youts[sb % 2]
            for s_ in range(4):
                for half in range(2):
                    psy, pky = self.bank()
                    for f in range(4):
                        self.mm(psy[:, :], gT[0][:, f, s_ * 128:(s_ + 1) * 128], a2[:, f, half * 512:(half + 1) * 512], f == 0, f == 3,
                                [gT[1] + str(f), k2], [pky])
                    self.cp("act" if half == 0 else "dve", yo[:, s_, half * 512:(half + 1) * 512], psy[:, :], [pky], [yok])
            self.dma(self.YB[sb * SBW:(sb + 1) * SBW, :].rearrange("(s p) d -> p s d", p=128), yo, [yok], ["YB"])

        LW(0)
        LX(0)
        Tr(0)
        for sb in range(NSB):
            if sb + 1 < NSB:
                LW(sb + 1)
                LX(sb + 1)
            H(sb)
            if sb + 1 < NSB:
                Tr(sb + 1)
            Y(sb)
        S.barrier()

    def p5(self, l, last, final):
        self.reset(self.keep)
        S = self.S
        C = self.C
        rt_gslot, rgsk = C["rt_slot"]
        rt_gate, rgk = C["rt_gate"]
        m5 = {}
        for r in ((0, 1) if last else (0, 1, 2)):
            a, ak = self.tile([128, D], F32, "m5")
            self.bcast(a, ak, self.modv(l, r, 5))
            m5[r] = (a, ak)
        if final:
            fn, fnk = self.tile([128, D], F32, "fn")
            self.bcast(fn, fnk, self.din["final_norm"])
        NBUF = 3
        y1s = [self.tile([128, D], F32, "y1") for _ in range(NBUF)]
        y2s = [self.tile([128, D], F32, "y2") for _ in range(NBUF)]
        xts = [self.tile([128, D], F32, "xt") for _ in range(NBUF)]
        junk = self.tile([128, D], BF16, "junk")
        ssqs = [self.tile([128, 1], F32, "ssq") for _ in range(2)]
        rstds = [self.tile([128, 1], F32, "rstd") for _ in range(2)]
        tiles = [(bl, ti) for bl in range(NB) for ti in range(32 if last else NCH)]

        def loads(it):
            bl, ti = tiles[it]
            tok0 = ti * 128
            gti = bl * NCH + ti
            for (yy, yk), kk_ in ((y1s[it % NBUF], 0), (y2s[it % NBUF], 1)):
                self.S.dma("poolq", (lambda o_, i_, s_: (lambda e: e.indirect_dma_start(
                    out=o_, out_offset=None, in_=i_, in_offset=bass.IndirectOffsetOnAxis(ap=s_, axis=0),
                    bounds_check=self.regs["bslot"], oob_is_err=False)))(yy, self.YB, rt_gslot[:, gti, kk_:kk_ + 1]),
                    ["YB"], [yk])
            xt, xk = xts[it % NBUF]
            self.dma(xt, self.xs[bl, tok0:tok0 + 128, :], ["xs_t%d" % gti], [xk])

        def compute(it):
            bl, ti = tiles[it]
            tok0 = ti * 128
            r = bl if tok0 < SEQ else 2
            gti = bl * NCH + ti
            y1, y1k = y1s[it % NBUF]
            y2, y2k = y2s[it % NBUF]
            xt, xk = xts[it % NBUF]
            self.ts("dve", y1, y1, rt_gate[:, gti, 0:1], None, ALU.mult, None, [y1k], [y1k])
            self.stt("dve", y1, y2, rt_gate[:, gti, 1:2], y1, ALU.mult, ALU.add, [y2k, y1k], [y1k])
            self.tt("dve", y1, y1, m5[r][0], ALU.mult, [y1k, m5[r][1]], [y1k])
            self.tt("dve", xt, xt, y1, ALU.add, [xk, y1k], [xk])
            if final and tok0 < SEQ:
                ssq = ssqs[it % 2]
                rstd = rstds[it % 2]
                self.memset("pool", ssq[0], 0.0, [ssq[1]])
                self.act(junk[0], xt, AF.Square, [xk], [junk[1], ssq[1]], accum_out=ssq[0])
                self.act(rstd[0], ssq[0], AF.Sqrt, [ssq[1]], [rstd[1]], scale=1.0 / D, bias=self.epsap(EPS))
                self.recip(rstd[0], rstd[0], [rstd[1]], [rstd[1]])
                self.stt("dve", xt, xt, rstd[0][:, 0:1], fn, ALU.mult, ALU.mult, [xk, rstd[1], fnk], [xk])
                self.dma(self.out[bl, tok0:tok0 + 128, :], xt, [xk], ["out"])
            else:
                if tok0 < SEQ and not final and last:
                    self.dma(self.out[bl, tok0:tok0 + 128, :], xt, [xk], ["out"])
                self.dma(self.xs[bl, tok0:tok0 + 128, :], xt, [xk], ["xs_t%d" % gti])

        n = len(tiles)
        for it in range(min(NBUF - 1, n)):
            loads(it)
        for it in range(n):
            if it + NBUF - 1 < n:
                loads(it + NBUF - 1)
            compute(it)
        S.barrier()

    def zero_scratch(self):
        self.reset(self.keep)
        z, zk = self.tile([128, 8192], BF16, "zero")
        self.memset("pool", z, 0.0, [zk])
        XBv = self.XB.rearrange("(n p a) d -> n p (a d)", p=128, a=8)
        for i in range(NSLOT // 1024):
            self.dma(XBv[i], z, [zk], ["XB"])
        self.S.barrier()

    def build(self, stop=None):
        self.setup_consts()
        self.setup_eps()
        self.setup_regs()
        self.zero_scratch()

        def fin():
            self.S.drain()
            self.S.emit()
            return self.nc
        for l in self.layers:
            last = (l == DEPTH - 1)
            self.p0_mod(l)
            if stop == "p0":
                return fin()
            for bl in range(NB):
                if l % 2 == 0:
                    self.p1_even(l, bl)
                    if stop == "p1":
                        return fin()
                    self.p2_even(l, bl, not last)
                else:
                    self.p1_diff(l, bl)
                    if stop == "p1":
                        return fin()
                    self.p2_diff(l, bl, not last)
                if stop == "p2":
                    return fin()
            for bl in range(NB):
                self.p3(l, bl, last)
            if stop == "p3":
                return fin()
            self.p3b(l, last)
            if stop == "p3b":
                return fin()
            self.p4(l)
            if stop == "p4":
                return fin()
            self.p5(l, last, self.final and l == self.layers[-1])
        return fin()


_CONSTS = None


def make_in_maps(inp, layers, n_cores, x_override=None, ctx_override=None):
    global _CONSTS
    if _CONSTS is None:
        _CONSTS = _host_consts()
    shared = dict(_CONSTS)
    shared["final_norm"] = np.ascontiguousarray(inp["final_norm"], dtype=np.float32)
    for l in layers:
        shared.update(_layer_arrays(inp, l))
    x = inp["x"] if x_override is None else x_override
    ctx = inp["ctx"] if ctx_override is None else ctx_override
    maps = []
    for i in range(n_cores):
        m = dict(shared)
        m["x_in"] = np.ascontiguousarray(x[NB * i:NB * (i + 1)], dtype=np.float32)
        m["ctx_in"] = np.ascontiguousarray(ctx[NB * i:NB * (i + 1)], dtype=np.float32)
        cc = np.stack([inp["c"][NB * i], inp["c"][NB * i + 1], inp["c_ctx"]], axis=0).astype(np.float32)
        m["ccT"] = np.ascontiguousarray(cc.reshape(3, 8, 128).transpose(2, 1, 0))
        maps.append(m)
    return maps


def kernel(**inputs):
    inp = {k: np.asarray(v) for k, v in inputs.items()}
    layers = list(range(DEPTH))
    mk = MK(layers, final=True)
    nc = mk.build()
    maps = make_in_maps(inp, layers, 8)
    res = run_bass_kernel_spmd(nc, maps, core_ids=list(range(8)))
    out = np.concatenate([np.asarray(r["out"]) for r in res.results], axis=0)
    return out.astype(np.float32)
```
